# Optimizing a Trainium2 kernel written in Bass

```python
import math
import jax
import jax.numpy as jnp
from jax import lax
import numpy as np

D_MODEL = 1024
BATCH = 1
SEQ = 16384
DEPTH = 2

GRID_W = 64
CTX_LEN = 256
D_HEAD = 64
ROPE_THETA = 10000.0
EPS = 1e-6
QBLOCK = 128
NEG_INF = -1e30

NA_HEADS = 4
NA_KH = 8
NA_KW = 16
DIFF_HEADS = 4
DIFF_QK_DIM = 32
SWA_HEADS = 4
SWA_KV_HEADS = 2
SWA_WINDOW = 128
GQA_HEADS = 4
GQA_KV_HEADS = 2

N_BRANCH = 4
BRANCH_W = 4 * D_HEAD

IN_WIDTHS = (NA_HEADS * D_HEAD, NA_HEADS * D_HEAD, NA_HEADS * D_HEAD,
             DIFF_HEADS * 2 * DIFF_QK_DIM, DIFF_HEADS * 2 * DIFF_QK_DIM, DIFF_HEADS * D_HEAD,
             SWA_HEADS * D_HEAD, SWA_KV_HEADS * D_HEAD, SWA_KV_HEADS * D_HEAD,
             GQA_HEADS * D_HEAD, GQA_KV_HEADS * D_HEAD, GQA_KV_HEADS * D_HEAD)
IN_WIDTH = sum(IN_WIDTHS)

PEER_HEADS = 8
N_KEYS = 128
N_EXPERTS = N_KEYS * N_KEYS
PEER_KEY_DIM = 128
PEER_TOPK = 16
PEER_BLOCK = 128

kernel_name = 'hybrid_diffusion_trunk'


def rmsnorm(x, w):
    xf = x.astype(jnp.float32)
    y = xf * lax.rsqrt(jnp.mean(xf * xf, axis=-1, keepdims=True) + EPS)
    return (y * w.astype(jnp.float32)).astype(x.dtype)


def modulate(h, shift, scale):
    return h * (1 + scale) + shift


def axial_rope_tables(n_tokens, dim):
    t = jnp.arange(n_tokens, dtype=jnp.int32)
    row = (t // GRID_W).astype(jnp.float32)
    col = (t % GRID_W).astype(jnp.float32)
    n_freq = dim // 4
    inv_freq = ROPE_THETA ** (-jnp.arange(n_freq, dtype=jnp.float32) / n_freq)
    ang_r = row[:, None] * inv_freq[None, :]
    ang_c = col[:, None] * inv_freq[None, :]
    ang = jnp.concatenate([ang_r, ang_r, ang_c, ang_c], axis=-1)
    return jnp.cos(ang), jnp.sin(ang)


def apply_axial_rope(x, cos, sin):
    shape = (1, cos.shape[0]) + (1,) * (x.ndim - 3) + (cos.shape[1],)
    cos = cos.reshape(shape).astype(x.dtype)
    sin = sin.reshape(shape).astype(x.dtype)
    x1, x2, x3, x4 = jnp.split(x, 4, axis=-1)
    rot = jnp.concatenate([-x2, x1, -x4, x3], axis=-1)
    return x * cos + rot * sin


def attend(q, k, v, sink=None):
    B, Lq, Hq, Dh = q.shape
    Hkv = k.shape[2]
    G = Hq // Hkv
    qg = q.reshape(B, Lq, Hkv, G, Dh)
    s = jnp.einsum('bqhgd,bkhd->bhgqk', qg, k).astype(jnp.float32) * (Dh ** -0.5)
    if sink is not None:
        sk = jnp.broadcast_to(sink.astype(jnp.float32).reshape(1, Hkv, G, 1, 1), s.shape[:-1] + (1,))
        s = jnp.concatenate([s, sk], axis=-1)
    p = jax.nn.softmax(s, axis=-1)
    if sink is not None:
        p = p[..., :-1]
    o = jnp.einsum('bhgqk,bkhd->bqhgd', p.astype(v.dtype), v)
    return o.reshape(B, Lq, Hq * Dh)


def diff_attend(q, k, v, lam):
    s = jnp.einsum('bqhmd,bkhmd->bhmqk', q, k).astype(jnp.float32) * (q.shape[-1] ** -0.5)
    p = jax.nn.softmax(s, axis=-1)
    a = p[:, :, 0] - lam * p[:, :, 1]
    return jnp.einsum('bhqk,bkhd->bqhd', a.astype(v.dtype), v)


def blocked_queries(fn, q):
    B, S = q.shape[:2]
    nb = S // QBLOCK
    qb = jnp.moveaxis(q.reshape((B, nb, QBLOCK) + q.shape[2:]), 1, 0)
    out = jnp.moveaxis(lax.map(fn, qb), 0, 1)
    return out.reshape((B, S) + out.shape[3:])


def neighbourhood_attention(q, k, v, kc, vc, rpb):
    B, S, H, Dh = q.shape
    rows = S // GRID_W
    kh = min(NA_KH, rows)
    r = jnp.arange(rows)
    row_start = jnp.clip(r - kh // 2, 0, rows - kh)
    band_rows = row_start[:, None] + jnp.arange(kh)[None, :]
    qg = q.reshape(B, rows, GRID_W, H, Dh)
    kg = k.reshape(B, rows, GRID_W, H, Dh)[:, band_rows]
    vg = v.reshape(B, rows, GRID_W, H, Dh)[:, band_rows]
    scale = Dh ** -0.5
    s_loc = jnp.einsum('brqhd,brjchd->bhrqjc', qg, kg).astype(jnp.float32) * scale
    qcol = jnp.arange(GRID_W)
    col_start = jnp.clip(qcol - NA_KW // 2, 0, GRID_W - NA_KW)
    col_ok = (qcol[None, :] >= col_start[:, None]) & (qcol[None, :] < col_start[:, None] + NA_KW)
    roff = band_rows - r[:, None] + (NA_KH - 1)
    coff = jnp.clip(qcol[None, :] - qcol[:, None], -(NA_KW - 1), NA_KW - 1) + (NA_KW - 1)
    bias = rpb[:, roff[:, None, :, None], coff[None, :, None, :]]
    s_loc = jnp.where(col_ok[None, None, None, :, None, :], s_loc + bias[None].astype(jnp.float32), NEG_INF)
    s_loc = s_loc.reshape(B, H, rows, GRID_W, kh * GRID_W)
    s_ctx = jnp.einsum('brqhd,bkhd->bhrqk', qg, kc).astype(jnp.float32) * scale
    p = jax.nn.softmax(jnp.concatenate([s_loc, s_ctx], axis=-1), axis=-1).astype(v.dtype)
    n_loc = kh * GRID_W
    vg = vg.reshape(B, rows, n_loc, H, Dh)
    o = (jnp.einsum('bhrqk,brkhd->brqhd', p[..., :n_loc], vg)
         + jnp.einsum('bhrqk,bkhd->brqhd', p[..., n_loc:], vc))
    return o.reshape(B, S, H * Dh)


def swa_attention(q, k, v, kc, vc, sink):
    B, S, Hq, Dh = q.shape
    Hkv = k.shape[2]
    G = Hq // Hkv
    nb = S // QBLOCK
    span = QBLOCK + 2 * SWA_WINDOW
    pad = ((0, 0), (SWA_WINDOW, SWA_WINDOW), (0, 0), (0, 0))
    kp = jnp.pad(k, pad)
    vp = jnp.pad(v, pad)
    idx = jnp.arange(nb)[:, None] * QBLOCK + jnp.arange(span)[None, :]
    kb = kp[:, idx]
    vb = vp[:, idx]
    qb = q.reshape(B, nb, QBLOCK, Hkv, G, Dh)
    scale = Dh ** -0.5
    s_loc = jnp.einsum('bnqhgd,bnkhd->bhgnqk', qb, kb).astype(jnp.float32) * scale
    qpos = jnp.arange(nb)[:, None] * QBLOCK + jnp.arange(QBLOCK)[None, :]
    kpos = idx - SWA_WINDOW
    ok = ((kpos[:, None, :] >= 0) & (kpos[:, None, :] < S)
          & (jnp.abs(qpos[:, :, None] - kpos[:, None, :]) <= SWA_WINDOW))
    s_loc = jnp.where(ok[None, None, None], s_loc, NEG_INF)
    s_ctx = jnp.einsum('bnqhgd,bkhd->bhgnqk', qb, kc).astype(jnp.float32) * scale
    s_sink = jnp.broadcast_to(sink.astype(jnp.float32).reshape(1, Hkv, G, 1, 1, 1), s_loc.shape[:-1] + (1,))
    p = jax.nn.softmax(jnp.concatenate([s_loc, s_ctx, s_sink], axis=-1), axis=-1).astype(v.dtype)
    C = kc.shape[1]
    o = (jnp.einsum('bhgnqk,bnkhd->bnqhgd', p[..., :span], vb)
         + jnp.einsum('bhgnqk,bkhd->bnqhgd', p[..., span:span + C], vc))
    return o.reshape(B, S, Hq * Dh)


def project_heads(h, w_in):
    B, L, _ = h.shape
    offsets = [int(o) for o in np.cumsum(IN_WIDTHS)[:-1]]
    (na_q, na_k, na_v, df_q, df_k, df_v, sw_q, sw_k, sw_v,
     gq_q, gq_k, gq_v) = jnp.split(h @ w_in, offsets, axis=-1)
    hd = lambda t, n: t.reshape(B, L, n, D_HEAD)
    dq = lambda t: t.reshape(B, L, DIFF_HEADS, 2, DIFF_QK_DIM)
    return (hd(na_q, NA_HEADS), hd(na_k, NA_HEADS), hd(na_v, NA_HEADS),
            dq(df_q), dq(df_k), hd(df_v, DIFF_HEADS),
            hd(sw_q, SWA_HEADS), hd(sw_k, SWA_KV_HEADS), hd(sw_v, SWA_KV_HEADS),
            hd(gq_q, GQA_HEADS), hd(gq_k, GQA_KV_HEADS), hd(gq_v, GQA_KV_HEADS))


def diff_subln(o, w, lam_init):
    B, L, H, Dh = o.shape
    return (rmsnorm(o, w) * (1.0 - lam_init)).reshape(B, L, H * Dh)


def token_mixers(h, hc, w_in, rpb, lam_p, subln_w, sink, qk_norm_w, lam_init, cos, sin, cos_d, sin_d, need_ctx):
    (na_q, na_k, na_v, df_q, df_k, df_v, sw_q, sw_k, sw_v, gq_q, gq_k, gq_v) = project_heads(h, w_in)
    (na_qc, na_kc, na_vc, df_qc, df_kc, df_vc, sw_qc, sw_kc, sw_vc, gq_qc, gq_kc, gq_vc) = project_heads(hc, w_in)
    lf = lam_p.astype(jnp.float32)
    lam = jnp.exp(jnp.sum(lf[0] * lf[1])) - jnp.exp(jnp.sum(lf[2] * lf[3])) + lam_init

    y_na = neighbourhood_attention(na_q, na_k, na_v, na_kc, na_vc, rpb)

    dk_all = jnp.concatenate([apply_axial_rope(df_k, cos_d, sin_d), df_kc], axis=1)
    dv_all = jnp.concatenate([df_v, df_vc], axis=1)
    y_df = blocked_queries(lambda qb: diff_attend(qb, dk_all, dv_all, lam), apply_axial_rope(df_q, cos_d, sin_d))
    y_df = diff_subln(y_df, subln_w, lam_init)

    y_sw = swa_attention(apply_axial_rope(sw_q, cos, sin), apply_axial_rope(sw_k, cos, sin), sw_v, sw_kc, sw_vc, sink)

    gq_q = apply_axial_rope(rmsnorm(gq_q, qk_norm_w[0]), cos, sin)
    gk_c = rmsnorm(gq_kc, qk_norm_w[1])
    gk_all = jnp.concatenate([apply_axial_rope(rmsnorm(gq_k, qk_norm_w[1]), cos, sin), gk_c], axis=1)
    gv_all = jnp.concatenate([gq_v, gq_vc], axis=1)
    y_gq = blocked_queries(lambda qb: attend(qb, gk_all, gv_all), gq_q)

    ys = jnp.stack([y_na, y_df, y_sw, y_gq], axis=2)
    if not need_ctx:
        return ys, None
    yc_na = attend(na_qc, na_kc, na_vc)
    yc_df = diff_subln(diff_attend(df_qc, df_kc, df_vc, lam), subln_w, lam_init)
    yc_sw = attend(sw_qc, sw_kc, sw_vc, sink)
    yc_gq = attend(rmsnorm(gq_qc, qk_norm_w[0]), gk_c, gq_vc)
    ysc = jnp.stack([yc_na, yc_df, yc_sw, yc_gq], axis=2)
    return ys, ysc


def merge_branches(h, ys, w_branch, w_gate, b_gate, w_out):
    B, L, D = h.shape
    gates = jax.nn.sigmoid(h @ w_gate + b_gate).reshape(B, L, N_BRANCH, D)
    br = jnp.einsum('blnw,nwd->blnd', ys, w_branch)
    return jnp.sum(gates * br, axis=2) @ w_out


def peer_ffn(h, wq, keys, u, v):
    B, L, D = h.shape
    T = B * L
    hf = h.reshape(T, D)
    q = (hf @ wq).reshape(T, PEER_HEADS, 2, PEER_KEY_DIM)
    s = jnp.einsum('thpd,phnd->thpn', q, keys).astype(jnp.float32)
    top_s, top_i = lax.top_k(s, PEER_TOPK)
    cand_s = (top_s[:, :, 0, :, None] + top_s[:, :, 1, None, :]).reshape(T, PEER_HEADS, PEER_TOPK * PEER_TOPK)
    cand_i = (top_i[:, :, 0, :, None] * N_KEYS + top_i[:, :, 1, None, :]).reshape(T, PEER_HEADS, PEER_TOPK * PEER_TOPK)
    best_s, pos = lax.top_k(cand_s, PEER_TOPK)
    expert = jnp.take_along_axis(cand_i, pos, axis=-1)
    g = jax.nn.softmax(best_s, axis=-1).astype(h.dtype)
    nb = T // PEER_BLOCK

    def one_block(args):
        xb, eb, gb = args
        a = jax.nn.gelu(jnp.einsum('td,thkd->thk', xb, u[eb]))
        return jnp.einsum('thk,thkd->td', gb * a, v[eb])

    out = lax.map(one_block, (hf.reshape(nb, PEER_BLOCK, D),
                              expert.reshape(nb, PEER_BLOCK, PEER_HEADS, PEER_TOPK),
                              g.reshape(nb, PEER_BLOCK, PEER_HEADS, PEER_TOPK)))
    return out.reshape(B, L, D)


def setup_inputs(seed: int = 0) -> dict:
    key = jax.random.key(seed)
    ks = jax.random.split(key, 24)
    nrm = lambda k, shape, s: jax.random.normal(k, shape, jnp.float32) * s
    L = DEPTH
    D = D_MODEL
    return {
        'x': nrm(ks[0], (BATCH, SEQ, D), 1.0),
        'c': nrm(ks[1], (BATCH, D), 1.0),
        'ctx': nrm(ks[2], (BATCH, CTX_LEN, D), 1.0),
        'c_ctx': nrm(ks[3], (D,), 1.0),
        'norm1_w': 1.0 + nrm(ks[4], (L, D), 0.02),
        'norm2_w': 1.0 + nrm(ks[5], (L, D), 0.02),
        'ada_w': nrm(ks[6], (L, D, 6 * D), 0.5 * D ** -0.5),
        'ada_b': nrm(ks[7], (L, 6 * D), 0.01),
        'w_in': nrm(ks[8], (L, D, IN_WIDTH), D ** -0.5),
        'na_rpb': nrm(ks[9], (L, NA_HEADS, 2 * NA_KH - 1, 2 * NA_KW - 1), 0.02),
        'diff_lam': nrm(ks[10], (L, 4, DIFF_QK_DIM), 0.1),
        'diff_subln_w': 1.0 + nrm(ks[11], (L, D_HEAD), 0.02),
        'swa_sink': nrm(ks[12], (L, SWA_HEADS), 0.5),
        'gqa_qk_norm_w': 1.0 + nrm(ks[13], (L, 2, D_HEAD), 0.02),
        'w_branch': nrm(ks[14], (L, N_BRANCH, BRANCH_W, D), BRANCH_W ** -0.5),
        'w_gate': nrm(ks[15], (L, D, N_BRANCH * D), D ** -0.5),
        'b_gate': nrm(ks[16], (L, N_BRANCH * D), 0.01),
        'w_out': nrm(ks[17], (L, D, D), D ** -0.5),
        'peer_wq': nrm(ks[18], (L, D, PEER_HEADS * 2 * PEER_KEY_DIM), D ** -0.5),
        'peer_keys': nrm(ks[19], (L, 2, PEER_HEADS, N_KEYS, PEER_KEY_DIM), PEER_KEY_DIM ** -0.5),
        'peer_u': nrm(ks[20], (L, N_EXPERTS, D), D ** -0.5),
        'peer_v': nrm(ks[21], (L, N_EXPERTS, D), PEER_HEADS ** -0.5),
        'final_norm_w': 1.0 + nrm(ks[22], (D,), 0.02),
    }


def reference(x, c, ctx, c_ctx, norm1_w, norm2_w, ada_w, ada_b, w_in, na_rpb, diff_lam, diff_subln_w,
              swa_sink, gqa_qk_norm_w, w_branch, w_gate, b_gate, w_out, peer_wq, peer_keys, peer_u, peer_v,
              final_norm_w):
    S = x.shape[1]
    cos, sin = axial_rope_tables(S, D_HEAD)
    cos_d, sin_d = axial_rope_tables(S, DIFF_QK_DIM)
    xc = ctx
    for l in range(DEPTH):
        need_ctx = l < DEPTH - 1
        lam_init = 0.8 - 0.6 * math.exp(-0.3 * l)
        mod = jax.nn.silu(c) @ ada_w[l] + ada_b[l]
        sh1, sc1, g1, sh2, sc2, g2 = [m[:, None, :] for m in jnp.split(mod, 6, axis=-1)]
        modc = jax.nn.silu(c_ctx) @ ada_w[l] + ada_b[l]
        sh1c, sc1c, g1c, sh2c, sc2c, g2c = jnp.split(modc, 6, axis=-1)

        h = modulate(rmsnorm(x, norm1_w[l]), sh1, sc1)
        hc = modulate(rmsnorm(xc, norm1_w[l]), sh1c, sc1c)
        ys, ysc = token_mixers(h, hc, w_in[l], na_rpb[l], diff_lam[l], diff_subln_w[l], swa_sink[l],
                               gqa_qk_norm_w[l], lam_init, cos, sin, cos_d, sin_d, need_ctx)
        x = x + g1 * merge_branches(h, ys, w_branch[l], w_gate[l], b_gate[l], w_out[l])
        if need_ctx:
            xc = xc + g1c * merge_branches(hc, ysc, w_branch[l], w_gate[l], b_gate[l], w_out[l])

        h2 = modulate(rmsnorm(x, norm2_w[l]), sh2, sc2)
        x = x + g2 * peer_ffn(h2, peer_wq[l], peer_keys[l], peer_u[l], peer_v[l])
        if need_ctx:
            h2c = modulate(rmsnorm(xc, norm2_w[l]), sh2c, sc2c)
            xc = xc + g2c * peer_ffn(h2c, peer_wq[l], peer_keys[l], peer_u[l], peer_v[l])
    return rmsnorm(x, final_norm_w)
```

```python
import math
from contextlib import ExitStack

import numpy as np
import ml_dtypes
import concourse.bass as bass
import concourse.mybir as mybir
from concourse.bass_utils import run_bass_kernel_spmd

F32 = mybir.dt.float32
BF16 = mybir.dt.bfloat16
AF = mybir.ActivationFunctionType
ALU = mybir.AluOpType
AX = mybir.AxisListType

D = 1024
SEQ = 16384
NCORE = 8
TOK = SEQ // NCORE
CTX = 256
NTT = TOK + CTX
GW = 64
ROWS = TOK // GW
EPS = 1e-6
NEG = -30000.0
SC64 = 64 ** -0.5
SC32 = 32 ** -0.5
NWIN = ROWS + 14
SWT = TOK + 256


class Tk:
    __slots__ = ("w", "r")

    def __init__(self):
        self.w = None
        self.r = {}


class Sched:
    ENG = ("pe", "act", "dve", "pool", "sp")

    def __init__(self, ndma=32, same_engine_sync=True):
        self.ops = {e: [] for e in self.ENG}
        self.cnt = {e: 0 for e in self.ENG}
        self.seen = {e: {} for e in self.ENG}
        self.ndma = ndma
        self.dma_cnt = [0] * ndma
        self.dma_next = 0
        self.same = same_engine_sync
        self.ncc = 0

    def cc(self, fn, eng="pool"):
        idx = self.ncc
        self.ncc += 1
        self.ops[eng].append(("cc", fn, idx))

    def _deps(self, reads, writes):
        deps = {}

        def add(v):
            if v is None:
                return
            key, val = v
            if deps.get(key, 0) < val:
                deps[key] = val
        for t in reads:
            add(t.w)
        for t in writes:
            add(t.w)
            for k, v in t.r.items():
                add((k, v))
        return deps

    def _wait(self, eng, deps):
        for key, val in deps.items():
            if key == eng and (eng == "pe" or not self.same):
                continue
            if self.seen[eng].get(key, 0) >= val:
                continue
            self.seen[eng][key] = val
            self.ops[eng].append(("wait", key, val))

    def op(self, eng, fn, reads=(), writes=(), sig=True):
        self._wait(eng, self._deps(reads, writes))
        if sig:
            self.cnt[eng] += 1
            n = self.cnt[eng]
            self.ops[eng].append(("op", fn))
        else:
            n = self.cnt[eng] + 1
            self.ops[eng].append(("opq", fn))
        for t in reads:
            t.r[eng] = n
        for t in writes:
            t.w = (eng, n)
            t.r = {}

    def dma(self, eng, out_ap, in_ap, reads=(), writes=()):
        deps = self._deps(reads, writes)
        k = self.dma_next
        self.dma_next = (k + 1) % self.ndma
        key = ("dma", k)
        prev = self.dma_cnt[k] * 16
        if prev and deps.get(key, 0) < prev:
            deps[key] = prev
        self._wait(eng, deps)
        self.dma_cnt[k] += 1
        val = self.dma_cnt[k] * 16
        self.ops[eng].append(("dma", out_ap, in_ap, k))
        for t in reads:
            t.r[key] = val
        for t in writes:
            t.w = (key, val)
            t.r = {}

    def barrier(self):
        deps = {}
        for k in range(self.ndma):
            if self.dma_cnt[k]:
                deps[("dma", k)] = self.dma_cnt[k] * 16
        for e in self.ENG:
            if self.cnt[e]:
                deps[e] = self.cnt[e]
        for i in range(self.ncc):
            deps[("cc", i)] = 1
        for e in self.ENG:
            d = {k: v for k, v in deps.items() if k != e}
            self._wait(e, d)

    def emit(self, block, sems, dsems, ccsems=()):
        engobj = {"pe": "tensor", "act": "scalar", "dve": "vector", "pool": "gpsimd", "sp": "sync"}

        def semof(key):
            if isinstance(key, tuple):
                return dsems[key[1]] if key[0] == "dma" else ccsems[key[1]]
            return sems[key]

        def make(ename):
            ops = self.ops[ename]
            mysem = sems[ename]

            def body(eng):
                for o in ops:
                    if o[0] == "wait":
                        eng.wait_ge(semof(o[1]), o[2])
                    elif o[0] == "op":
                        o[1](eng).then_inc(mysem, 1)
                    elif o[0] == "opq":
                        o[1](eng)
                    elif o[0] == "cc":
                        o[1](eng).then_inc(ccsems[o[2]])
                    else:
                        eng.dma_start(out=o[1], in_=o[2]).then_inc(dsems[o[3]], 16)
            return body
        for ename in self.ENG:
            if self.ops[ename]:
                getattr(block, engobj[ename])(make(ename))


class Ring:
    def __init__(self, items):
        self.items = items
        self.i = 0

    def next(self):
        it = self.items[self.i]
        self.i = (self.i + 1) % len(self.items)
        return it


class B:
    __slots__ = ("a", "k")

    def __init__(self, a, k=None):
        self.a = a
        self.k = k if k is not None else Tk()


class KB:
    def __init__(self, layers_a, layers_b, final, dbg=False, own_blocks=4, fused=False):
        self.nc = bass.Bass("TRN2", target_bir_lowering=False)
        self.S = Sched()
        self.es = ExitStack()
        self.dbg = dbg
        self.dumps = []
        self.own_blocks = own_blocks
        self.fused = fused
        self.kvb = {}
        self.ins = {}
        self.build(layers_a, layers_b, final)

    def din(self, name, shape, dt=F32):
        if name not in self.ins:
            self.ins[name] = self.nc.dram_tensor(name, list(shape), dt, kind="ExternalInput").ap()
        return self.ins[name]

    def dout(self, name, shape, dt=F32):
        return self.nc.dram_tensor(name, list(shape), dt, kind="ExternalOutput").ap()

    def sb(self, name, shape, dt):
        return self.es.enter_context(self.nc.sbuf_tensor(name, list(shape), dt))

    def arena_reset(self):
        self.S.barrier()
        self.aoff = 0

    def al(self, shape, dt):
        n = 1
        for s in shape[1:]:
            n *= s
        words = n if dt == F32 else (n + 1) // 2
        a = self.arena[0:shape[0], self.aoff:self.aoff + words]
        self.aoff += words
        assert self.aoff <= self.AW, ("arena overflow", self.aoff)
        if dt != F32:
            a = a.bitcast(dt)
        if len(shape) == 3:
            a = a.rearrange("p (a b) -> p a b", a=shape[1])
        elif len(shape) == 4:
            a = a.rearrange("p (a b c) -> p a b c", a=shape[1], b=shape[2])
        return a

    def alb(self, shape, dt):
        return B(self.al(shape, dt))

    def ring(self, n, shape, dt):
        return Ring([self.alb(shape, dt) for _ in range(n)])

    def MM(self, out, lhsT, rhs, start, stop, R, W, sig=True):
        self.S.op("pe", lambda e: e.matmul(out, lhsT=lhsT, rhs=rhs, start=start, stop=stop, skip_group_check=True),
                  reads=R, writes=W, sig=sig)

    def TR(self, out, in_, ident, R, W, sig=True):
        self.S.op("pe", lambda e: e.transpose(out, in_, ident), reads=R, writes=W, sig=sig)

    def ACT(self, out, in_, func, R, W, bias=None, scale=None, accum=None):
        kw = {}
        if bias is not None:
            kw["bias"] = bias
        if scale is not None:
            kw["scale"] = scale
        if accum is not None:
            kw["accum_out"] = accum
        self.S.op("act", lambda e: e.activation(out=out, in_=in_, func=func, **kw), reads=R, writes=W)

    def TT(self, eng, out, in0, in1, op, R, W):
        self.S.op(eng, lambda e: e.tensor_tensor(out=out, in0=in0, in1=in1, op=op), reads=R, writes=W)

    def TS(self, eng, out, in0, s1, s2, op0, op1, R, W):
        if op1 is None:
            self.S.op(eng, lambda e: e.tensor_scalar(out=out, in0=in0, scalar1=s1, scalar2=None, op0=op0),
                      reads=R, writes=W)
        else:
            self.S.op(eng, lambda e: e.tensor_scalar(out=out, in0=in0, scalar1=s1, scalar2=s2, op0=op0, op1=op1),
                      reads=R, writes=W)

    def STT(self, out, in0, scalar, in1, op0, op1, R, W):
        self.S.op("dve", lambda e: e.scalar_tensor_tensor(out=out, in0=in0, scalar=scalar, in1=in1, op0=op0, op1=op1),
                  reads=R, writes=W)

    def CP(self, eng, out, in_, R, W):
        if eng == "act":
            self.S.op("act", lambda e: e.activation(out=out, in_=in_, func=AF.Copy), reads=R, writes=W)
        else:
            self.S.op(eng, lambda e: e.tensor_copy(out=out, in_=in_), reads=R, writes=W)

    def RCP(self, out, in_, R, W):
        self.S.op("dve", lambda e: e.reciprocal(out=out, in_=in_), reads=R, writes=W)

    def MSET(self, eng, ap, val, W):
        self.S.op(eng, lambda e: e.memset(ap, val), writes=W)

    def DMA(self, q, out, in_, R=(), W=()):
        self.S.dma(q, out, in_, reads=R, writes=W)

    @staticmethod
    def sap(base, dims):
        return bass.AP(base.tensor, base.offset, [list(base.ap[0])] + [list(d) for d in dims])

    def MAX8(self, out, in_, R, W):
        self.S.op("dve", lambda e: e.max(out=out, in_=in_), reads=R, writes=W)

    def MREP(self, out, rep, vals, R, W):
        self.S.op("dve", lambda e: e.match_replace(out=out, in_to_replace=rep, in_values=vals, imm_value=-1e30),
                  reads=R, writes=W)

    def dump(self, name, ap, k, shape, dt=F32):
        if not self.dbg:
            return
        o = self.dout("dbg_" + name, shape, dt)
        self.DMA("sp", o, ap, R=[k], W=[Tk()])

    def build(self, layers_a, layers_b, final):
        nc = self.nc
        self.banks = [B(self.es.enter_context(nc.psum_tensor("ps%d" % i, [128, 512], F32))) for i in range(8)]
        self.RS = Ring(self.banks[0:4])
        self.RO = Ring(self.banks[4:6])
        self.RX = Ring(self.banks[6:8])
        self.RS6 = Ring(self.banks[0:4] + self.banks[6:8])
        self.xT = self.sb("xT", [128, 8, NTT], F32)
        self.xk = [Tk() for _ in range(5)]
        self.hT = B(self.sb("hT", [128, 8, 512], BF16))
        self.cst = self.sb("cst", [128, 8], F32)
        self.cstk = Tk()
        self.identf = B(self.sb("identf", [128, 128], F32))
        self.identb = B(self.sb("identb", [128, 128], BF16))
        self.perm64 = B(self.sb("perm64", [128, 128], BF16))
        self.perm32 = B(self.sb("perm32", [128, 128], BF16))
        self.blk64 = B(self.sb("blk64", [128, 128], BF16))
        self.onesb = B(self.sb("onesb", [128, 128], BF16))
        self.vec = self.sb("vec", [128, 512], F32)
        self.veck = Tk()
        self.mod = B(self.sb("mod", [128, 48, 2], F32))
        self.drv = B(self.sb("drv", [128, 6, 8, 2], F32))
        self.ctxK = B(self.sb("ctxK", [128, 6, CTX], BF16))
        self.ctxV = B(self.sb("ctxV", [128, 2, 16, 128], BF16))
        self.keysT = B(self.sb("keysT", [128, 16, 128], BF16))
        self.AW = 27648
        self.arena = self.sb("arena", [128, self.AW], F32)
        self.aoff = 0

        S = self.S
        self.MSET("dve", self.cst[:, 0:1], EPS, [self.cstk])
        self.MSET("dve", self.cst[:, 1:2], 0.0, [self.cstk])
        self.MSET("dve", self.cst[:, 2:3], 1.0, [self.cstk])
        self.MSET("dve", self.onesb.a[:, :], 1.0, [self.onesb.k])
        cmat = self.din("cmat", [4, 128, 128])
        self.DMA("sp", self.identf.a[:, :], cmat[0], W=[self.identf.k])
        self.DMA("pool", self.identb.a[:, :], cmat[0], W=[self.identb.k])
        self.DMA("pool", self.perm64.a[:, :], cmat[1], W=[self.perm64.k])
        self.DMA("pool", self.perm32.a[:, :], cmat[2], W=[self.perm32.k])
        self.DMA("pool", self.blk64.a[:, :], cmat[3], W=[self.blk64.k])
        self.MSET("pool", self.ctxV.a[:, :, :, :], 1.0, [self.ctxV.k])

        xin = self.din("xT_in", [D, NTT])
        xv = xin.rearrange("(c p) t -> p c t", p=128)
        for b in range(5):
            t0, nt = self.blk(b)
            self.DMA("sp", self.xT[:, :, t0:t0 + nt], xv[:, :, t0:t0 + nt], W=[self.xk[b]])

        if self.fused:
            self.sel = self.sb("sel_sb", [128, 16], F32)
            self.selk = Tk()
            self.DMA("sp", self.sel[:, :], self.din("sel", [128, 16]), W=[self.selk])
            for l in (0, 1):
                self.layer_setup(l)
                outs = self.kv_outs(l)
                for b in range(4):
                    self.arena_reset()
                    self.block_kv(l, b, outs)
                self.exchange(l)
                self.arena_reset()
                self.block_kv(l, 4, None, ctx_only=True)
                for b in list(range(4)) + ([4] if l == 0 else []):
                    self.block_full(l, b)
            layers_b = [1]
        else:
            for l in layers_b:
                self.layer_setup(l)
                self.arena_reset()
                self.block_kv(l, 4, None, ctx_only=True)
                blocks = list(range(self.own_blocks)) + ([4] if l == 0 else [])
                for b in blocks:
                    self.block_full(l, b)
            for l in layers_a:
                self.layer_setup(l)
                outs = self.kv_outs(l)
                for b in range(self.own_blocks):
                    self.arena_reset()
                    self.block_kv(l, b, outs)
        if final:
            self.final_norm()
        elif layers_b:
            xo = self.dout("xT_out", [D, NTT]).rearrange("(c p) t -> p c t", p=128)
            for b in range(5):
                t0, nt = self.blk(b)
                self.DMA("sp", xo[:, :, t0:t0 + nt], self.xT[:, :, t0:t0 + nt], R=[self.xk[b]], W=[Tk()])
        S.barrier()
        sems = {e: self.es.enter_context(nc.semaphore("s_" + e)) for e in S.ENG}
        dsems = [self.es.enter_context(nc.semaphore("d%d" % i)) for i in range(S.ndma)]
        ccsems = [self.es.enter_context(nc.semaphore("c%d" % i)) for i in range(S.ncc)]
        block = self.es.enter_context(nc.Block())
        S.emit(block, sems, dsems, ccsems)
        self.es.close()

    def blk(self, b):
        return (b * 512, 512) if b < 4 else (TOK, CTX)

    def layer_setup(self, l):
        sfx = "_l%d" % l
        self.arena_reset()
        vec_d = self.din("vec" + sfx, [128, 512])
        self.DMA("sp", self.vec[:, :], vec_d, W=[self.veck])
        v = self.vec
        vk = self.veck
        keys_d = self.din("keysT" + sfx, [128, 16, 128])
        self.DMA("pool", self.keysT.a[:, :, :], keys_d, W=[self.keysT.k])
        sv = self.alb([128, 8, 2], F32)
        self.ACT(sv.a[:, :, 0], v[:, 104:112], AF.Silu, [vk], [sv.k])
        self.ACT(sv.a[:, :, 1], v[:, 112:120], AF.Silu, [vk], [sv.k])
        adaw = self.din("ada_w" + sfx, [D, 6 * D]).rearrange("(k p) n -> p k n", p=128)
        wr = self.ring(3, [128, 8, 128], F32)
        for j in range(48):
            wt = wr.next()
            self.DMA("sp", wt.a[:, :, :], adaw[:, :, j * 128:(j + 1) * 128], W=[wt.k])
            ps = self.RS.next()
            for k in range(8):
                self.MM(ps.a[:, 0:2], wt.a[:, k, :], sv.a[:, k, :], k == 0, k == 7, [wt.k, sv.k], [ps.k], sig=(k == 7))
            self.TS("dve", self.mod.a[:, j, :], ps.a[:, 0:2], v[:, 16 + j:17 + j], None, ALU.add, None,
                    [ps.k, vk], [self.mod.k])
        m = self.mod
        d = self.drv
        n1 = v[:, 0:8].unsqueeze(2).to_broadcast([128, 8, 2])
        n2 = v[:, 8:16].unsqueeze(2).to_broadcast([128, 8, 2])
        self.STT(d.a[:, 0, :, :], m.a[:, 8:16, :], 1.0, n1, ALU.add, ALU.mult, [m.k, vk], [d.k])
        self.CP("dve", d.a[:, 1, :, :], m.a[:, 0:8, :], [m.k], [d.k])
        self.CP("dve", d.a[:, 2, :, :], m.a[:, 16:24, :], [m.k], [d.k])
        self.STT(d.a[:, 3, :, :], m.a[:, 32:40, :], 1.0, n2, ALU.add, ALU.mult, [m.k, vk], [d.k])
        self.CP("dve", d.a[:, 4, :, :], m.a[:, 24:32, :], [m.k], [d.k])
        self.CP("dve", d.a[:, 5, :, :], m.a[:, 40:48, :], [m.k], [d.k])
        pr = self.alb([128, 64], F32)
        self.TT("dve", pr.a[:, 0:32], v[:, 128:160], v[:, 160:192], ALU.mult, [vk], [pr.k])
        self.TT("dve", pr.a[:, 32:64], v[:, 192:224], v[:, 224:256], ALU.mult, [vk], [pr.k])
        sm = self.alb([128, 4], F32)
        self.S.op("dve", lambda e: e.tensor_reduce(out=sm.a[:, 0:2], in_=pr.a[:, :].rearrange("p (a b) -> p a b", a=2),
                                                   axis=AX.X, op=ALU.add), reads=[pr.k], writes=[sm.k])
        self.ACT(sm.a[:, 2:4], sm.a[:, 0:2], AF.Exp, [sm.k], [sm.k])
        self.lamk = Tk()
        self.TT("dve", v[:, 256:257], sm.a[:, 2:3], sm.a[:, 3:4], ALU.subtract, [sm.k, vk], [self.lamk])
        lam_init = 0.8 - 0.6 * math.exp(-0.3 * l)
        self.lam_init = lam_init
        self.TS("dve", v[:, 256:257], v[:, 256:257], lam_init, None, ALU.add, None, [self.lamk], [self.lamk])
        self.ACT(v[:, 260:264], v[:, 120:124], AF.Exp, [vk], [self.lamk])
        self.dump("mod%d" % l, self.mod.a[:, :, :], self.mod.k, [128, 48, 2])

    def dr(self, which, c, ctx):
        return self.drv.a[:, which, c, (1 if ctx else 0):(2 if ctx else 1)]

    def norm_mod(self, b, wa, wb, tf, tb):
        t0, nt = self.blk(b)
        ctx = (b == 4)
        ps = self.RS.next()
        for c in range(8):
            sq = tb.next()
            self.ACT(sq.a[:, :nt], self.xT[:, c, t0:t0 + nt], AF.Square, [self.xk[b]], [sq.k])
            self.MM(ps.a[:, :nt], self.onesb.a[:, :], sq.a[:, :nt], c == 0, c == 7, [self.onesb.k, sq.k], [ps.k])
        rs = self.alb([128, 512], F32)
        self.ACT(rs.a[:, :nt], ps.a[:, :nt], AF.Sqrt, [ps.k, self.cstk], [rs.k], bias=self.cst[:, 0:1], scale=1.0 / D)
        self.RCP(rs.a[:, :nt], rs.a[:, :nt], [rs.k], [rs.k])
        for c in range(8):
            t = tf.next()
            self.TT("dve", t.a[:, :nt], self.xT[:, c, t0:t0 + nt], rs.a[:, :nt], ALU.mult, [self.xk[b], rs.k], [t.k])
            self.TS("pool", self.hT.a[:, c, :nt], t.a[:, :nt], self.dr(wa, c, ctx), self.dr(wb, c, ctx),
                    ALU.mult, ALU.add, [t.k, self.drv.k], [self.hT.k])

    def proj(self, wview, col0, nt, wring, ring=None):
        wt = wring.next()
        self.DMA("pool", wt.a[:, :, :], wview[:, :, col0:col0 + 128], W=[wt.k])
        ps = (ring or self.RX).next()
        for k in range(8):
            self.MM(ps.a[:, :nt], wt.a[:, k, :], self.hT.a[:, k, :nt], k == 0, k == 7, [wt.k, self.hT.k], [ps.k],
                    sig=(k == 7))
        return ps

    def rope(self, src, dst_ap, dst_k, nt, cos, sin, perm, tf):
        ps = self.RX.next()
        self.MM(ps.a[:, :nt], perm.a[:, :], src.a[:, :nt], True, True, [perm.k, src.k], [ps.k])
        t1 = tf.next()
        t2 = tf.next()
        self.TT("dve", t1.a[:, :nt], src.a[:, :nt], cos.a[:, :nt], ALU.mult, [src.k, cos.k], [t1.k])
        self.TT("dve", t2.a[:, :nt], ps.a[:, :nt], sin.a[:, :nt], ALU.mult, [ps.k, sin.k], [t2.k])
        self.TT("pool", dst_ap, t1.a[:, :nt], t2.a[:, :nt], ALU.add, [t1.k, t2.k], [dst_k])

    def qknorm(self, ps, nt, wcol, tf, tb, out):
        sq = tb.next()
        self.ACT(sq.a[:, :nt], ps.a[:, :nt], AF.Square, [ps.k], [sq.k])
        p2 = self.RX.next()
        self.MM(p2.a[:, :nt], self.blk64.a[:, :], sq.a[:, :nt], True, True, [self.blk64.k, sq.k], [p2.k])
        rs = tf.next()
        self.ACT(rs.a[:, :nt], p2.a[:, :nt], AF.Sqrt, [p2.k, self.cstk], [rs.k], bias=self.cst[:, 0:1], scale=1.0 / 64)
        self.RCP(rs.a[:, :nt], rs.a[:, :nt], [rs.k], [rs.k])
        t = tf.next()
        self.TT("dve", t.a[:, :nt], ps.a[:, :nt], rs.a[:, :nt], ALU.mult, [ps.k, rs.k], [t.k])
        self.TS("pool", out.a[:, :nt], t.a[:, :nt], self.vec[:, wcol:wcol + 1], None, ALU.mult, None,
                [t.k, self.veck], [out.k])

    def load_rope(self, b):
        if b == 4:
            return None
        t0, nt = self.blk(b)
        rt = self.din("ropeT", [4, 128, TOK])
        tabs = []
        for i in range(4):
            t = self.alb([128, 512], F32)
            self.DMA("sp", t.a[:, :], rt[i][:, t0:t0 + nt], W=[t.k])
            tabs.append(t)
        return tabs

    def dram(self, name, shape, dt):
        return self.nc.dram_tensor(name, list(shape), dt).ap()

    def kv_outs(self, l):
        sfx = "_l%d" % l
        if self.fused:
            d = {}
            d["s_kT"] = self.dram("s_kT" + sfx, [6 * 128, TOK], BF16)
            d["s_vd"] = self.dram("s_vd" + sfx, [8 * 128, 16 * 128], BF16)
            d["s_nv"] = self.dram("s_nv" + sfx, [4 * 64, ROWS * 128], BF16)
            d["s_sv"] = self.dram("s_sv" + sfx, [4 * 128, 16 * 128], BF16)
            d["g_kT"] = self.dram("g_kT" + sfx, [8 * 6 * 128, TOK], BF16)
            d["g_vd"] = self.dram("g_vd" + sfx, [8 * 8 * 128, 16 * 128], BF16)
            d["g_nv"] = self.dram("g_nv" + sfx, [8 * 4 * 64, ROWS * 128], BF16)
            d["g_sv"] = self.dram("g_sv" + sfx, [8 * 4 * 128, 16 * 128], BF16)
            d["naKwin"] = self.dram("naKwin" + sfx, [2, 128, NWIN * 64], BF16)
            d["naVwin"] = self.dram("naVwin" + sfx, [4, 64, NWIN, 128], BF16)
            d["swKwin"] = self.dram("swKwin" + sfx, [128, SWT], BF16)
            d["swVwin"] = self.dram("swVwin" + sfx, [4, 128, 18, 128], BF16)
            self.kvb[l] = d
            return dict(
                kT=d["s_kT"].rearrange("(c p) t -> c p t", p=128),
                vd=d["s_vd"].rearrange("(s p) (k c) -> s p k c", p=128, c=128),
                nv=d["s_nv"].rearrange("(h k) (r c) -> h k r c", k=64, c=128),
                sv=d["s_sv"].rearrange("(s p) (k c) -> s p k c", p=128, c=128),
            )
        return dict(
            kT=self.dout("kT_own" + sfx, [6, 128, TOK], BF16),
            vd=self.dout("VAd_own" + sfx, [8, 128, 16, 128], BF16),
            nv=self.dout("NV_own" + sfx, [4, 64, ROWS, 128], BF16),
            sv=self.dout("SV_own" + sfx, [4, 128, 16, 128], BF16),
        )

    def exchange(self, l):
        d = self.kvb[l]
        S = self.S
        S.barrier()
        for a, g in (("s_kT", "g_kT"), ("s_vd", "g_vd"), ("s_nv", "g_nv"), ("s_sv", "g_sv")):
            src, dst = d[a], d[g]
            S.cc((lambda src=src, dst=dst: lambda e: e.collective_compute(
                "AllGather", ALU.bypass, replica_groups=[list(range(NCORE))], ins=[src], outs=[dst]))())
        S.barrier()
        self.aoff = 0
        skT = d["s_kT"].rearrange("(c p) t -> c p t", p=128)
        snv = d["s_nv"].rearrange("(h k) (r c) -> h k r c", k=64, c=128)
        ssv = d["s_sv"].rearrange("(s p) (k c) -> s p k c", p=128, c=128)
        for c in range(2):
            self.DMA("sp", d["naKwin"][c][:, 448:448 + TOK], skT[c], W=[Tk()])
        for h in range(4):
            self.DMA("sp", d["naVwin"][h][:, 7:7 + ROWS, :], snv[h], W=[Tk()])
            self.DMA("sp", d["swVwin"][h][:, 1:17, :], ssv[h], W=[Tk()])
        self.DMA("sp", d["swKwin"][:, 128:128 + TOK], skT[4], W=[Tk()])
        gk = d["g_kT"].rearrange("(j c p) t -> p j c t", c=6, p=128)
        gn = d["g_nv"].rearrange("(j h k) (r c) -> k j h r c", h=4, k=64, c=128)
        gs = d["g_sv"].rearrange("(j s p) (t c) -> p j s t c", s=4, p=128, c=128)
        cring = self.ring(3, [128, 8, 896], BF16)
        aring = self.ring(2, [128, 896], F32)
        oring = self.ring(3, [128, 896], BF16)

        def select(cand, P, n, side, dst):
            cb = cring.next()
            self.DMA("sp", cb.a[0:P, :, 0:n], cand, W=[cb.k])
            acc = aring.next()
            self.TS("dve", acc.a[0:P, 0:n], cb.a[0:P, 0, 0:n], self.sel[0:P, side * 8:side * 8 + 1], None, ALU.mult, None,
                    [cb.k, self.selk], [acc.k])
            ob = oring.next()
            for j in range(1, 8):
                out = ob.a[0:P, 0:n] if j == 7 else acc.a[0:P, 0:n]
                self.STT(out, cb.a[0:P, j, 0:n], self.sel[0:P, side * 8 + j:side * 8 + j + 1], acc.a[0:P, 0:n],
                         ALU.mult, ALU.add, [cb.k, self.selk, acc.k], [ob.k if j == 7 else acc.k])
            self.DMA("sp", dst, ob.a[0:P, 0:n], R=[ob.k], W=[Tk()])
        for c in range(2):
            select(gk[:, :, c, TOK - 448:TOK], 128, 448, 0, d["naKwin"][c][:, 0:448])
            select(gk[:, :, c, 0:448], 128, 448, 1, d["naKwin"][c][:, 448 + TOK:448 + TOK + 448])
        select(gk[:, :, 4, TOK - 128:TOK], 128, 128, 0, d["swKwin"][:, 0:128])
        select(gk[:, :, 4, 0:128], 128, 128, 1, d["swKwin"][:, 128 + TOK:256 + TOK])
        for h in range(4):
            select(gn[:, :, h, ROWS - 7:ROWS, :].rearrange("k j r c -> k j (r c)"), 64, 896, 0,
                   d["naVwin"][h][:, 0:7, :].rearrange("k r c -> k (r c)"))
            select(gn[:, :, h, 0:7, :].rearrange("k j r c -> k j (r c)"), 64, 896, 1,
                   d["naVwin"][h][:, 7 + ROWS:14 + ROWS, :].rearrange("k r c -> k (r c)"))
            select(gs[:, :, h, 15, :], 128, 128, 0, d["swVwin"][h][:, 0, :])
            select(gs[:, :, h, 0, :], 128, 128, 1, d["swVwin"][h][:, 17, :])
        S.barrier()

    def block_kv(self, l, b, outs, ctx_only=False):
        sfx = "_l%d" % l
        t0, nt = self.blk(b)
        ctx = (b == 4)
        tf = self.ring(4, [128, 512], F32)
        tb = self.ring(3, [128, 512], BF16)
        wring = self.ring(3, [128, 8, 128], BF16)
        tabs = self.load_rope(b)
        self.norm_mod(b, 0, 1, tf, tb)
        win = self.din("w_in" + sfx, [D, 2560]).rearrange("(k p) n -> p k n", p=128)
        specs = [(256, None, False), (384, None, False), (1024, 32, False), (1152, 32, False),
                 (1792, 64, False), (2304, 64, True)]
        kst = self.ring(2, [128, 512], BF16)
        for ci, (col0, rk, nrm) in enumerate(specs):
            ps = self.proj(win, col0, nt, wring)
            if ctx:
                dst_ap, dst_k = self.ctxK.a[:, ci, :], self.ctxK.k
            else:
                st = kst.next()
                dst_ap, dst_k = st.a[:, :nt], st.k
            if nrm:
                xn = tb.next()
                self.qknorm(ps, nt, 125, tf, tb, xn)
                if ctx:
                    self.CP("act", dst_ap, xn.a[:, :nt], [xn.k], [dst_k])
                else:
                    self.rope(xn, dst_ap, dst_k, nt, tabs[0], tabs[1], self.perm64, tf)
            elif rk is None or ctx:
                self.CP("act", dst_ap, ps.a[:, :nt], [ps.k], [dst_k])
            else:
                xb = tb.next()
                self.CP("act", xb.a[:, :nt], ps.a[:, :nt], [ps.k], [xb.k])
                if rk == 64:
                    self.rope(xb, dst_ap, dst_k, nt, tabs[0], tabs[1], self.perm64, tf)
                else:
                    self.rope(xb, dst_ap, dst_k, nt, tabs[2], tabs[3], self.perm32, tf)
            if not ctx:
                self.DMA("sp", outs["kT"][ci][:, t0:t0 + nt], dst_ap, R=[dst_k], W=[Tk()])
        wv = self.din("w_v" + sfx, [D, 768]).rearrange("(k p) n -> p k n", p=128)
        wvt = self.alb([128, 8, 768], BF16)
        self.DMA("pool", wvt.a[:, :, :], wv, W=[wvt.k])
        if not ctx:
            vs = self.alb([128, 4, 16, 128], BF16)
            self.MSET("pool", vs.a[:, :, :, :], 1.0, [vs.k])
        for tt in range(nt // 128):
            pa = self.RX.next()
            pb = self.RX.next()
            for k in range(8):
                self.MM(pa.a[:, :512], self.hT.a[:, k, tt * 128:(tt + 1) * 128], wvt.a[:, k, 0:512], k == 0, k == 7,
                        [self.hT.k, wvt.k], [pa.k], sig=(k == 7))
            for k in range(8):
                self.MM(pb.a[:, :256], self.hT.a[:, k, tt * 128:(tt + 1) * 128], wvt.a[:, k, 512:768], k == 0, k == 7,
                        [self.hT.k, wvt.k], [pb.k], sig=(k == 7))
            if ctx:
                dst, dk = self.ctxV.a[:, tt, :, :], self.ctxV.k
            else:
                dst, dk = vs.a[:, tt, :, :], vs.k
            def hv(base_col, par, pa=pa):
                c0 = base_col + par * 64
                return self.sap(pa.a[:, c0:c0 + 1], [[128, 2], [1, 64]])
            def dv(slot0, par, dst=dst):
                return self.sap(dst[:, slot0 + par, par * 64:par * 64 + 1], [[256, 2], [1, 64]])
            self.CP("act", dv(8, 0), hv(0, 0), [pa.k], [dk])
            self.CP("dve", dv(8, 1), hv(0, 1), [pa.k], [dk])
            self.CP("act", dv(0, 0), hv(256, 0), [pa.k], [dk])
            self.CP("dve", dv(0, 1), hv(256, 1), [pa.k], [dk])
            def gsrc(base, pb=pb):
                return pb.a[:, base:base + 128].rearrange("p (g d) -> p g d", g=2)
            def gdst(slot0, par, dst=dst):
                return self.sap(dst[:, slot0 + par, par * 64:par * 64 + 1], [[256, 2], [1, 64]])
            self.CP("act", gdst(12, 0), gsrc(0), [pb.k], [dk])
            self.CP("dve", gdst(12, 1), gsrc(0), [pb.k], [dk])
            self.CP("act", gdst(4, 0), gsrc(128), [pb.k], [dk])
            self.CP("dve", gdst(4, 1), gsrc(128), [pb.k], [dk])
        if not ctx:
            kt0 = b * 4
            for s in range(8):
                self.DMA("sp", outs["vd"][s][:, kt0:kt0 + 4, :], vs.a[:, :, s, :], R=[vs.k], W=[Tk()])
            for s in range(4):
                self.DMA("sp", outs["sv"][s][:, kt0:kt0 + 4, :], vs.a[:, :, 12 + s, :], R=[vs.k], W=[Tk()])
                nvv = outs["nv"][s][:, 2 * kt0:2 * kt0 + 8, :].rearrange("k (t two) c -> two k t c", two=2)
                self.DMA("sp", nvv[0], vs.a[0:64, :, 8 + s, :], R=[vs.k], W=[Tk()])
                self.DMA("sp", nvv[1], vs.a[64:128, :, 8 + s, :], R=[vs.k], W=[Tk()])

    def finalize(self, O, par, nq, dst_ap, dst_k, tf, extra=None, mul=None):
        no = par * 64
        zo = (1 - par) * 64
        rz = tf.next()
        if extra is not None:
            self.TS("dve", rz.a[zo:zo + 64, :nq], O.a[zo:zo + 64, :nq], extra[zo:zo + 64, :], None, ALU.add, None,
                    [O.k, self.lamk], [rz.k])
            self.RCP(rz.a[zo:zo + 64, :nq], rz.a[zo:zo + 64, :nq], [rz.k], [rz.k])
        else:
            self.RCP(rz.a[zo:zo + 64, :nq], O.a[zo:zo + 64, :nq], [O.k], [rz.k])
        if mul is not None:
            self.TS("dve", rz.a[zo:zo + 64, :nq], rz.a[zo:zo + 64, :nq], mul[zo:zo + 64, :], None, ALU.mult, None,
                    [rz.k, self.lamk], [rz.k])
        self.TT("dve", dst_ap, O.a[no:no + 64, :nq], rz.a[zo:zo + 64, :nq], ALU.mult, [O.k, rz.k], [dst_k])

    def ctx_tiles(self, streams, O, nq, pt, scale, stop_last):
        for kt in range(2):
            for si, st in enumerate(streams):
                lo, hi = st["krows"]
                s = self.RS.next()
                self.MM(s.a[:, :nq], self.ctxK.a[lo:hi, st["ci"], kt * 128:(kt + 1) * 128], st["q"], True, True,
                        [self.ctxK.k, st["qk"]], [s.k])
                p = pt.next()
                self.ACT(p.a[:, :nq], s.a[:, :nq], AF.Exp, [s.k], [p.k], scale=scale)
                self.MM(O[si].a[:, :nq], self.ctxV.a[:, kt, st["cslot"], :], p.a[:, :nq], kt == 0,
                        stop_last and kt == 1, [self.ctxV.k, p.k], [O[si].k])

    def dense_pass(self, l, streams, nq, ci_d, vslots, pt, kring, vrings, scale):
        sfx = "_l%d" % l
        O = [self.RO.next() for _ in streams]
        self.ctx_tiles(streams, O, nq, pt, scale, False)
        NP = SEQ // 2048
        if self.fused:
            gk = self.kvb[l]["g_kT"].rearrange("(j c p) t -> j c p t", c=6, p=128)
            gv = self.kvb[l]["g_vd"].rearrange("(j s p) (k c) -> j s p k c", s=8, p=128, c=128)
            ksrc = lambda pc: gk[pc][(2, 3, 5)[ci_d]]
            vsrc = lambda vs, pc: gv[pc][vs]
        else:
            ktd = self.din("KTd" + sfx, [3, 128, SEQ], BF16)
            vad = self.din("VAd" + sfx, [8, 128, SEQ // 128, 128], BF16)
            ksrc = lambda pc: ktd[ci_d][:, pc * 2048:(pc + 1) * 2048]
            vsrc = lambda vs, pc: vad[vs][:, pc * 16:(pc + 1) * 16, :]

        def load(pc):
            kp = kring.next()
            self.DMA("sp", kp.a[:, :], ksrc(pc), W=[kp.k])
            vps = []
            for vi, vs in enumerate(vslots):
                vp = vrings[vi].next()
                self.DMA("sp", vp.a[:, :, :], vsrc(vs, pc), W=[vp.k])
                vps.append(vp)
            return kp, vps
        nxt = load(0)
        pend = []

        def flush():
            for (si, p, vt, stop) in pend:
                self.MM(O[si].a[:, :nq], vt[0], p.a[:, :nq], False, stop, [vt[1], p.k], [O[si].k])
            del pend[:]
        for pc in range(NP):
            kp, vps = nxt
            if pc + 1 < NP:
                flush()
                nxt = load(pc + 1)
            for kt in range(16):
                cur = []
                for si, st in enumerate(streams):
                    lo, hi = st["krows"]
                    s = self.RS6.next()
                    self.MM(s.a[:, :nq], kp.a[lo:hi, kt * 128:(kt + 1) * 128], st["q"], True, True, [kp.k, st["qk"]], [s.k])
                    p = pt.next()
                    self.ACT(p.a[:, :nq], s.a[:, :nq], AF.Exp, [s.k], [p.k], scale=scale)
                    vp = vps[st["vi"]]
                    cur.append((si, p, (vp.a[:, kt, :], vp.k), (pc == NP - 1 and kt == 15)))
                flush()
                pend.extend(cur)
        flush()
        return O

    def block_full(self, l, b):
        sfx = "_l%d" % l
        t0, nt = self.blk(b)
        ctx = (b == 4)
        v = self.vec
        self.arena_reset()
        qT = self.alb([128, 8, 512], BF16)
        ysT = self.alb([128, 8, 512], BF16)
        ysd = self.alb([128, 2, 512], F32)
        qz = self.alb([128, 2, 512], BF16)
        qpad = self.alb([128, 12, 512], BF16)
        keep = self.aoff
        tf = self.ring(4, [128, 512], F32)
        tb = self.ring(3, [128, 512], BF16)
        wring = self.ring(3, [128, 8, 128], BF16)
        tabs = self.load_rope(b)
        self.norm_mod(b, 0, 1, tf, tb)
        if self.dbg and b == 0:
            self.dump("h%d" % l, self.hT.a[:, :, :], self.hT.k, [128, 8, 512], BF16)
        win = self.din("w_in" + sfx, [D, 2560]).rearrange("(k p) n -> p k n", p=128)
        qspecs = [(0, None, False), (128, None, False), (768, 32, False), (896, 32, False),
                  (1536, 64, False), (1664, 64, False), (2048, 64, True), (2176, 64, True)]
        for qi, (col0, rk, nrm) in enumerate(qspecs):
            ps = self.proj(win, col0, nt, wring)
            dst_ap, dst_k = qT.a[:, qi, :nt], qT.k
            if nrm:
                xn = tb.next()
                self.qknorm(ps, nt, 124, tf, tb, xn)
                if ctx:
                    self.CP("act", dst_ap, xn.a[:, :nt], [xn.k], [dst_k])
                else:
                    self.rope(xn, dst_ap, dst_k, nt, tabs[0], tabs[1], self.perm64, tf)
            elif rk is None or ctx:
                self.CP("act", dst_ap, ps.a[:, :nt], [ps.k], [dst_k])
            else:
                xb = tb.next()
                self.CP("act", xb.a[:, :nt], ps.a[:, :nt], [ps.k], [xb.k])
                if rk == 64:
                    self.rope(xb, dst_ap, dst_k, nt, tabs[0], tabs[1], self.perm64, tf)
                else:
                    self.rope(xb, dst_ap, dst_k, nt, tabs[2], tabs[3], self.perm32, tf)
        for c in range(2):
            self.CP("dve", qz.a[64:128, c, :nt], qT.a[64:128, 2 + c, :nt], [qT.k], [qz.k])
            self.MSET("dve", qz.a[64:96, c, :nt], 0.0, [qz.k])
        self.MSET("pool", qpad.a[:, :, :], 0.0, [qpad.k])
        for c in range(2):
            for par in range(2):
                for m in range(2):
                    lo = par * 64 + m * 32
                    idx = c * 4 + par * 2 + m
                    if lo == 96:
                        self.CP("dve", qpad.a[64:128, idx, :nt], qz.a[64:128, c, :nt], [qz.k, qpad.k], [qpad.k])
                    else:
                        self.CP("dve", qpad.a[lo:lo + 32, idx, :nt], qT.a[lo:lo + 32, 2 + c, :nt], [qT.k, qpad.k], [qpad.k])
        for g in range(2):
            for par in range(2):
                self.CP("act", qpad.a[g * 64:g * 64 + 64, 8 + g * 2 + par, :nt], qT.a[g * 64:g * 64 + 64, 6 + par, :nt],
                        [qT.k, qpad.k], [qpad.k])
        if self.dbg and b == 0:
            self.dump("q%d" % l, qT.a[:, :, :], qT.k, [128, 8, 512], BF16)

        self.S.barrier()
        self.aoff = keep
        tf = self.ring(4, [128, 512], F32)
        pt = self.ring(4, [128, 512], BF16)
        nq = nt
        if not ctx:
            lr0 = 8 * b
            nak = self.alb([128, 2, 22 * 64], BF16)
            nakw = self.kvb[l]["naKwin"] if self.fused else self.din("naKwin" + sfx, [2, 128, NWIN * 64], BF16)
            for c in range(2):
                self.DMA("sp", nak.a[:, c, :], nakw[c][:, lr0 * 64:(lr0 + 22) * 64], W=[nak.k])
            navr = self.ring(2, [64, 22, 128], BF16)
            rpr = self.ring(2, [64, 15, 64], BF16)
            navw = self.kvb[l]["naVwin"] if self.fused else self.din("naVwin" + sfx, [4, 64, NWIN, 128], BF16)
            rpd = self.din("rpbr" + sfx, [4, 64, 15, 64])
            bn = self.alb([64, 512], F32)
            self.DMA("sp", bn.a[:, :], self.din("bandneg", [64, 512]), W=[bn.k])
        for h in range(4):
            c = h // 2
            po = (h % 2) * 64
            par = h % 2
            st = dict(krows=(po, po + 64), ci=c, q=qT.a[po:po + 64, c, :nq], qk=qT.k, cslot=8 + h)
            O = [self.RO.next()]
            self.ctx_tiles([st], O, nq, pt, SC64, ctx)
            if not ctx:
                nav = navr.next()
                self.DMA("sp", nav.a[:, :, :], navw[h][:, lr0:lr0 + 22, :], W=[nav.k])
                rp = rpr.next()
                self.DMA("pool", rp.a[:, :, :], rpd[h], W=[rp.k])
                for wr in range(22):
                    kr = lr0 - 7 + wr
                    r_lo = max(lr0, kr - 7)
                    r_hi = min(lr0 + 7, kr + 7)
                    nr = r_hi - r_lo + 1
                    n = nr * 64
                    qoff = (r_lo - lr0) * 64
                    e_lo = r_lo - kr + 7
                    s = self.RS.next()
                    self.MM(s.a[0:64, :n], nak.a[po:po + 64, c, wr * 64:(wr + 1) * 64], qT.a[po:po + 64, c, qoff:qoff + n],
                            True, True, [nak.k, qT.k], [s.k])
                    t = tf.next()
                    self.STT(t.a[0:64, :n].rearrange("p (r q) -> p r q", r=nr), s.a[0:64, :n].rearrange("p (r q) -> p r q", r=nr),
                             SC64, rp.a[:, e_lo:e_lo + nr, :], ALU.mult, ALU.add, [s.k, rp.k], [t.k])
                    bo = 17 * r_lo + 7 - kr
                    bnap = self.sap(bn.a[:, bo:bo + 1], [[17, nr], [0, 64]])
                    self.TT("pool", t.a[0:64, :n].rearrange("p (r q) -> p r q", r=nr),
                            t.a[0:64, :n].rearrange("p (r q) -> p r q", r=nr), bnap, ALU.add, [t.k, bn.k], [t.k])
                    p = pt.next()
                    self.ACT(p.a[0:64, :n], t.a[0:64, :n], AF.Exp, [t.k], [p.k])
                    self.MM(O[0].a[:, qoff:qoff + n], nav.a[:, wr, :], p.a[0:64, :n], False, wr == 21,
                            [nav.k, p.k], [O[0].k])
            self.finalize(O[0], par, nq, ysT.a[par * 64:par * 64 + 64, c, :nq], ysT.k, tf)

        self.S.barrier()
        self.aoff = keep
        tf = self.ring(4, [128, 512], F32)
        pt = self.ring(4, [128, 512], BF16)
        if not ctx:
            swk = self.alb([128, 768], BF16)
            swkw = self.kvb[l]["swKwin"] if self.fused else self.din("swKwin" + sfx, [128, SWT], BF16)
            self.DMA("sp", swk.a[:, :], swkw[:, t0:t0 + 768], W=[swk.k])
            swv = self.alb([128, 4, 6, 128], BF16)
            swvw = self.kvb[l]["swVwin"] if self.fused else self.din("swVwin" + sfx, [4, 128, 18, 128], BF16)
            for s4 in range(4):
                self.DMA("sp", swv.a[:, s4, :, :], swvw[s4][:, 4 * b:4 * b + 6, :], W=[swv.k])
            swm = self.alb([128, 6, 512], BF16)
            swmd = self.din("swm", [8, 128, 512])
            for j in range(6):
                mi = j
                if b == 0 and j == 0:
                    mi = 6
                if b == 3 and j == 5:
                    mi = 7
                self.DMA("pool", swm.a[:, j, :], swmd[mi], W=[swm.k])
        for h in range(4):
            g = h // 2
            par = h % 2
            qc = 4 + par
            st = dict(krows=(g * 64, g * 64 + 64), ci=4, q=qT.a[g * 64:g * 64 + 64, qc, :nq], qk=qT.k, cslot=12 + 2 * g + par)
            O = [self.RO.next()]
            self.ctx_tiles([st], O, nq, pt, SC64, ctx)
            if not ctx:
                for j in range(6):
                    s = self.RS.next()
                    self.MM(s.a[:, :nq], swk.a[g * 64:g * 64 + 64, j * 128:(j + 1) * 128], st["q"], True, True,
                            [swk.k, qT.k], [s.k])
                    p = pt.next()
                    self.ACT(p.a[:, :nq], s.a[:, :nq], AF.Exp, [s.k], [p.k], scale=SC64)
                    p2 = pt.next()
                    self.TT("pool", p2.a[:, :nq], p.a[:, :nq], swm.a[:, j, :nq], ALU.mult, [p.k, swm.k], [p2.k])
                    self.MM(O[0].a[:, :nq], swv.a[:, 2 * g + par, j, :], p2.a[:, :nq], False, j == 5, [swv.k, p2.k], [O[0].k])
            self.finalize(O[0], par, nq, ysT.a[par * 64:par * 64 + 64, 4 + g, :nq], ysT.k, tf,
                          extra=v[:, 260 + h:261 + h])

        self.S.barrier()
        self.aoff = keep
        tf = self.ring(4, [128, 512], F32)
        tb = self.ring(2, [128, 512], BF16)
        pt = self.ring(8, [128, 512], BF16)
        if not ctx:
            kring = self.ring(2, [128, 2048], BF16)
            vrings = [self.ring(2, [128, 16, 128], BF16), self.ring(2, [128, 16, 128], BF16)]
        for h in range(4):
            c = h // 2
            par = h % 2
            sts = []
            for m in range(2):
                sts.append(dict(krows=(0, 128), ci=2 + c, q=qpad.a[:, c * 4 + par * 2 + m, :nq], qk=qpad.k, cslot=h, vi=0))
            if ctx:
                O = [self.RO.next(), self.RO.next()]
                self.ctx_tiles(sts, O, nq, pt, SC32, True)
            else:
                O = self.dense_pass(l, sts, nq, c, [h], pt, kring, vrings, SC32)
            a0 = tf.next()
            a1 = tf.next()
            ro = par * 64
            self.finalize(O[0], par, nq, a0.a[ro:ro + 64, :nq], a0.k, tf)
            self.finalize(O[1], par, nq, a1.a[ro:ro + 64, :nq], a1.k, tf, mul=v[:, 256:257])
            self.TT("dve", ysd.a[ro:ro + 64, c, :nq], a0.a[ro:ro + 64, :nq], a1.a[ro:ro + 64, :nq], ALU.subtract,
                    [a0.k, a1.k], [ysd.k])
            if par == 1:
                sq = tb.next()
                self.ACT(sq.a[:, :nq], ysd.a[:, c, :nq], AF.Square, [ysd.k], [sq.k])
                p2 = self.RX.next()
                self.MM(p2.a[:, :nq], self.blk64.a[:, :], sq.a[:, :nq], True, True, [self.blk64.k, sq.k], [p2.k])
                rs = tf.next()
                self.ACT(rs.a[:, :nq], p2.a[:, :nq], AF.Sqrt, [p2.k, self.cstk], [rs.k], bias=self.cst[:, 0:1], scale=1.0 / 64)
                self.RCP(rs.a[:, :nq], rs.a[:, :nq], [rs.k], [rs.k])
                t = tf.next()
                self.TT("dve", t.a[:, :nq], ysd.a[:, c, :nq], rs.a[:, :nq], ALU.mult, [ysd.k, rs.k], [t.k])
                self.TS("pool", ysT.a[:, 2 + c, :nq], t.a[:, :nq], v[:, 126:127], 1.0 - self.lam_init, ALU.mult, ALU.mult,
                        [t.k, self.veck], [ysT.k])

        for g in range(2):
            sts = []
            for par in range(2):
                sts.append(dict(krows=(0, 128), ci=5, q=qpad.a[:, 8 + g * 2 + par, :nq], qk=qpad.k,
                                cslot=4 + 2 * g + par, vi=par))
            if ctx:
                O = [self.RO.next(), self.RO.next()]
                self.ctx_tiles(sts, O, nq, pt, SC64, True)
            else:
                O = self.dense_pass(l, sts, nq, 2, [4 + 2 * g, 5 + 2 * g], pt, kring, vrings, SC64)
            for par in range(2):
                self.finalize(O[par], par, nq, ysT.a[par * 64:par * 64 + 64, 6 + g, :nq], ysT.k, tf)
        if self.dbg and b in (0, 4):
            self.dump("ys%d_%d" % (l, b), ysT.a[:, :, :], ysT.k, [128, 8, 512], BF16)

        self.S.barrier()
        self.aoff = keep
        tf = self.ring(3, [128, 512], F32)
        wring = self.ring(3, [128, 8, 128], BF16)
        wbr = self.alb([128, 8, 1024], BF16)
        self.DMA("pool", wbr.a[:, :, :], self.din("w_branch" + sfx, [4, 256, D]).rearrange("n (w p) d -> p (n w) d", p=128),
                 W=[wbr.k])
        G = self.alb([128, 8, 512], BF16)
        Gf = self.ring(2, [128, 512], F32)
        gt = self.ring(2, [128, 512], F32)
        wg = self.din("w_gate" + sfx, [D, 4 * D]).rearrange("(k p) n -> p k n", p=128)
        for dc in range(8):
            gf = Gf.next()
            for n in range(4):
                ps = self.proj(wg, n * D + dc * 128, nt, wring, self.RS)
                ga = gt.next()
                self.ACT(ga.a[:, :nt], ps.a[:, :nt], AF.Sigmoid, [ps.k, self.veck], [ga.k],
                         bias=v[:, 64 + n * 8 + dc:65 + n * 8 + dc])
                pb = self.RX.next()
                for w in range(2):
                    self.MM(pb.a[:, :nt], wbr.a[:, 2 * n + w, dc * 128:(dc + 1) * 128], ysT.a[:, 2 * n + w, :nt], w == 0, w == 1,
                            [wbr.k, ysT.k], [pb.k], sig=(w == 1))
                if n == 0:
                    self.TT("dve", gf.a[:, :nt], ga.a[:, :nt], pb.a[:, :nt], ALU.mult, [ga.k, pb.k], [gf.k])
                else:
                    t = tf.next()
                    self.TT("dve", t.a[:, :nt], ga.a[:, :nt], pb.a[:, :nt], ALU.mult, [ga.k, pb.k], [t.k])
                    if n < 3:
                        self.TT("pool", gf.a[:, :nt], gf.a[:, :nt], t.a[:, :nt], ALU.add, [gf.k, t.k], [gf.k])
                    else:
                        self.TT("pool", G.a[:, dc, :nt], gf.a[:, :nt], t.a[:, :nt], ALU.add, [gf.k, t.k], [G.k])
        wo = self.din("w_out" + sfx, [D, D]).rearrange("(k p) n -> p k n", p=128)
        for dc in range(8):
            wt = wring.next()
            self.DMA("pool", wt.a[:, :, :], wo[:, :, dc * 128:(dc + 1) * 128], W=[wt.k])
            ps = self.RX.next()
            for k in range(8):
                self.MM(ps.a[:, :nt], wt.a[:, k, :], G.a[:, k, :nt], k == 0, k == 7, [wt.k, G.k], [ps.k], sig=(k == 7))
            self.STT(self.xT[:, dc, t0:t0 + nt], ps.a[:, :nt], self.dr(2, dc, ctx), self.xT[:, dc, t0:t0 + nt],
                     ALU.mult, ALU.add, [ps.k, self.drv.k, self.xk[b]], [self.xk[b]])
        if self.dbg and b in (0, 4):
            self.dump("xmid%d_%d" % (l, b), self.xT[:, :, t0:t0 + nt], self.xk[b], [128, 8, nt])

        self.peer(l, b)
        if self.dbg and b in (0, 4):
            self.dump("xout%d_%d" % (l, b), self.xT[:, :, t0:t0 + nt], self.xk[b], [128, 8, nt])

    def peer(self, l, b):
        sfx = "_l%d" % l
        t0, nt = self.blk(b)
        ctx = (b == 4)
        ntile = nt // 128
        self.arena_reset()
        sc = self.alb([128, 4, 16, 128], F32)
        stat = self.alb([128, 4, 8, 4], F32)
        keep = self.aoff
        tf = self.ring(3, [128, 512], F32)
        tb = self.ring(3, [128, 512], BF16)
        self.norm_mod(b, 3, 4, tf, tb)
        wring = self.ring(3, [128, 8, 128], BF16)
        qp = self.alb([128, 16, 512], BF16)
        wq = self.din("peer_wq" + sfx, [D, 2048]).rearrange("(k p) n -> p k n", p=128)
        for j in range(16):
            ps = self.proj(wq, j * 128, nt, wring)
            self.CP("act" if j % 2 else "dve", qp.a[:, j, :nt], ps.a[:, :nt], [ps.k], [qp.k])
        for tt in range(ntile):
            for q4 in range(4):
                ps = self.RS.next()
                for i in range(4):
                    j = q4 * 4 + i
                    self.MM(ps.a[:, i * 128:(i + 1) * 128], qp.a[:, j, tt * 128:(tt + 1) * 128], self.keysT.a[:, j, :],
                            True, True, [qp.k, self.keysT.k], [ps.k], sig=(i == 3))
                self.CP("act" if q4 % 2 else "dve", sc.a[:, tt, q4 * 4:(q4 + 1) * 4, :],
                        ps.a[:, :].rearrange("p (a b) -> p a b", a=4), [ps.k], [sc.k])
        top = self.alb([128, 16, 16], F32)
        mx = self.alb([128, 16, 8], F32)
        wk = self.ring(2, [128, 256], F32)
        cand = self.alb([128, 256], F32)
        best = self.alb([128, 16], F32)
        c3 = cand.a[:, :].rearrange("p (a b) -> p a b", a=16)
        for tt in range(ntile):
            for j in range(16):
                self.MAX8(mx.a[:, j, :], sc.a[:, tt, j, :], [sc.k], [mx.k])
                self.TS("dve", mx.a[:, j, 1:2], mx.a[:, j, 0:1], -1.0, None, ALU.mult, None, [mx.k], [mx.k])
                self.ACT(sc.a[:, tt, j, :], sc.a[:, tt, j, :], AF.Exp, [sc.k, mx.k], [sc.k], bias=mx.a[:, j, 1:2])
            for j in range(16):
                self.MAX8(top.a[:, j, 0:8], sc.a[:, tt, j, :], [sc.k], [top.k])
                w1 = wk.next()
                self.MREP(w1.a[:, 0:128], top.a[:, j, 0:8], sc.a[:, tt, j, :], [sc.k, top.k], [w1.k])
                self.MAX8(top.a[:, j, 8:16], w1.a[:, 0:128], [w1.k], [top.k])
            for h in range(8):
                sa = stat.a[:, tt, h, :]
                b0 = top.a[:, 2 * h, :].unsqueeze(2).to_broadcast([128, 16, 16])
                b1 = top.a[:, 2 * h + 1, :].unsqueeze(1).to_broadcast([128, 16, 16])
                for rnd in range(2):
                    self.TT("pool", c3, b0, b1, ALU.mult, [top.k], [cand.k])
                    self.MAX8(best.a[:, 0:8], cand.a[:, :], [cand.k], [best.k])
                    w1 = wk.next()
                    self.MREP(w1.a[:, :], best.a[:, 0:8], cand.a[:, :], [cand.k, best.k], [w1.k])
                    self.MAX8(best.a[:, 8:16], w1.a[:, :], [w1.k], [best.k])
                    if rnd == 0:
                        self.S.op("dve", (lambda sa=sa: lambda e: e.tensor_reduce(out=sa[:, 2:3], in_=best.a[:, :], axis=AX.X,
                                                                                   op=ALU.add))(), reads=[best.k], writes=[stat.k])
                        self.RCP(sa[:, 1:2], sa[:, 2:3], [stat.k], [stat.k])
                        self.TS("dve", sc.a[:, tt, 2 * h, :], sc.a[:, tt, 2 * h, :], sa[:, 1:2], None, ALU.mult, None,
                                [sc.k, stat.k], [sc.k])
                        self.TS("dve", top.a[:, 2 * h, :], top.a[:, 2 * h, :], sa[:, 1:2], None, ALU.mult, None,
                                [top.k, stat.k], [top.k])
                    else:
                        self.CP("dve", sa[:, 0:1], best.a[:, 15:16], [best.k], [stat.k])
        if self.dbg and b == 0:
            self.dump("pstat%d" % l, stat.a[:, :, :, :], stat.k, [128, 4, 8, 4])
        self.S.barrier()
        self.aoff = keep
        acc = self.alb([128, 4, 1024], F32)
        zb = self.alb([128, 512], BF16)
        self.MSET("dve", zb.a[:, :], 0.0, [zb.k])
        GE = 512
        NG = 16384 // GE
        utr = self.ring(2, [128, 8, GE], BF16)
        vwr = self.ring(2, [128, 4, 1024], BF16)
        tP = self.ring(6, [128, 512], F32)
        ppk = {id(it): [Tk() for _ in range(4)] for it in tP.items}
        tM = self.ring(6, [128, 512], BF16)
        Ag = self.ring(2, [128, 512], BF16)
        WAT = self.ring(2, [128, 4, 128], BF16)
        uT = self.din("uT" + sfx, [D, 16384]).rearrange("(k p) e -> p k e", p=128)
        vv = self.din("peer_v" + sfx, [16384, D]).rearrange("(g c p) d -> g p c d", c=4, p=128)

        def load(g):
            ut = utr.next()
            self.DMA("pool", ut.a[:, :, :], uT[:, :, g * GE:(g + 1) * GE], W=[ut.k])
            vw = vwr.next()
            self.DMA("pool", vw.a[:, :, :], vv[g], W=[vw.k])
            return ut, vw
        items = [(g, tt) for g in range(NG) for tt in range(ntile)]
        loads = {0: load(0)}
        st_ = {}

        def emit_AT(i, c4):
            g, tt = items[i]
            ut = loads[g][0]
            if c4 == 0:
                st_[i] = {"pa": self.RS.next()}
            pa = st_[i]["pa"]
            for k in range(8):
                self.MM(pa.a[:, c4 * 128:(c4 + 1) * 128], ut.a[:, k, c4 * 128:(c4 + 1) * 128],
                        self.hT.a[:, k, tt * 128:(tt + 1) * 128], k == 0, k == 7, [self.hT.k, ut.k], [pa.k], sig=(k == 7))
            if c4 == 3:
                ag = Ag.next()
                self.ACT(ag.a[:, :], pa.a[:, :], AF.Gelu, [pa.k], [ag.k])
                st_[i]["ag"] = ag

        def emit_wat(i):
            d = st_[i]
            wat = WAT.next()
            self.TT("dve", wat.a[:, :, :], d["ag"].a[:, :].rearrange("p (a b) -> p a b", a=4),
                    d["pw"].a[:, :].rearrange("p (a b) -> p a b", a=4), ALU.mult, [d["ag"].k, d["pw"].k], [wat.k])
            d["wat"] = wat

        def emit_out(i, half):
            g, tt = items[i]
            vw = loads[g][1]
            wat = st_[i]["wat"]
            po = (self.RO if half == 0 else self.RX).next()
            for c4 in range(4):
                self.MM(po.a[:, :], wat.a[:, c4, :], vw.a[:, c4, half * 512:(half + 1) * 512], c4 == 0, c4 == 3,
                        [wat.k, vw.k], [po.k], sig=(c4 == 3))
            if g == 0:
                self.CP("dve", acc.a[:, tt, half * 512:(half + 1) * 512], po.a[:, :], [po.k], [acc.k])
            else:
                self.TT("dve", acc.a[:, tt, half * 512:(half + 1) * 512], acc.a[:, tt, half * 512:(half + 1) * 512],
                        po.a[:, :], ALU.add, [acc.k, po.k], [acc.k])
        for c4 in range(4):
            emit_AT(0, c4)
        for i, (g, tt) in enumerate(items):
            i0 = g * 4
            if tt == min(1, ntile - 1) and g + 1 < NG and (g + 1) not in loads:
                loads[g + 1] = load(g + 1)
            if i > 0:
                emit_wat(i - 1)
            pw = self.RS.next()
            st_[i]["pw"] = pw
            self.MM(pw.a[:, :], zb.a[:, 0:128], zb.a[:, :], True, False, [zb.k], [pw.k], sig=False)
            for h in range(8):
                pp = tP.next()
                pks = ppk[id(pp)]
                p3 = pp.a[:, :].rearrange("p (i j) -> p i j", i=4)
                if h in (1, 4, 6):
                    for i4 in range(4):
                        self.ACT(pp.a[:, i4 * 128:(i4 + 1) * 128], sc.a[:, tt, 2 * h + 1, :], AF.Copy, [sc.k], [pks[i4]],
                                 scale=sc.a[:, tt, 2 * h, i0 + i4:i0 + i4 + 1])
                else:
                    self.TT("pool", p3, sc.a[:, tt, 2 * h, i0:i0 + 4].unsqueeze(2).to_broadcast([128, 4, 128]),
                            sc.a[:, tt, 2 * h + 1, :].unsqueeze(1).to_broadcast([128, 4, 128]), ALU.mult, [sc.k], pks)
                tm = tM.next()
                self.STT(tm.a[:, :], pp.a[:, :], stat.a[:, tt, h, 0:1], pp.a[:, :], ALU.is_ge, ALU.mult,
                         pks + [stat.k], [tm.k])
                for c4 in range(4):
                    self.MM(pw.a[:, c4 * 128:(c4 + 1) * 128], tm.a[:, c4 * 128:(c4 + 1) * 128], self.identb.a[:, :],
                            False, h == 7, [tm.k, self.identb.k], [pw.k], sig=(c4 == 3))
                if h < 4 and i + 1 < len(items):
                    emit_AT(i + 1, h)
                if h in (4, 5) and i > 0:
                    emit_out(i - 1, h - 4)
                    if h == 5:
                        del st_[i - 1]
        last = len(items) - 1
        emit_wat(last)
        emit_out(last, 0)
        emit_out(last, 1)
        for tt in range(ntile):
            for half in range(2):
                pt4 = self.RX.next()
                for c4 in range(4):
                    dc = half * 4 + c4
                    self.TR(pt4.a[:, c4 * 128:(c4 + 1) * 128], acc.a[:, tt, dc * 128:(dc + 1) * 128], self.identf.a[:, :],
                            [acc.k, self.identf.k], [pt4.k], sig=(c4 == 3))
                for c4 in range(4):
                    dc = half * 4 + c4
                    xs = self.xT[:, dc, t0 + tt * 128:t0 + (tt + 1) * 128]
                    self.STT(xs, pt4.a[:, c4 * 128:(c4 + 1) * 128], self.dr(5, dc, ctx), xs, ALU.mult, ALU.add,
                             [pt4.k, self.drv.k, self.xk[b]], [self.xk[b]])

    def final_norm(self):
        self.arena_reset()
        tf = self.ring(4, [128, 512], F32)
        tb = self.ring(3, [128, 512], BF16)
        oT = self.dout("outT", [D, TOK]).rearrange("(c p) t -> p c t", p=128)
        ob = self.ring(2, [128, 8, 512], F32)
        rsr = self.ring(2, [128, 512], F32)
        for b in range(self.own_blocks):
            t0, nt = self.blk(b)
            ps = self.RS.next()
            for c in range(8):
                sq = tb.next()
                self.ACT(sq.a[:, :nt], self.xT[:, c, t0:t0 + nt], AF.Square, [self.xk[b]], [sq.k])
                self.MM(ps.a[:, :nt], self.onesb.a[:, :], sq.a[:, :nt], c == 0, c == 7, [self.onesb.k, sq.k], [ps.k])
            rs = rsr.next()
            self.ACT(rs.a[:, :nt], ps.a[:, :nt], AF.Sqrt, [ps.k, self.cstk], [rs.k], bias=self.cst[:, 0:1], scale=1.0 / D)
            self.RCP(rs.a[:, :nt], rs.a[:, :nt], [rs.k], [rs.k])
            o = ob.next()
            for c in range(8):
                t = tf.next()
                self.TT("dve", t.a[:, :nt], self.xT[:, c, t0:t0 + nt], rs.a[:, :nt], ALU.mult, [self.xk[b], rs.k], [t.k])
                self.TS("pool", o.a[:, c, :nt], t.a[:, :nt], self.vec[:, 96 + c:97 + c], None, ALU.mult, None,
                        [t.k, self.veck], [o.k])
            self.DMA("sp", oT[:, :, t0:t0 + nt], o.a[:, :, :], R=[o.k], W=[Tk()])


def _cols(vv, n):
    return np.ascontiguousarray(np.asarray(vv, np.float32).reshape(n, 128).T)


def _consts():
    ident = np.eye(128, dtype=np.float32)

    def perm(dh):
        q = dh // 4
        P = np.zeros((128, 128), np.float32)
        for blk in range(128 // dh):
            o = blk * dh
            for i in range(q):
                P[o + q + i, o + i] = -1.0
                P[o + i, o + q + i] = 1.0
                P[o + 3 * q + i, o + 2 * q + i] = -1.0
                P[o + 2 * q + i, o + 3 * q + i] = 1.0
        return P
    blk64 = np.zeros((128, 128), np.float32)
    blk64[0:64, 0:64] = 1.0
    blk64[64:128, 64:128] = 1.0
    cmat = np.stack([ident, perm(64), perm(32), blk64])
    jj = np.arange(128)[:, None]
    ii = np.arange(128)[None, :]
    swm = np.zeros((8, 128, 512), np.float32)
    for j in range(6):
        for qb in range(4):
            dlt = (j - 1) - qb
            if dlt == -1:
                m = (jj >= ii)
            elif dlt == 0:
                m = np.ones((128, 128), bool)
            elif dlt == 1:
                m = (jj <= ii)
            else:
                m = np.zeros((128, 128), bool)
            swm[j, :, qb * 128:(qb + 1) * 128] = m
    return cmat, swm


def _rope_tables(core):
    t = np.arange(TOK, dtype=np.int64) + core * TOK
    row = (t // GW).astype(np.float32)
    col = (t % GW).astype(np.float32)
    out = np.zeros((4, 128, TOK), np.float32)
    for ti, dh in ((0, 64), (2, 32)):
        nf = dh // 4
        inv = (np.float32(10000.0) ** (-(np.arange(nf, dtype=np.float32)) / np.float32(nf))).astype(np.float32)
        ang_r = (row[:, None] * inv[None, :]).astype(np.float32)
        ang_c = (col[:, None] * inv[None, :]).astype(np.float32)
        ang = np.concatenate([ang_r, ang_r, ang_c, ang_c], axis=-1)
        reps = 128 // dh
        out[ti] = np.tile(np.cos(ang).T, (reps, 1))
        out[ti + 1] = np.tile(np.sin(ang).T, (reps, 1))
    return out


def _bandneg(core):
    bn = np.zeros((512,), np.float32)
    r0 = core * ROWS
    for r in range(ROWS):
        rg = r0 + r
        start = min(max(rg - 4, 0), 256 - 8)
        for e in range(15):
            krg = rg - e + 7
            ok = (start <= krg <= start + 7)
            bn[r * 16 + e] = 0.0 if ok else NEG
    return np.ascontiguousarray(np.tile(bn[None, :], (64, 1)))


def _layer_inputs(inp, l):
    sfx = "_l%d" % l
    f = lambda a: np.ascontiguousarray(np.asarray(a, np.float32))
    vec = np.zeros((128, 512), np.float32)
    vec[:, 0:8] = _cols(inp["norm1_w"][l], 8)
    vec[:, 8:16] = _cols(inp["norm2_w"][l], 8)
    vec[:, 16:64] = _cols(inp["ada_b"][l], 48)
    vec[:, 64:96] = _cols(inp["b_gate"][l], 32)
    vec[:, 96:104] = _cols(inp["final_norm_w"], 8)
    vec[:, 104:112] = _cols(np.asarray(inp["c"]).reshape(-1), 8)
    vec[:, 112:120] = _cols(inp["c_ctx"], 8)
    vec[:, 120:124] = np.asarray(inp["swa_sink"][l], np.float32)[None, :]
    vec[:, 124] = np.tile(np.asarray(inp["gqa_qk_norm_w"][l][0], np.float32), 2)
    vec[:, 125] = np.tile(np.asarray(inp["gqa_qk_norm_w"][l][1], np.float32), 2)
    vec[:, 126] = np.tile(np.asarray(inp["diff_subln_w"][l], np.float32), 2)
    vec[:, 128:256] = np.asarray(inp["diff_lam"][l], np.float32).reshape(1, 128)
    w_in = np.array(inp["w_in"][l], np.float32)
    for base in (1536, 2048):
        blk = w_in[:, base:base + 256].reshape(D, 4, 64)
        w_in[:, base:base + 256] = blk[:, [0, 2, 1, 3], :].reshape(D, 256)
    w_v = np.concatenate([w_in[:, 512:768], w_in[:, 1280:1536], w_in[:, 1920:2048], w_in[:, 2432:2560]], axis=1)
    rpb = np.asarray(inp["na_rpb"][l], np.float32)
    kc = np.arange(64)[:, None]
    qc = np.arange(64)[None, :]
    coff = np.clip(kc - qc, -15, 15) + 15
    cs = np.clip(qc - 8, 0, 48)
    ok = (kc >= cs) & (kc < cs + 16)
    rp = np.full((4, 64, 15, 64), NEG, np.float32)
    for e in range(15):
        val = rpb[:, 14 - e, :][:, coff]
        rp[:, :, e, :] = np.where(ok[None], val, NEG)
    keysT = np.ascontiguousarray(np.transpose(np.asarray(inp["peer_keys"][l], np.float32), (3, 1, 0, 2)).reshape(128, 16, 128))
    return {
        "vec" + sfx: vec, "ada_w" + sfx: f(inp["ada_w"][l]), "w_in" + sfx: np.ascontiguousarray(w_in),
        "w_v" + sfx: np.ascontiguousarray(w_v), "rpbr" + sfx: rp, "keysT" + sfx: keysT,
        "w_branch" + sfx: f(inp["w_branch"][l]), "w_gate" + sfx: f(inp["w_gate"][l]), "w_out" + sfx: f(inp["w_out"][l]),
        "peer_wq" + sfx: f(inp["peer_wq"][l]), "uT" + sfx: np.ascontiguousarray(np.asarray(inp["peer_u"][l], np.float32).T),
        "peer_v" + sfx: f(inp["peer_v"][l]),
    }


def _gather_kv(results, l):
    sfx = "_l%d" % l
    bf = ml_dtypes.bfloat16
    kT = np.concatenate([np.asarray(r["kT_own" + sfx]) for r in results], axis=2)
    VAd = np.concatenate([np.asarray(r["VAd_own" + sfx]) for r in results], axis=2)
    NV = np.concatenate([np.asarray(r["NV_own" + sfx]) for r in results], axis=2)
    SV = np.concatenate([np.asarray(r["SV_own" + sfx]) for r in results], axis=2)
    KTd = np.ascontiguousarray(kT[[2, 3, 5]])
    outs = []
    for i in range(NCORE):
        r0 = i * ROWS
        nak = np.zeros((2, 128, NWIN * 64), bf)
        nav = np.zeros((4, 64, NWIN, 128), bf)
        lo = max(r0 - 7, 0)
        hi = min(r0 + ROWS + 7, 256)
        nak[:, :, (lo - (r0 - 7)) * 64:(hi - (r0 - 7)) * 64] = kT[0:2, :, lo * 64:hi * 64]
        nav[:, :, lo - (r0 - 7):hi - (r0 - 7), :] = NV[:, :, lo:hi, :]
        swk = np.zeros((128, SWT), bf)
        t_lo = max(i * TOK - 128, 0)
        t_hi = min((i + 1) * TOK + 128, SEQ)
        swk[:, t_lo - (i * TOK - 128):t_hi - (i * TOK - 128)] = kT[4][:, t_lo:t_hi]
        swv = np.zeros((4, 128, 18, 128), bf)
        k_lo = max(i * 16 - 1, 0)
        k_hi = min((i + 1) * 16 + 1, 128)
        swv[:, :, k_lo - (i * 16 - 1):k_hi - (i * 16 - 1), :] = SV[:, :, k_lo:k_hi, :]
        outs.append({"KTd" + sfx: KTd, "VAd" + sfx: VAd, "naKwin" + sfx: nak, "naVwin" + sfx: nav,
                     "swKwin" + sfx: swk, "swVwin" + sfx: swv})
    return outs


def _percore(swm):
    out = []
    for i in range(NCORE):
        m = swm.copy()
        if i > 0:
            m[6] = swm[0]
        if i < NCORE - 1:
            m[7] = swm[5]
        sel = np.zeros((128, 16), np.float32)
        if i > 0:
            sel[:, i - 1] = 1.0
        if i < NCORE - 1:
            sel[:, 8 + i + 1] = 1.0
        out.append({"ropeT": _rope_tables(i), "bandneg": _bandneg(i), "swm": m, "sel": sel})
    return out


_PROG = {}


def _prog(key, *args, **kw):
    if key not in _PROG:
        _PROG[key] = KB(*args, **kw)
    return _PROG[key]


def _run(kb, provs):
    in_maps = []
    for i in range(NCORE):
        m = {}
        for name in kb.ins:
            for p in provs:
                src = p[i] if isinstance(p, list) else p
                if name in src:
                    m[name] = src[name]
                    break
            else:
                raise KeyError(name)
        in_maps.append(m)
    res = run_bass_kernel_spmd(kb.nc, in_maps, core_ids=list(range(NCORE)))
    return res.results


def kernel_unfused(**inp):
    x = np.asarray(inp["x"], np.float32)[0]
    ctx = np.asarray(inp["ctx"], np.float32)[0]
    cmat, swm = _consts()
    common = {"cmat": cmat}
    percore = _percore(swm)
    L = [_layer_inputs(inp, 0), _layer_inputs(inp, 1)]
    xin = [{"xT_in": np.ascontiguousarray(np.concatenate([x[i * TOK:(i + 1) * TOK], ctx], axis=0).T)} for i in range(NCORE)]
    r0 = _run(_prog("s0", [0], [], False), [xin, common, percore, L[0]])
    kv0 = _gather_kv(r0, 0)
    r1 = _run(_prog("s1", [1], [0], False), [xin, common, percore, L[0], L[1], kv0])
    kv1 = _gather_kv(r1, 1)
    xin2 = [{"xT_in": np.asarray(r["xT_out"])} for r in r1]
    r2 = _run(_prog("s2", [], [1], True), [xin2, common, percore, L[1], kv1])
    out = np.concatenate([np.asarray(r["outT"]).T for r in r2], axis=0)
    return out[None].astype(np.float32)


def kernel(**inp):
    x = np.asarray(inp["x"], np.float32)[0]
    ctx = np.asarray(inp["ctx"], np.float32)[0]
    cmat, swm = _consts()
    common = {"cmat": cmat}
    percore = _percore(swm)
    L = [_layer_inputs(inp, 0), _layer_inputs(inp, 1)]
    xin = [{"xT_in": np.ascontiguousarray(np.concatenate([x[i * TOK:(i + 1) * TOK], ctx], axis=0).T)} for i in range(NCORE)]
    r = _run(_prog("fused", [], [], True, fused=True), [xin, common, percore, L[0], L[1]])
    out = np.concatenate([np.asarray(q["outT"]).T for q in r], axis=0)
    return out[None].astype(np.float32)
```

```python
import math
from contextlib import ExitStack

import numpy as np
import ml_dtypes
import concourse.bass as bass
import concourse.mybir as mybir
from concourse.bass_utils import run_bass_kernel_spmd

F32 = mybir.dt.float32
BF16 = mybir.dt.bfloat16
AF = mybir.ActivationFunctionType
ALU = mybir.AluOpType
AX = mybir.AxisListType

D = 1024
SEQ = 16384
NCORE = 8
TOK = SEQ // NCORE
CTX = 256
NTT = TOK + CTX
GW = 64
ROWS = TOK // GW
EPS = 1e-6
NEG = -30000.0
SC64 = 64 ** -0.5
SC32 = 32 ** -0.5
NWIN = ROWS + 14
SWT = TOK + 256


class Tk:
    __slots__ = ("w", "r")

    def __init__(self):
        self.w = None
        self.r = {}


class Sched:
    ENG = ("pe", "act", "dve", "pool", "sp")

    def __init__(self, ndma=32, same_engine_sync=True):
        self.ops = {e: [] for e in self.ENG}
        self.cnt = {e: 0 for e in self.ENG}
        self.seen = {e: {} for e in self.ENG}
        self.ndma = ndma
        self.dma_cnt = [0] * ndma
        self.dma_next = 0
        self.same = same_engine_sync
        self.ncc = 0

    def cc(self, fn, eng="pool"):
        idx = self.ncc
        self.ncc += 1
        self.ops[eng].append(("cc", fn, idx))

    def _deps(self, reads, writes):
        deps = {}

        def add(v):
            if v is None:
                return
            key, val = v
            if deps.get(key, 0) < val:
                deps[key] = val
        for t in reads:
            add(t.w)
        for t in writes:
            add(t.w)
            for k, v in t.r.items():
                add((k, v))
        return deps

    def _wait(self, eng, deps):
        for key, val in deps.items():
            if key == eng and (eng == "pe" or not self.same):
                continue
            if self.seen[eng].get(key, 0) >= val:
                continue
            self.seen[eng][key] = val
            self.ops[eng].append(("wait", key, val))

    def op(self, eng, fn, reads=(), writes=(), sig=True):
        self._wait(eng, self._deps(reads, writes))
        if sig:
            self.cnt[eng] += 1
            n = self.cnt[eng]
            self.ops[eng].append(("op", fn))
        else:
            n = self.cnt[eng] + 1
            self.ops[eng].append(("opq", fn))
        for t in reads:
            t.r[eng] = n
        for t in writes:
            t.w = (eng, n)
            t.r = {}

    def dma(self, eng, out_ap, in_ap, reads=(), writes=()):
        deps = self._deps(reads, writes)
        k = self.dma_next
        self.dma_next = (k + 1) % self.ndma
        key = ("dma", k)
        prev = self.dma_cnt[k] * 16
        if prev and deps.get(key, 0) < prev:
            deps[key] = prev
        self._wait(eng, deps)
        self.dma_cnt[k] += 1
        val = self.dma_cnt[k] * 16
        self.ops[eng].append(("dma", out_ap, in_ap, k))
        for t in reads:
            t.r[key] = val
        for t in writes:
            t.w = (key, val)
            t.r = {}

    def barrier(self):
        deps = {}
        for k in range(self.ndma):
            if self.dma_cnt[k]:
                deps[("dma", k)] = self.dma_cnt[k] * 16
        for e in self.ENG:
            if self.cnt[e]:
                deps[e] = self.cnt[e]
        for i in range(self.ncc):
            deps[("cc", i)] = 1
        for e in self.ENG:
            d = {k: v for k, v in deps.items() if k != e}
            self._wait(e, d)

    def emit(self, block, sems, dsems, ccsems=()):
        engobj = {"pe": "tensor", "act": "scalar", "dve": "vector", "pool": "gpsimd", "sp": "sync"}

        def semof(key):
            if isinstance(key, tuple):
                return dsems[key[1]] if key[0] == "dma" else ccsems[key[1]]
            return sems[key]

        def make(ename):
            ops = self.ops[ename]
            mysem = sems[ename]

            def body(eng):
                for o in ops:
                    if o[0] == "wait":
                        eng.wait_ge(semof(o[1]), o[2])
                    elif o[0] == "op":
                        o[1](eng).then_inc(mysem, 1)
                    elif o[0] == "opq":
                        o[1](eng)
                    elif o[0] == "cc":
                        o[1](eng).then_inc(ccsems[o[2]])
                    else:
                        eng.dma_start(out=o[1], in_=o[2]).then_inc(dsems[o[3]], 16)
            return body
        for ename in self.ENG:
            if self.ops[ename]:
                getattr(block, engobj[ename])(make(ename))


class Ring:
    def __init__(self, items):
        self.items = items
        self.i = 0

    def next(self):
        it = self.items[self.i]
        self.i = (self.i + 1) % len(self.items)
        return it


class B:
    __slots__ = ("a", "k")

    def __init__(self, a, k=None):
        self.a = a
        self.k = k if k is not None else Tk()


class KB:
    def __init__(self, layers_a, layers_b, final, dbg=False, own_blocks=4, fused=False):
        self.nc = bass.Bass("TRN2", target_bir_lowering=False)
        self.S = Sched()
        self.es = ExitStack()
        self.dbg = dbg
        self.dumps = []
        self.own_blocks = own_blocks
        self.fused = fused
        self.kvb = {}
        self.ins = {}
        self.build(layers_a, layers_b, final)

    def din(self, name, shape, dt=F32):
        if name not in self.ins:
            self.ins[name] = self.nc.dram_tensor(name, list(shape), dt, kind="ExternalInput").ap()
        return self.ins[name]

    def dout(self, name, shape, dt=F32):
        return self.nc.dram_tensor(name, list(shape), dt, kind="ExternalOutput").ap()

    def sb(self, name, shape, dt):
        return self.es.enter_context(self.nc.sbuf_tensor(name, list(shape), dt))

    def arena_reset(self):
        self.S.barrier()
        self.aoff = 0

    def al(self, shape, dt):
        n = 1
        for s in shape[1:]:
            n *= s
        words = n if dt == F32 else (n + 1) // 2
        a = self.arena[0:shape[0], self.aoff:self.aoff + words]
        self.aoff += words
        assert self.aoff <= self.AW, ("arena overflow", self.aoff)
        if dt != F32:
            a = a.bitcast(dt)
        if len(shape) == 3:
            a = a.rearrange("p (a b) -> p a b", a=shape[1])
        elif len(shape) == 4:
            a = a.rearrange("p (a b c) -> p a b c", a=shape[1], b=shape[2])
        return a

    def alb(self, shape, dt):
        return B(self.al(shape, dt))

    def ring(self, n, shape, dt):
        return Ring([self.alb(shape, dt) for _ in range(n)])

    def MM(self, out, lhsT, rhs, start, stop, R, W, sig=True):
        self.S.op("pe", lambda e: e.matmul(out, lhsT=lhsT, rhs=rhs, start=start, stop=stop, skip_group_check=True),
                  reads=R, writes=W, sig=sig)

    def TR(self, out, in_, ident, R, W, sig=True):
        self.S.op("pe", lambda e: e.transpose(out, in_, ident), reads=R, writes=W, sig=sig)

    def ACT(self, out, in_, func, R, W, bias=None, scale=None, accum=None):
        kw = {}
        if bias is not None:
            kw["bias"] = bias
        if scale is not None:
            kw["scale"] = scale
        if accum is not None:
            kw["accum_out"] = accum
        self.S.op("act", lambda e: e.activation(out=out, in_=in_, func=func, **kw), reads=R, writes=W)

    def TT(self, eng, out, in0, in1, op, R, W):
        self.S.op(eng, lambda e: e.tensor_tensor(out=out, in0=in0, in1=in1, op=op), reads=R, writes=W)

    def TS(self, eng, out, in0, s1, s2, op0, op1, R, W):
        if op1 is None:
            self.S.op(eng, lambda e: e.tensor_scalar(out=out, in0=in0, scalar1=s1, scalar2=None, op0=op0),
                      reads=R, writes=W)
        else:
            self.S.op(eng, lambda e: e.tensor_scalar(out=out, in0=in0, scalar1=s1, scalar2=s2, op0=op0, op1=op1),
                      reads=R, writes=W)

    def STT(self, out, in0, scalar, in1, op0, op1, R, W):
        self.S.op("dve", lambda e: e.scalar_tensor_tensor(out=out, in0=in0, scalar=scalar, in1=in1, op0=op0, op1=op1),
                  reads=R, writes=W)

    def CP(self, eng, out, in_, R, W):
        if eng == "act":
            self.S.op("act", lambda e: e.activation(out=out, in_=in_, func=AF.Copy), reads=R, writes=W)
        else:
            self.S.op(eng, lambda e: e.tensor_copy(out=out, in_=in_), reads=R, writes=W)

    def RCP(self, out, in_, R, W):
        self.S.op("dve", lambda e: e.reciprocal(out=out, in_=in_), reads=R, writes=W)

    def MSET(self, eng, ap, val, W):
        self.S.op(eng, lambda e: e.memset(ap, val), writes=W)

    def DMA(self, q, out, in_, R=(), W=()):
        self.S.dma(q, out, in_, reads=R, writes=W)

    @staticmethod
    def sap(base, dims):
        return bass.AP(base.tensor, base.offset, [list(base.ap[0])] + [list(d) for d in dims])

    def MAX8(self, out, in_, R, W):
        self.S.op("dve", lambda e: e.max(out=out, in_=in_), reads=R, writes=W)

    def MREP(self, out, rep, vals, R, W):
        self.S.op("dve", lambda e: e.match_replace(out=out, in_to_replace=rep, in_values=vals, imm_value=-1e30),
                  reads=R, writes=W)

    def dump(self, name, ap, k, shape, dt=F32):
        if not self.dbg:
            return
        o = self.dout("dbg_" + name, shape, dt)
        self.DMA("sp", o, ap, R=[k], W=[Tk()])

    def build(self, layers_a, layers_b, final):
        nc = self.nc
        self.banks = [B(self.es.enter_context(nc.psum_tensor("ps%d" % i, [128, 512], F32))) for i in range(8)]
        self.RS = Ring(self.banks[0:4])
        self.RO = Ring(self.banks[4:6])
        self.RX = Ring(self.banks[6:8])
        self.RS6 = Ring(self.banks[0:4] + self.banks[6:8])
        self.xT = self.sb("xT", [128, 8, NTT], F32)
        self.xk = [Tk() for _ in range(5)]
        self.hT = B(self.sb("hT", [128, 8, 512], BF16))
        self.cst = self.sb("cst", [128, 8], F32)
        self.cstk = Tk()
        self.identf = B(self.sb("identf", [128, 128], F32))
        self.identb = B(self.sb("identb", [128, 128], BF16))
        self.perm64 = B(self.sb("perm64", [128, 128], BF16))
        self.perm32 = B(self.sb("perm32", [128, 128], BF16))
        self.blk64 = B(self.sb("blk64", [128, 128], BF16))
        self.onesb = B(self.sb("onesb", [128, 128], BF16))
        self.vec = self.sb("vec", [128, 512], F32)
        self.veck = Tk()
        self.mod = B(self.sb("mod", [128, 48, 2], F32))
        self.drv = B(self.sb("drv", [128, 6, 8, 2], F32))
        self.ctxK = B(self.sb("ctxK", [128, 6, CTX], BF16))
        self.ctxV = B(self.sb("ctxV", [128, 2, 16, 128], BF16))
        self.keysT = B(self.sb("keysT", [128, 16, 128], BF16))
        self.AW = 27648
        self.arena = self.sb("arena", [128, self.AW], F32)
        self.aoff = 0

        S = self.S
        self.MSET("dve", self.cst[:, 0:1], EPS, [self.cstk])
        self.MSET("dve", self.cst[:, 1:2], 0.0, [self.cstk])
        self.MSET("dve", self.cst[:, 2:3], 1.0, [self.cstk])
        self.MSET("dve", self.onesb.a[:, :], 1.0, [self.onesb.k])
        cmat = self.din("cmat", [4, 128, 128])
        self.DMA("sp", self.identf.a[:, :], cmat[0], W=[self.identf.k])
        self.DMA("pool", self.identb.a[:, :], cmat[0], W=[self.identb.k])
        self.DMA("pool", self.perm64.a[:, :], cmat[1], W=[self.perm64.k])
        self.DMA("pool", self.perm32.a[:, :], cmat[2], W=[self.perm32.k])
        self.DMA("pool", self.blk64.a[:, :], cmat[3], W=[self.blk64.k])
        self.MSET("pool", self.ctxV.a[:, :, :, :], 1.0, [self.ctxV.k])

        xin = self.din("xT_in", [D, NTT])
        xv = xin.rearrange("(c p) t -> p c t", p=128)
        for b in range(5):
            t0, nt = self.blk(b)
            self.DMA("sp", self.xT[:, :, t0:t0 + nt], xv[:, :, t0:t0 + nt], W=[self.xk[b]])

        if self.fused:
            self.sel = self.sb("sel_sb", [128, 16], F32)
            self.selk = Tk()
            self.DMA("sp", self.sel[:, :], self.din("sel", [128, 16]), W=[self.selk])
            for l in (0, 1):
                self.layer_setup(l)
                outs = self.kv_outs(l)
                for b in range(4):
                    self.arena_reset()
                    self.block_kv(l, b, outs)
                self.exchange(l)
                self.arena_reset()
                self.block_kv(l, 4, None, ctx_only=True)
                for b in list(range(4)) + ([4] if l == 0 else []):
                    self.block_full(l, b)
            layers_b = [1]
        else:
            for l in layers_b:
                self.layer_setup(l)
                self.arena_reset()
                self.block_kv(l, 4, None, ctx_only=True)
                blocks = list(range(self.own_blocks)) + ([4] if l == 0 else [])
                for b in blocks:
                    self.block_full(l, b)
            for l in layers_a:
                self.layer_setup(l)
                outs = self.kv_outs(l)
                for b in range(self.own_blocks):
                    self.arena_reset()
                    self.block_kv(l, b, outs)
        if final:
            self.final_norm()
        elif layers_b:
            xo = self.dout("xT_out", [D, NTT]).rearrange("(c p) t -> p c t", p=128)
            for b in range(5):
                t0, nt = self.blk(b)
                self.DMA("sp", xo[:, :, t0:t0 + nt], self.xT[:, :, t0:t0 + nt], R=[self.xk[b]], W=[Tk()])
        S.barrier()
        sems = {e: self.es.enter_context(nc.semaphore("s_" + e)) for e in S.ENG}
        dsems = [self.es.enter_context(nc.semaphore("d%d" % i)) for i in range(S.ndma)]
        ccsems = [self.es.enter_context(nc.semaphore("c%d" % i)) for i in range(S.ncc)]
        block = self.es.enter_context(nc.Block())
        S.emit(block, sems, dsems, ccsems)
        self.es.close()

    def blk(self, b):
        return (b * 512, 512) if b < 4 else (TOK, CTX)

    def layer_setup(self, l):
        sfx = "_l%d" % l
        self.arena_reset()
        vec_d = self.din("vec" + sfx, [128, 512])
        self.DMA("sp", self.vec[:, :], vec_d, W=[self.veck])
        v = self.vec
        vk = self.veck
        keys_d = self.din("keysT" + sfx, [128, 16, 128])
        self.DMA("pool", self.keysT.a[:, :, :], keys_d, W=[self.keysT.k])
        sv = self.alb([128, 8, 2], F32)
        self.ACT(sv.a[:, :, 0], v[:, 104:112], AF.Silu, [vk], [sv.k])
        self.ACT(sv.a[:, :, 1], v[:, 112:120], AF.Silu, [vk], [sv.k])
        adaw = self.din("ada_w" + sfx, [D, 6 * D]).rearrange("(k p) n -> p k n", p=128)
        wr = self.ring(3, [128, 8, 128], F32)
        for j in range(48):
            wt = wr.next()
            self.DMA("sp", wt.a[:, :, :], adaw[:, :, j * 128:(j + 1) * 128], W=[wt.k])
            ps = self.RS.next()
            for k in range(8):
                self.MM(ps.a[:, 0:2], wt.a[:, k, :], sv.a[:, k, :], k == 0, k == 7, [wt.k, sv.k], [ps.k], sig=(k == 7))
            self.TS("dve", self.mod.a[:, j, :], ps.a[:, 0:2], v[:, 16 + j:17 + j], None, ALU.add, None,
                    [ps.k, vk], [self.mod.k])
        m = self.mod
        d = self.drv
        n1 = v[:, 0:8].unsqueeze(2).to_broadcast([128, 8, 2])
        n2 = v[:, 8:16].unsqueeze(2).to_broadcast([128, 8, 2])
        self.STT(d.a[:, 0, :, :], m.a[:, 8:16, :], 1.0, n1, ALU.add, ALU.mult, [m.k, vk], [d.k])
        self.CP("dve", d.a[:, 1, :, :], m.a[:, 0:8, :], [m.k], [d.k])
        self.CP("dve", d.a[:, 2, :, :], m.a[:, 16:24, :], [m.k], [d.k])
        self.STT(d.a[:, 3, :, :], m.a[:, 32:40, :], 1.0, n2, ALU.add, ALU.mult, [m.k, vk], [d.k])
        self.CP("dve", d.a[:, 4, :, :], m.a[:, 24:32, :], [m.k], [d.k])
        self.CP("dve", d.a[:, 5, :, :], m.a[:, 40:48, :], [m.k], [d.k])
        pr = self.alb([128, 64], F32)
        self.TT("dve", pr.a[:, 0:32], v[:, 128:160], v[:, 160:192], ALU.mult, [vk], [pr.k])
        self.TT("dve", pr.a[:, 32:64], v[:, 192:224], v[:, 224:256], ALU.mult, [vk], [pr.k])
        sm = self.alb([128, 4], F32)
        self.S.op("dve", lambda e: e.tensor_reduce(out=sm.a[:, 0:2], in_=pr.a[:, :].rearrange("p (a b) -> p a b", a=2),
                                                   axis=AX.X, op=ALU.add), reads=[pr.k], writes=[sm.k])
        self.ACT(sm.a[:, 2:4], sm.a[:, 0:2], AF.Exp, [sm.k], [sm.k])
        self.lamk = Tk()
        self.TT("dve", v[:, 256:257], sm.a[:, 2:3], sm.a[:, 3:4], ALU.subtract, [sm.k, vk], [self.lamk])
        lam_init = 0.8 - 0.6 * math.exp(-0.3 * l)
        self.lam_init = lam_init
        self.TS("dve", v[:, 256:257], v[:, 256:257], lam_init, None, ALU.add, None, [self.lamk], [self.lamk])
        self.ACT(v[:, 260:264], v[:, 120:124], AF.Exp, [vk], [self.lamk])
        self.dump("mod%d" % l, self.mod.a[:, :, :], self.mod.k, [128, 48, 2])

    def dr(self, which, c, ctx):
        return self.drv.a[:, which, c, (1 if ctx else 0):(2 if ctx else 1)]

    def norm_mod(self, b, wa, wb, tf, tb):
        t0, nt = self.blk(b)
        ctx = (b == 4)
        ps = self.RS.next()
        for c in range(8):
            sq = tb.next()
            self.ACT(sq.a[:, :nt], self.xT[:, c, t0:t0 + nt], AF.Square, [self.xk[b]], [sq.k])
            self.MM(ps.a[:, :nt], self.onesb.a[:, :], sq.a[:, :nt], c == 0, c == 7, [self.onesb.k, sq.k], [ps.k])
        rs = self.alb([128, 512], F32)
        self.ACT(rs.a[:, :nt], ps.a[:, :nt], AF.Sqrt, [ps.k, self.cstk], [rs.k], bias=self.cst[:, 0:1], scale=1.0 / D)
        self.RCP(rs.a[:, :nt], rs.a[:, :nt], [rs.k], [rs.k])
        for c in range(8):
            t = tf.next()
            self.TT("dve", t.a[:, :nt], self.xT[:, c, t0:t0 + nt], rs.a[:, :nt], ALU.mult, [self.xk[b], rs.k], [t.k])
            self.TS("pool", self.hT.a[:, c, :nt], t.a[:, :nt], self.dr(wa, c, ctx), self.dr(wb, c, ctx),
                    ALU.mult, ALU.add, [t.k, self.drv.k], [self.hT.k])

    def proj(self, wview, col0, nt, wring, ring=None):
        wt = wring.next()
        self.DMA("pool", wt.a[:, :, :], wview[:, :, col0:col0 + 128], W=[wt.k])
        ps = (ring or self.RX).next()
        for k in range(8):
            self.MM(ps.a[:, :nt], wt.a[:, k, :], self.hT.a[:, k, :nt], k == 0, k == 7, [wt.k, self.hT.k], [ps.k],
                    sig=(k == 7))
        return ps

    def rope(self, src, dst_ap, dst_k, nt, cos, sin, perm, tf):
        ps = self.RX.next()
        self.MM(ps.a[:, :nt], perm.a[:, :], src.a[:, :nt], True, True, [perm.k, src.k], [ps.k])
        t1 = tf.next()
        t2 = tf.next()
        self.TT("dve", t1.a[:, :nt], src.a[:, :nt], cos.a[:, :nt], ALU.mult, [src.k, cos.k], [t1.k])
        self.TT("dve", t2.a[:, :nt], ps.a[:, :nt], sin.a[:, :nt], ALU.mult, [ps.k, sin.k], [t2.k])
        self.TT("pool", dst_ap, t1.a[:, :nt], t2.a[:, :nt], ALU.add, [t1.k, t2.k], [dst_k])

    def qknorm(self, ps, nt, wcol, tf, tb, out):
        sq = tb.next()
        self.ACT(sq.a[:, :nt], ps.a[:, :nt], AF.Square, [ps.k], [sq.k])
        p2 = self.RX.next()
        self.MM(p2.a[:, :nt], self.blk64.a[:, :], sq.a[:, :nt], True, True, [self.blk64.k, sq.k], [p2.k])
        rs = tf.next()
        self.ACT(rs.a[:, :nt], p2.a[:, :nt], AF.Sqrt, [p2.k, self.cstk], [rs.k], bias=self.cst[:, 0:1], scale=1.0 / 64)
        self.RCP(rs.a[:, :nt], rs.a[:, :nt], [rs.k], [rs.k])
        t = tf.next()
        self.TT("dve", t.a[:, :nt], ps.a[:, :nt], rs.a[:, :nt], ALU.mult, [ps.k, rs.k], [t.k])
        self.TS("pool", out.a[:, :nt], t.a[:, :nt], self.vec[:, wcol:wcol + 1], None, ALU.mult, None,
                [t.k, self.veck], [out.k])

    def load_rope(self, b):
        if b == 4:
            return None
        t0, nt = self.blk(b)
        rt = self.din("ropeT", [4, 128, TOK])
        tabs = []
        for i in range(4):
            t = self.alb([128, 512], F32)
            self.DMA("sp", t.a[:, :], rt[i][:, t0:t0 + nt], W=[t.k])
            tabs.append(t)
        return tabs

    def dram(self, name, shape, dt):
        return self.nc.dram_tensor(name, list(shape), dt).ap()

    def kv_outs(self, l):
        sfx = "_l%d" % l
        if self.fused:
            d = {}
            d["s_kT"] = self.dram("s_kT" + sfx, [6 * 128, TOK], BF16)
            d["s_vd"] = self.dram("s_vd" + sfx, [8 * 128, 16 * 128], BF16)
            d["s_nv"] = self.dram("s_nv" + sfx, [4 * 64, ROWS * 128], BF16)
            d["s_sv"] = self.dram("s_sv" + sfx, [4 * 128, 16 * 128], BF16)
            d["g_kT"] = self.dram("g_kT" + sfx, [8 * 6 * 128, TOK], BF16)
            d["g_vd"] = self.dram("g_vd" + sfx, [8 * 8 * 128, 16 * 128], BF16)
            d["g_nv"] = self.dram("g_nv" + sfx, [8 * 4 * 64, ROWS * 128], BF16)
            d["g_sv"] = self.dram("g_sv" + sfx, [8 * 4 * 128, 16 * 128], BF16)
            d["naKwin"] = self.dram("naKwin" + sfx, [2, 128, NWIN * 64], BF16)
            d["naVwin"] = self.dram("naVwin" + sfx, [4, 64, NWIN, 128], BF16)
            d["swKwin"] = self.dram("swKwin" + sfx, [128, SWT], BF16)
            d["swVwin"] = self.dram("swVwin" + sfx, [4, 128, 18, 128], BF16)
            self.kvb[l] = d
            return dict(
                kT=d["s_kT"].rearrange("(c p) t -> c p t", p=128),
                vd=d["s_vd"].rearrange("(s p) (k c) -> s p k c", p=128, c=128),
                nv=d["s_nv"].rearrange("(h k) (r c) -> h k r c", k=64, c=128),
                sv=d["s_sv"].rearrange("(s p) (k c) -> s p k c", p=128, c=128),
            )
        return dict(
            kT=self.dout("kT_own" + sfx, [6, 128, TOK], BF16),
            vd=self.dout("VAd_own" + sfx, [8, 128, 16, 128], BF16),
            nv=self.dout("NV_own" + sfx, [4, 64, ROWS, 128], BF16),
            sv=self.dout("SV_own" + sfx, [4, 128, 16, 128], BF16),
        )

    def exchange(self, l):
        d = self.kvb[l]
        S = self.S
        S.barrier()
        for a, g in (("s_kT", "g_kT"), ("s_vd", "g_vd"), ("s_nv", "g_nv"), ("s_sv", "g_sv")):
            src, dst = d[a], d[g]
            S.cc((lambda src=src, dst=dst: lambda e: e.collective_compute(
                "AllGather", ALU.bypass, replica_groups=[list(range(NCORE))], ins=[src], outs=[dst]))())
        S.barrier()
        self.aoff = 0
        skT = d["s_kT"].rearrange("(c p) t -> c p t", p=128)
        snv = d["s_nv"].rearrange("(h k) (r c) -> h k r c", k=64, c=128)
        ssv = d["s_sv"].rearrange("(s p) (k c) -> s p k c", p=128, c=128)
        for c in range(2):
            self.DMA("sp", d["naKwin"][c][:, 448:448 + TOK], skT[c], W=[Tk()])
        for h in range(4):
            self.DMA("sp", d["naVwin"][h][:, 7:7 + ROWS, :], snv[h], W=[Tk()])
            self.DMA("sp", d["swVwin"][h][:, 1:17, :], ssv[h], W=[Tk()])
        self.DMA("sp", d["swKwin"][:, 128:128 + TOK], skT[4], W=[Tk()])
        gk = d["g_kT"].rearrange("(j c p) t -> p j c t", c=6, p=128)
        gn = d["g_nv"].rearrange("(j h k) (r c) -> k j h r c", h=4, k=64, c=128)
        gs = d["g_sv"].rearrange("(j s p) (t c) -> p j s t c", s=4, p=128, c=128)
        cring = self.ring(3, [128, 8, 896], BF16)
        aring = self.ring(2, [128, 896], F32)
        oring = self.ring(3, [128, 896], BF16)

        def select(cand, P, n, side, dst):
            cb = cring.next()
            self.DMA("sp", cb.a[0:P, :, 0:n], cand, W=[cb.k])
            acc = aring.next()
            self.TS("dve", acc.a[0:P, 0:n], cb.a[0:P, 0, 0:n], self.sel[0:P, side * 8:side * 8 + 1], None, ALU.mult, None,
                    [cb.k, self.selk], [acc.k])
            ob = oring.next()
            for j in range(1, 8):
                out = ob.a[0:P, 0:n] if j == 7 else acc.a[0:P, 0:n]
                self.STT(out, cb.a[0:P, j, 0:n], self.sel[0:P, side * 8 + j:side * 8 + j + 1], acc.a[0:P, 0:n],
                         ALU.mult, ALU.add, [cb.k, self.selk, acc.k], [ob.k if j == 7 else acc.k])
            self.DMA("sp", dst, ob.a[0:P, 0:n], R=[ob.k], W=[Tk()])
        for c in range(2):
            select(gk[:, :, c, TOK - 448:TOK], 128, 448, 0, d["naKwin"][c][:, 0:448])
            select(gk[:, :, c, 0:448], 128, 448, 1, d["naKwin"][c][:, 448 + TOK:448 + TOK + 448])
        select(gk[:, :, 4, TOK - 128:TOK], 128, 128, 0, d["swKwin"][:, 0:128])
        select(gk[:, :, 4, 0:128], 128, 128, 1, d["swKwin"][:, 128 + TOK:256 + TOK])
        for h in range(4):
            select(gn[:, :, h, ROWS - 7:ROWS, :].rearrange("k j r c -> k j (r c)"), 64, 896, 0,
                   d["naVwin"][h][:, 0:7, :].rearrange("k r c -> k (r c)"))
            select(gn[:, :, h, 0:7, :].rearrange("k j r c -> k j (r c)"), 64, 896, 1,
                   d["naVwin"][h][:, 7 + ROWS:14 + ROWS, :].rearrange("k r c -> k (r c)"))
            select(gs[:, :, h, 15, :], 128, 128, 0, d["swVwin"][h][:, 0, :])
            select(gs[:, :, h, 0, :], 128, 128, 1, d["swVwin"][h][:, 17, :])
        S.barrier()

    def block_kv(self, l, b, outs, ctx_only=False):
        sfx = "_l%d" % l
        t0, nt = self.blk(b)
        ctx = (b == 4)
        tf = self.ring(4, [128, 512], F32)
        tb = self.ring(3, [128, 512], BF16)
        wring = self.ring(3, [128, 8, 128], BF16)
        tabs = self.load_rope(b)
        self.norm_mod(b, 0, 1, tf, tb)
        win = self.din("w_in" + sfx, [D, 2560]).rearrange("(k p) n -> p k n", p=128)
        specs = [(256, None, False), (384, None, False), (1024, 32, False), (1152, 32, False),
                 (1792, 64, False), (2304, 64, True)]
        kst = self.ring(2, [128, 512], BF16)
        for ci, (col0, rk, nrm) in enumerate(specs):
            ps = self.proj(win, col0, nt, wring)
            if ctx:
                dst_ap, dst_k = self.ctxK.a[:, ci, :], self.ctxK.k
            else:
                st = kst.next()
                dst_ap, dst_k = st.a[:, :nt], st.k
            if nrm:
                xn = tb.next()
                self.qknorm(ps, nt, 125, tf, tb, xn)
                if ctx:
                    self.CP("act", dst_ap, xn.a[:, :nt], [xn.k], [dst_k])
                else:
                    self.rope(xn, dst_ap, dst_k, nt, tabs[0], tabs[1], self.perm64, tf)
            elif rk is None or ctx:
                self.CP("act", dst_ap, ps.a[:, :nt], [ps.k], [dst_k])
            else:
                xb = tb.next()
                self.CP("act", xb.a[:, :nt], ps.a[:, :nt], [ps.k], [xb.k])
                if rk == 64:
                    self.rope(xb, dst_ap, dst_k, nt, tabs[0], tabs[1], self.perm64, tf)
                else:
                    self.rope(xb, dst_ap, dst_k, nt, tabs[2], tabs[3], self.perm32, tf)
            if not ctx:
                self.DMA("sp", outs["kT"][ci][:, t0:t0 + nt], dst_ap, R=[dst_k], W=[Tk()])
        wv = self.din("w_v" + sfx, [D, 768]).rearrange("(k p) n -> p k n", p=128)
        wvt = self.alb([128, 8, 768], BF16)
        self.DMA("pool", wvt.a[:, :, :], wv, W=[wvt.k])
        if not ctx:
            vs = self.alb([128, 4, 16, 128], BF16)
            self.MSET("pool", vs.a[:, :, :, :], 1.0, [vs.k])
        for tt in range(nt // 128):
            pa = self.RX.next()
            pb = self.RX.next()
            for k in range(8):
                self.MM(pa.a[:, :512], self.hT.a[:, k, tt * 128:(tt + 1) * 128], wvt.a[:, k, 0:512], k == 0, k == 7,
                        [self.hT.k, wvt.k], [pa.k], sig=(k == 7))
            for k in range(8):
                self.MM(pb.a[:, :256], self.hT.a[:, k, tt * 128:(tt + 1) * 128], wvt.a[:, k, 512:768], k == 0, k == 7,
                        [self.hT.k, wvt.k], [pb.k], sig=(k == 7))
            if ctx:
                dst, dk = self.ctxV.a[:, tt, :, :], self.ctxV.k
            else:
                dst, dk = vs.a[:, tt, :, :], vs.k
            def hv(base_col, par, pa=pa):
                c0 = base_col + par * 64
                return self.sap(pa.a[:, c0:c0 + 1], [[128, 2], [1, 64]])
            def dv(slot0, par, dst=dst):
                return self.sap(dst[:, slot0 + par, par * 64:par * 64 + 1], [[256, 2], [1, 64]])
            self.CP("act", dv(8, 0), hv(0, 0), [pa.k], [dk])
            self.CP("dve", dv(8, 1), hv(0, 1), [pa.k], [dk])
            self.CP("act", dv(0, 0), hv(256, 0), [pa.k], [dk])
            self.CP("dve", dv(0, 1), hv(256, 1), [pa.k], [dk])
            def gsrc(base, pb=pb):
                return pb.a[:, base:base + 128].rearrange("p (g d) -> p g d", g=2)
            def gdst(slot0, par, dst=dst):
                return self.sap(dst[:, slot0 + par, par * 64:par * 64 + 1], [[256, 2], [1, 64]])
            self.CP("act", gdst(12, 0), gsrc(0), [pb.k], [dk])
            self.CP("dve", gdst(12, 1), gsrc(0), [pb.k], [dk])
            self.CP("act", gdst(4, 0), gsrc(128), [pb.k], [dk])
            self.CP("dve", gdst(4, 1), gsrc(128), [pb.k], [dk])
        if not ctx:
            kt0 = b * 4
            for s in range(8):
                self.DMA("sp", outs["vd"][s][:, kt0:kt0 + 4, :], vs.a[:, :, s, :], R=[vs.k], W=[Tk()])
            for s in range(4):
                self.DMA("sp", outs["sv"][s][:, kt0:kt0 + 4, :], vs.a[:, :, 12 + s, :], R=[vs.k], W=[Tk()])
                nvv = outs["nv"][s][:, 2 * kt0:2 * kt0 + 8, :].rearrange("k (t two) c -> two k t c", two=2)
                self.DMA("sp", nvv[0], vs.a[0:64, :, 8 + s, :], R=[vs.k], W=[Tk()])
                self.DMA("sp", nvv[1], vs.a[64:128, :, 8 + s, :], R=[vs.k], W=[Tk()])

    def finalize(self, O, par, nq, dst_ap, dst_k, tf, extra=None, mul=None):
        no = par * 64
        zo = (1 - par) * 64
        rz = tf.next()
        if extra is not None:
            self.TS("dve", rz.a[zo:zo + 64, :nq], O.a[zo:zo + 64, :nq], extra[zo:zo + 64, :], None, ALU.add, None,
                    [O.k, self.lamk], [rz.k])
            self.RCP(rz.a[zo:zo + 64, :nq], rz.a[zo:zo + 64, :nq], [rz.k], [rz.k])
        else:
            self.RCP(rz.a[zo:zo + 64, :nq], O.a[zo:zo + 64, :nq], [O.k], [rz.k])
        if mul is not None:
            self.TS("dve", rz.a[zo:zo + 64, :nq], rz.a[zo:zo + 64, :nq], mul[zo:zo + 64, :], None, ALU.mult, None,
                    [rz.k, self.lamk], [rz.k])
        self.TT("dve", dst_ap, O.a[no:no + 64, :nq], rz.a[zo:zo + 64, :nq], ALU.mult, [O.k, rz.k], [dst_k])

    def ctx_tiles(self, streams, O, nq, pt, scale, stop_last):
        for kt in range(2):
            for si, st in enumerate(streams):
                lo, hi = st["krows"]
                s = self.RS.next()
                self.MM(s.a[:, :nq], self.ctxK.a[lo:hi, st["ci"], kt * 128:(kt + 1) * 128], st["q"], True, True,
                        [self.ctxK.k, st["qk"]], [s.k])
                p = pt.next()
                self.ACT(p.a[:, :nq], s.a[:, :nq], AF.Exp, [s.k], [p.k], scale=scale)
                self.MM(O[si].a[:, :nq], self.ctxV.a[:, kt, st["cslot"], :], p.a[:, :nq], kt == 0,
                        stop_last and kt == 1, [self.ctxV.k, p.k], [O[si].k])

    def dense_pass(self, l, streams, nq, ci_d, vslots, pt, kring, vrings, scale):
        sfx = "_l%d" % l
        O = [self.RO.next() for _ in streams]
        self.ctx_tiles(streams, O, nq, pt, scale, False)
        NP = SEQ // 2048
        if self.fused:
            gk = self.kvb[l]["g_kT"].rearrange("(j c p) t -> j c p t", c=6, p=128)
            gv = self.kvb[l]["g_vd"].rearrange("(j s p) (k c) -> j s p k c", s=8, p=128, c=128)
            ksrc = lambda pc: gk[pc][(2, 3, 5)[ci_d]]
            vsrc = lambda vs, pc: gv[pc][vs]
        else:
            ktd = self.din("KTd" + sfx, [3, 128, SEQ], BF16)
            vad = self.din("VAd" + sfx, [8, 128, SEQ // 128, 128], BF16)
            ksrc = lambda pc: ktd[ci_d][:, pc * 2048:(pc + 1) * 2048]
            vsrc = lambda vs, pc: vad[vs][:, pc * 16:(pc + 1) * 16, :]

        def load(pc):
            kp = kring.next()
            self.DMA("sp", kp.a[:, :], ksrc(pc), W=[kp.k])
            vps = []
            for vi, vs in enumerate(vslots):
                vp = vrings[vi].next()
                self.DMA("sp", vp.a[:, :, :], vsrc(vs, pc), W=[vp.k])
                vps.append(vp)
            return kp, vps
        nxt = load(0)
        pend = []

        def flush():
            for (si, p, vt, stop) in pend:
                self.MM(O[si].a[:, :nq], vt[0], p.a[:, :nq], False, stop, [vt[1], p.k], [O[si].k])
            del pend[:]
        for pc in range(NP):
            kp, vps = nxt
            if pc + 1 < NP:
                flush()
                nxt = load(pc + 1)
            for kt in range(16):
                cur = []
                for si, st in enumerate(streams):
                    lo, hi = st["krows"]
                    s = self.RS6.next()
                    self.MM(s.a[:, :nq], kp.a[lo:hi, kt * 128:(kt + 1) * 128], st["q"], True, True, [kp.k, st["qk"]], [s.k])
                    p = pt.next()
                    self.ACT(p.a[:, :nq], s.a[:, :nq], AF.Exp, [s.k], [p.k], scale=scale)
                    vp = vps[st["vi"]]
                    cur.append((si, p, (vp.a[:, kt, :], vp.k), (pc == NP - 1 and kt == 15)))
                flush()
                pend.extend(cur)
        flush()
        return O

    def block_full(self, l, b):
        sfx = "_l%d" % l
        t0, nt = self.blk(b)
        ctx = (b == 4)
        v = self.vec
        self.arena_reset()
        qT = self.alb([128, 8, 512], BF16)
        ysT = self.alb([128, 8, 512], BF16)
        ysd = self.alb([128, 2, 512], F32)
        qz = self.alb([128, 2, 512], BF16)
        qpad = self.alb([128, 12, 512], BF16)
        keep = self.aoff
        tf = self.ring(4, [128, 512], F32)
        tb = self.ring(3, [128, 512], BF16)
        wring = self.ring(3, [128, 8, 128], BF16)
        tabs = self.load_rope(b)
        self.norm_mod(b, 0, 1, tf, tb)
        if self.dbg and b == 0:
            self.dump("h%d" % l, self.hT.a[:, :, :], self.hT.k, [128, 8, 512], BF16)
        win = self.din("w_in" + sfx, [D, 2560]).rearrange("(k p) n -> p k n", p=128)
        qspecs = [(0, None, False), (128, None, False), (768, 32, False), (896, 32, False),
                  (1536, 64, False), (1664, 64, False), (2048, 64, True), (2176, 64, True)]
        for qi, (col0, rk, nrm) in enumerate(qspecs):
            ps = self.proj(win, col0, nt, wring)
            dst_ap, dst_k = qT.a[:, qi, :nt], qT.k
            if nrm:
                xn = tb.next()
                self.qknorm(ps, nt, 124, tf, tb, xn)
                if ctx:
                    self.CP("act", dst_ap, xn.a[:, :nt], [xn.k], [dst_k])
                else:
                    self.rope(xn, dst_ap, dst_k, nt, tabs[0], tabs[1], self.perm64, tf)
            elif rk is None or ctx:
                self.CP("act", dst_ap, ps.a[:, :nt], [ps.k], [dst_k])
            else:
                xb = tb.next()
                self.CP("act", xb.a[:, :nt], ps.a[:, :nt], [ps.k], [xb.k])
                if rk == 64:
                    self.rope(xb, dst_ap, dst_k, nt, tabs[0], tabs[1], self.perm64, tf)
                else:
                    self.rope(xb, dst_ap, dst_k, nt, tabs[2], tabs[3], self.perm32, tf)
        for c in range(2):
            self.CP("dve", qz.a[64:128, c, :nt], qT.a[64:128, 2 + c, :nt], [qT.k], [qz.k])
            self.MSET("dve", qz.a[64:96, c, :nt], 0.0, [qz.k])
        self.MSET("pool", qpad.a[:, :, :], 0.0, [qpad.k])
        for c in range(2):
            for par in range(2):
                for m in range(2):
                    lo = par * 64 + m * 32
                    idx = c * 4 + par * 2 + m
                    if lo == 96:
                        self.CP("dve", qpad.a[64:128, idx, :nt], qz.a[64:128, c, :nt], [qz.k, qpad.k], [qpad.k])
                    else:
                        self.CP("dve", qpad.a[lo:lo + 32, idx, :nt], qT.a[lo:lo + 32, 2 + c, :nt], [qT.k, qpad.k], [qpad.k])
        for g in range(2):
            for par in range(2):
                self.CP("act", qpad.a[g * 64:g * 64 + 64, 8 + g * 2 + par, :nt], qT.a[g * 64:g * 64 + 64, 6 + par, :nt],
                        [qT.k, qpad.k], [qpad.k])
        if self.dbg and b == 0:
            self.dump("q%d" % l, qT.a[:, :, :], qT.k, [128, 8, 512], BF16)

        self.S.barrier()
        self.aoff = keep
        tf = self.ring(4, [128, 512], F32)
        pt = self.ring(4, [128, 512], BF16)
        nq = nt
        if not ctx:
            lr0 = 8 * b
            nak = self.alb([128, 2, 22 * 64], BF16)
            nakw = self.kvb[l]["naKwin"] if self.fused else self.din("naKwin" + sfx, [2, 128, NWIN * 64], BF16)
            for c in range(2):
                self.DMA("sp", nak.a[:, c, :], nakw[c][:, lr0 * 64:(lr0 + 22) * 64], W=[nak.k])
            navr = self.ring(2, [64, 22, 128], BF16)
            rpr = self.ring(2, [64, 15, 64], BF16)
            navw = self.kvb[l]["naVwin"] if self.fused else self.din("naVwin" + sfx, [4, 64, NWIN, 128], BF16)
            rpd = self.din("rpbr" + sfx, [4, 64, 15, 64])
            bn = self.alb([64, 512], F32)
            self.DMA("sp", bn.a[:, :], self.din("bandneg", [64, 512]), W=[bn.k])
        for h in range(4):
            c = h // 2
            po = (h % 2) * 64
            par = h % 2
            st = dict(krows=(po, po + 64), ci=c, q=qT.a[po:po + 64, c, :nq], qk=qT.k, cslot=8 + h)
            O = [self.RO.next()]
            self.ctx_tiles([st], O, nq, pt, SC64, ctx)
            if not ctx:
                nav = navr.next()
                self.DMA("sp", nav.a[:, :, :], navw[h][:, lr0:lr0 + 22, :], W=[nav.k])
                rp = rpr.next()
                self.DMA("pool", rp.a[:, :, :], rpd[h], W=[rp.k])
                for wr in range(22):
                    kr = lr0 - 7 + wr
                    r_lo = max(lr0, kr - 7)
                    r_hi = min(lr0 + 7, kr + 7)
                    nr = r_hi - r_lo + 1
                    n = nr * 64
                    qoff = (r_lo - lr0) * 64
                    e_lo = r_lo - kr + 7
                    s = self.RS.next()
                    self.MM(s.a[0:64, :n], nak.a[po:po + 64, c, wr * 64:(wr + 1) * 64], qT.a[po:po + 64, c, qoff:qoff + n],
                            True, True, [nak.k, qT.k], [s.k])
                    t = tf.next()
                    self.STT(t.a[0:64, :n].rearrange("p (r q) -> p r q", r=nr), s.a[0:64, :n].rearrange("p (r q) -> p r q", r=nr),
                             SC64, rp.a[:, e_lo:e_lo + nr, :], ALU.mult, ALU.add, [s.k, rp.k], [t.k])
                    bo = 17 * r_lo + 7 - kr
                    bnap = self.sap(bn.a[:, bo:bo + 1], [[17, nr], [0, 64]])
                    self.TT("pool", t.a[0:64, :n].rearrange("p (r q) -> p r q", r=nr),
                            t.a[0:64, :n].rearrange("p (r q) -> p r q", r=nr), bnap, ALU.add, [t.k, bn.k], [t.k])
                    p = pt.next()
                    self.ACT(p.a[0:64, :n], t.a[0:64, :n], AF.Exp, [t.k], [p.k])
                    self.MM(O[0].a[:, qoff:qoff + n], nav.a[:, wr, :], p.a[0:64, :n], False, wr == 21,
                            [nav.k, p.k], [O[0].k])
            self.finalize(O[0], par, nq, ysT.a[par * 64:par * 64 + 64, c, :nq], ysT.k, tf)

        self.S.barrier()
        self.aoff = keep
        tf = self.ring(4, [128, 512], F32)
        pt = self.ring(4, [128, 512], BF16)
        if not ctx:
            swk = self.alb([128, 768], BF16)
            swkw = self.kvb[l]["swKwin"] if self.fused else self.din("swKwin" + sfx, [128, SWT], BF16)
            self.DMA("sp", swk.a[:, :], swkw[:, t0:t0 + 768], W=[swk.k])
            swv = self.alb([128, 4, 6, 128], BF16)
            swvw = self.kvb[l]["swVwin"] if self.fused else self.din("swVwin" + sfx, [4, 128, 18, 128], BF16)
            for s4 in range(4):
                self.DMA("sp", swv.a[:, s4, :, :], swvw[s4][:, 4 * b:4 * b + 6, :], W=[swv.k])
            swm = self.alb([128, 6, 512], BF16)
            swmd = self.din("swm", [8, 128, 512])
            for j in range(6):
                mi = j
                if b == 0 and j == 0:
                    mi = 6
                if b == 3 and j == 5:
                    mi = 7
                self.DMA("pool", swm.a[:, j, :], swmd[mi], W=[swm.k])
        for h in range(4):
            g = h // 2
            par = h % 2
            qc = 4 + par
            st = dict(krows=(g * 64, g * 64 + 64), ci=4, q=qT.a[g * 64:g * 64 + 64, qc, :nq], qk=qT.k, cslot=12 + 2 * g + par)
            O = [self.RO.next()]
            self.ctx_tiles([st], O, nq, pt, SC64, ctx)
            if not ctx:
                for j in range(6):
                    s = self.RS.next()
                    self.MM(s.a[:, :nq], swk.a[g * 64:g * 64 + 64, j * 128:(j + 1) * 128], st["q"], True, True,
                            [swk.k, qT.k], [s.k])
                    p = pt.next()
                    self.ACT(p.a[:, :nq], s.a[:, :nq], AF.Exp, [s.k], [p.k], scale=SC64)
                    p2 = pt.next()
                    self.TT("pool", p2.a[:, :nq], p.a[:, :nq], swm.a[:, j, :nq], ALU.mult, [p.k, swm.k], [p2.k])
                    self.MM(O[0].a[:, :nq], swv.a[:, 2 * g + par, j, :], p2.a[:, :nq], False, j == 5, [swv.k, p2.k], [O[0].k])
            self.finalize(O[0], par, nq, ysT.a[par * 64:par * 64 + 64, 4 + g, :nq], ysT.k, tf,
                          extra=v[:, 260 + h:261 + h])

        self.S.barrier()
        self.aoff = keep
        tf = self.ring(4, [128, 512], F32)
        tb = self.ring(2, [128, 512], BF16)
        pt = self.ring(8, [128, 512], BF16)
        if not ctx:
            kring = self.ring(2, [128, 2048], BF16)
            vrings = [self.ring(2, [128, 16, 128], BF16), self.ring(2, [128, 16, 128], BF16)]
        for h in range(4):
            c = h // 2
            par = h % 2
            sts = []
            for m in range(2):
                sts.append(dict(krows=(0, 128), ci=2 + c, q=qpad.a[:, c * 4 + par * 2 + m, :nq], qk=qpad.k, cslot=h, vi=0))
            if ctx:
                O = [self.RO.next(), self.RO.next()]
                self.ctx_tiles(sts, O, nq, pt, SC32, True)
            else:
                O = self.dense_pass(l, sts, nq, c, [h], pt, kring, vrings, SC32)
            a0 = tf.next()
            a1 = tf.next()
            ro = par * 64
            self.finalize(O[0], par, nq, a0.a[ro:ro + 64, :nq], a0.k, tf)
            self.finalize(O[1], par, nq, a1.a[ro:ro + 64, :nq], a1.k, tf, mul=v[:, 256:257])
            self.TT("dve", ysd.a[ro:ro + 64, c, :nq], a0.a[ro:ro + 64, :nq], a1.a[ro:ro + 64, :nq], ALU.subtract,
                    [a0.k, a1.k], [ysd.k])
            if par == 1:
                sq = tb.next()
                self.ACT(sq.a[:, :nq], ysd.a[:, c, :nq], AF.Square, [ysd.k], [sq.k])
                p2 = self.RX.next()
                self.MM(p2.a[:, :nq], self.blk64.a[:, :], sq.a[:, :nq], True, True, [self.blk64.k, sq.k], [p2.k])
                rs = tf.next()
                self.ACT(rs.a[:, :nq], p2.a[:, :nq], AF.Sqrt, [p2.k, self.cstk], [rs.k], bias=self.cst[:, 0:1], scale=1.0 / 64)
                self.RCP(rs.a[:, :nq], rs.a[:, :nq], [rs.k], [rs.k])
                t = tf.next()
                self.TT("dve", t.a[:, :nq], ysd.a[:, c, :nq], rs.a[:, :nq], ALU.mult, [ysd.k, rs.k], [t.k])
                self.TS("pool", ysT.a[:, 2 + c, :nq], t.a[:, :nq], v[:, 126:127], 1.0 - self.lam_init, ALU.mult, ALU.mult,
                        [t.k, self.veck], [ysT.k])

        for g in range(2):
            sts = []
            for par in range(2):
                sts.append(dict(krows=(0, 128), ci=5, q=qpad.a[:, 8 + g * 2 + par, :nq], qk=qpad.k,
                                cslot=4 + 2 * g + par, vi=par))
            if ctx:
                O = [self.RO.next(), self.RO.next()]
                self.ctx_tiles(sts, O, nq, pt, SC64, True)
            else:
                O = self.dense_pass(l, sts, nq, 2, [4 + 2 * g, 5 + 2 * g], pt, kring, vrings, SC64)
            for par in range(2):
                self.finalize(O[par], par, nq, ysT.a[par * 64:par * 64 + 64, 6 + g, :nq], ysT.k, tf)
        if self.dbg and b in (0, 4):
            self.dump("ys%d_%d" % (l, b), ysT.a[:, :, :], ysT.k, [128, 8, 512], BF16)

        self.S.barrier()
        self.aoff = keep
        tf = self.ring(3, [128, 512], F32)
        wring = self.ring(3, [128, 8, 128], BF16)
        wbr = self.alb([128, 8, 1024], BF16)
        self.DMA("pool", wbr.a[:, :, :], self.din("w_branch" + sfx, [4, 256, D]).rearrange("n (w p) d -> p (n w) d", p=128),
                 W=[wbr.k])
        G = self.alb([128, 8, 512], BF16)
        Gf = self.ring(2, [128, 512], F32)
        gt = self.ring(2, [128, 512], F32)
        wg = self.din("w_gate" + sfx, [D, 4 * D]).rearrange("(k p) n -> p k n", p=128)
        for dc in range(8):
            gf = Gf.next()
            for n in range(4):
                ps = self.proj(wg, n * D + dc * 128, nt, wring, self.RS)
                ga = gt.next()
                self.ACT(ga.a[:, :nt], ps.a[:, :nt], AF.Sigmoid, [ps.k, self.veck], [ga.k],
                         bias=v[:, 64 + n * 8 + dc:65 + n * 8 + dc])
                pb = self.RX.next()
                for w in range(2):
                    self.MM(pb.a[:, :nt], wbr.a[:, 2 * n + w, dc * 128:(dc + 1) * 128], ysT.a[:, 2 * n + w, :nt], w == 0, w == 1,
                            [wbr.k, ysT.k], [pb.k], sig=(w == 1))
                if n == 0:
                    self.TT("dve", gf.a[:, :nt], ga.a[:, :nt], pb.a[:, :nt], ALU.mult, [ga.k, pb.k], [gf.k])
                else:
                    t = tf.next()
                    self.TT("dve", t.a[:, :nt], ga.a[:, :nt], pb.a[:, :nt], ALU.mult, [ga.k, pb.k], [t.k])
                    if n < 3:
                        self.TT("pool", gf.a[:, :nt], gf.a[:, :nt], t.a[:, :nt], ALU.add, [gf.k, t.k], [gf.k])
                    else:
                        self.TT("pool", G.a[:, dc, :nt], gf.a[:, :nt], t.a[:, :nt], ALU.add, [gf.k, t.k], [G.k])
        wo = self.din("w_out" + sfx, [D, D]).rearrange("(k p) n -> p k n", p=128)
        for dc in range(8):
            wt = wring.next()
            self.DMA("pool", wt.a[:, :, :], wo[:, :, dc * 128:(dc + 1) * 128], W=[wt.k])
            ps = self.RX.next()
            for k in range(8):
                self.MM(ps.a[:, :nt], wt.a[:, k, :], G.a[:, k, :nt], k == 0, k == 7, [wt.k, G.k], [ps.k], sig=(k == 7))
            self.STT(self.xT[:, dc, t0:t0 + nt], ps.a[:, :nt], self.dr(2, dc, ctx), self.xT[:, dc, t0:t0 + nt],
                     ALU.mult, ALU.add, [ps.k, self.drv.k, self.xk[b]], [self.xk[b]])
        if self.dbg and b in (0, 4):
            self.dump("xmid%d_%d" % (l, b), self.xT[:, :, t0:t0 + nt], self.xk[b], [128, 8, nt])

        self.peer(l, b)
        if self.dbg and b in (0, 4):
            self.dump("xout%d_%d" % (l, b), self.xT[:, :, t0:t0 + nt], self.xk[b], [128, 8, nt])

    def peer(self, l, b):
        sfx = "_l%d" % l
        t0, nt = self.blk(b)
        ctx = (b == 4)
        ntile = nt // 128
        self.arena_reset()
        sc = self.alb([128, 4, 16, 128], F32)
        stat = self.alb([128, 4, 8, 4], F32)
        keep = self.aoff
        tf = self.ring(3, [128, 512], F32)
        tb = self.ring(3, [128, 512], BF16)
        self.norm_mod(b, 3, 4, tf, tb)
        wring = self.ring(3, [128, 8, 128], BF16)
        qp = self.alb([128, 16, 512], BF16)
        wq = self.din("peer_wq" + sfx, [D, 2048]).rearrange("(k p) n -> p k n", p=128)
        for j in range(16):
            ps = self.proj(wq, j * 128, nt, wring)
            self.CP("act" if j % 2 else "dve", qp.a[:, j, :nt], ps.a[:, :nt], [ps.k], [qp.k])
        for tt in range(ntile):
            for q4 in range(4):
                ps = self.RS.next()
                for i in range(4):
                    j = q4 * 4 + i
                    self.MM(ps.a[:, i * 128:(i + 1) * 128], qp.a[:, j, tt * 128:(tt + 1) * 128], self.keysT.a[:, j, :],
                            True, True, [qp.k, self.keysT.k], [ps.k], sig=(i == 3))
                self.CP("act" if q4 % 2 else "dve", sc.a[:, tt, q4 * 4:(q4 + 1) * 4, :],
                        ps.a[:, :].rearrange("p (a b) -> p a b", a=4), [ps.k], [sc.k])
        top = self.alb([128, 16, 16], F32)
        mx = self.alb([128, 16, 8], F32)
        wk = self.ring(2, [128, 256], F32)
        cand = self.alb([128, 256], F32)
        best = self.alb([128, 16], F32)
        c3 = cand.a[:, :].rearrange("p (a b) -> p a b", a=16)
        for tt in range(ntile):
            for j in range(16):
                self.MAX8(mx.a[:, j, :], sc.a[:, tt, j, :], [sc.k], [mx.k])
                self.TS("dve", mx.a[:, j, 1:2], mx.a[:, j, 0:1], -1.0, None, ALU.mult, None, [mx.k], [mx.k])
                self.ACT(sc.a[:, tt, j, :], sc.a[:, tt, j, :], AF.Exp, [sc.k, mx.k], [sc.k], bias=mx.a[:, j, 1:2])
            for j in range(16):
                self.MAX8(top.a[:, j, 0:8], sc.a[:, tt, j, :], [sc.k], [top.k])
                w1 = wk.next()
                self.MREP(w1.a[:, 0:128], top.a[:, j, 0:8], sc.a[:, tt, j, :], [sc.k, top.k], [w1.k])
                self.MAX8(top.a[:, j, 8:16], w1.a[:, 0:128], [w1.k], [top.k])
            for h in range(8):
                sa = stat.a[:, tt, h, :]
                b0 = top.a[:, 2 * h, :].unsqueeze(2).to_broadcast([128, 16, 16])
                b1 = top.a[:, 2 * h + 1, :].unsqueeze(1).to_broadcast([128, 16, 16])
                for rnd in range(2):
                    self.TT("dve" if h in (3, 7) else "pool", c3, b0, b1, ALU.mult, [top.k], [cand.k])
                    self.MAX8(best.a[:, 0:8], cand.a[:, :], [cand.k], [best.k])
                    w1 = wk.next()
                    self.MREP(w1.a[:, :], best.a[:, 0:8], cand.a[:, :], [cand.k, best.k], [w1.k])
                    self.MAX8(best.a[:, 8:16], w1.a[:, :], [w1.k], [best.k])
                    if rnd == 0:
                        self.S.op("dve", (lambda sa=sa: lambda e: e.tensor_reduce(out=sa[:, 2:3], in_=best.a[:, :], axis=AX.X,
                                                                                   op=ALU.add))(), reads=[best.k], writes=[stat.k])
                        self.RCP(sa[:, 1:2], sa[:, 2:3], [stat.k], [stat.k])
                        self.TS("dve", sc.a[:, tt, 2 * h, :], sc.a[:, tt, 2 * h, :], sa[:, 1:2], None, ALU.mult, None,
                                [sc.k, stat.k], [sc.k])
                        self.TS("dve", top.a[:, 2 * h, :], top.a[:, 2 * h, :], sa[:, 1:2], None, ALU.mult, None,
                                [top.k, stat.k], [top.k])
                    else:
                        self.CP("dve", sa[:, 0:1], best.a[:, 15:16], [best.k], [stat.k])
        if self.dbg and b == 0:
            self.dump("pstat%d" % l, stat.a[:, :, :, :], stat.k, [128, 4, 8, 4])
        self.S.barrier()
        self.aoff = keep
        acc = self.alb([128, 4, 1024], F32)
        zb = self.alb([128, 512], BF16)
        self.MSET("dve", zb.a[:, :], 0.0, [zb.k])
        GE = 512
        NG = 16384 // GE
        utr = self.ring(2, [128, 8, GE], BF16)
        vwr = self.ring(2, [128, 4, 1024], BF16)
        tP = self.ring(6, [128, 512], F32)
        tM = self.ring(6, [128, 512], BF16)
        Ag = self.ring(2, [128, 512], BF16)
        WAT = self.ring(2, [128, 4, 128], BF16)
        uT = self.din("uT" + sfx, [D, 16384]).rearrange("(k p) e -> p k e", p=128)
        vv = self.din("peer_v" + sfx, [16384, D]).rearrange("(g c p) d -> g p c d", c=4, p=128)

        def load(g):
            ut = utr.next()
            self.DMA("pool", ut.a[:, :, :], uT[:, :, g * GE:(g + 1) * GE], W=[ut.k])
            vw = vwr.next()
            self.DMA("pool", vw.a[:, :, :], vv[g], W=[vw.k])
            return ut, vw
        items = [(g, tt) for g in range(NG) for tt in range(ntile)]
        loads = {0: load(0)}
        st_ = {}

        def emit_AT(i, c4):
            g, tt = items[i]
            ut = loads[g][0]
            if c4 == 0:
                st_[i] = {"pa": self.RS.next()}
            pa = st_[i]["pa"]
            for k in range(8):
                self.MM(pa.a[:, c4 * 128:(c4 + 1) * 128], ut.a[:, k, c4 * 128:(c4 + 1) * 128],
                        self.hT.a[:, k, tt * 128:(tt + 1) * 128], k == 0, k == 7, [self.hT.k, ut.k], [pa.k], sig=(k == 7))
            if c4 == 3:
                ag = Ag.next()
                self.ACT(ag.a[:, :], pa.a[:, :], AF.Gelu, [pa.k], [ag.k])
                st_[i]["ag"] = ag

        def emit_wat(i):
            d = st_[i]
            wat = WAT.next()
            self.TT("dve", wat.a[:, :, :], d["ag"].a[:, :].rearrange("p (a b) -> p a b", a=4),
                    d["pw"].a[:, :].rearrange("p (a b) -> p a b", a=4), ALU.mult, [d["ag"].k, d["pw"].k], [wat.k])
            d["wat"] = wat

        def emit_out(i, half):
            g, tt = items[i]
            vw = loads[g][1]
            wat = st_[i]["wat"]
            po = (self.RO if half == 0 else self.RX).next()
            for c4 in range(4):
                self.MM(po.a[:, :], wat.a[:, c4, :], vw.a[:, c4, half * 512:(half + 1) * 512], c4 == 0, c4 == 3,
                        [wat.k, vw.k], [po.k], sig=(c4 == 3))
            if g == 0:
                self.CP("dve", acc.a[:, tt, half * 512:(half + 1) * 512], po.a[:, :], [po.k], [acc.k])
            else:
                self.TT("dve", acc.a[:, tt, half * 512:(half + 1) * 512], acc.a[:, tt, half * 512:(half + 1) * 512],
                        po.a[:, :], ALU.add, [acc.k, po.k], [acc.k])
        for c4 in range(4):
            emit_AT(0, c4)
        for i, (g, tt) in enumerate(items):
            i0 = g * 4
            if tt == min(1, ntile - 1) and g + 1 < NG and (g + 1) not in loads:
                loads[g + 1] = load(g + 1)
            if i > 0:
                emit_wat(i - 1)
            pw = self.RS.next()
            st_[i]["pw"] = pw
            self.MM(pw.a[:, :], zb.a[:, 0:128], zb.a[:, :], True, False, [zb.k], [pw.k], sig=False)
            for h in range(8):
                pp = tP.next()
                p3 = pp.a[:, :].rearrange("p (i j) -> p i j", i=4)
                self.TT("dve" if h in (3, 7) else "pool", p3,
                        sc.a[:, tt, 2 * h, i0:i0 + 4].unsqueeze(2).to_broadcast([128, 4, 128]),
                        sc.a[:, tt, 2 * h + 1, :].unsqueeze(1).to_broadcast([128, 4, 128]), ALU.mult, [sc.k], [pp.k])
                tm = tM.next()
                self.STT(tm.a[:, :], pp.a[:, :], stat.a[:, tt, h, 0:1], pp.a[:, :], ALU.is_ge, ALU.mult,
                         [pp.k, stat.k], [tm.k])
                for c4 in range(4):
                    self.MM(pw.a[:, c4 * 128:(c4 + 1) * 128], tm.a[:, c4 * 128:(c4 + 1) * 128], self.identb.a[:, :],
                            False, h == 7, [tm.k, self.identb.k], [pw.k], sig=(c4 == 3))
                if h < 4 and i + 1 < len(items):
                    emit_AT(i + 1, h)
                if h in (4, 5) and i > 0:
                    emit_out(i - 1, h - 4)
                    if h == 5:
                        del st_[i - 1]
        last = len(items) - 1
        emit_wat(last)
        emit_out(last, 0)
        emit_out(last, 1)
        for tt in range(ntile):
            for half in range(2):
                pt4 = self.RX.next()
                for c4 in range(4):
                    dc = half * 4 + c4
                    self.TR(pt4.a[:, c4 * 128:(c4 + 1) * 128], acc.a[:, tt, dc * 128:(dc + 1) * 128], self.identf.a[:, :],
                            [acc.k, self.identf.k], [pt4.k], sig=(c4 == 3))
                for c4 in range(4):
                    dc = half * 4 + c4
                    xs = self.xT[:, dc, t0 + tt * 128:t0 + (tt + 1) * 128]
                    self.STT(xs, pt4.a[:, c4 * 128:(c4 + 1) * 128], self.dr(5, dc, ctx), xs, ALU.mult, ALU.add,
                             [pt4.k, self.drv.k, self.xk[b]], [self.xk[b]])

    def final_norm(self):
        self.arena_reset()
        tf = self.ring(4, [128, 512], F32)
        tb = self.ring(3, [128, 512], BF16)
        oT = self.dout("outT", [D, TOK]).rearrange("(c p) t -> p c t", p=128)
        ob = self.ring(2, [128, 8, 512], F32)
        rsr = self.ring(2, [128, 512], F32)
        for b in range(self.own_blocks):
            t0, nt = self.blk(b)
            ps = self.RS.next()
            for c in range(8):
                sq = tb.next()
                self.ACT(sq.a[:, :nt], self.xT[:, c, t0:t0 + nt], AF.Square, [self.xk[b]], [sq.k])
                self.MM(ps.a[:, :nt], self.onesb.a[:, :], sq.a[:, :nt], c == 0, c == 7, [self.onesb.k, sq.k], [ps.k])
            rs = rsr.next()
            self.ACT(rs.a[:, :nt], ps.a[:, :nt], AF.Sqrt, [ps.k, self.cstk], [rs.k], bias=self.cst[:, 0:1], scale=1.0 / D)
            self.RCP(rs.a[:, :nt], rs.a[:, :nt], [rs.k], [rs.k])
            o = ob.next()
            for c in range(8):
                t = tf.next()
                self.TT("dve", t.a[:, :nt], self.xT[:, c, t0:t0 + nt], rs.a[:, :nt], ALU.mult, [self.xk[b], rs.k], [t.k])
                self.TS("pool", o.a[:, c, :nt], t.a[:, :nt], self.vec[:, 96 + c:97 + c], None, ALU.mult, None,
                        [t.k, self.veck], [o.k])
            self.DMA("sp", oT[:, :, t0:t0 + nt], o.a[:, :, :], R=[o.k], W=[Tk()])


def _cols(vv, n):
    return np.ascontiguousarray(np.asarray(vv, np.float32).reshape(n, 128).T)


def _consts():
    ident = np.eye(128, dtype=np.float32)

    def perm(dh):
        q = dh // 4
        P = np.zeros((128, 128), np.float32)
        for blk in range(128 // dh):
            o = blk * dh
            for i in range(q):
                P[o + q + i, o + i] = -1.0
                P[o + i, o + q + i] = 1.0
                P[o + 3 * q + i, o + 2 * q + i] = -1.0
                P[o + 2 * q + i, o + 3 * q + i] = 1.0
        return P
    blk64 = np.zeros((128, 128), np.float32)
    blk64[0:64, 0:64] = 1.0
    blk64[64:128, 64:128] = 1.0
    cmat = np.stack([ident, perm(64), perm(32), blk64])
    jj = np.arange(128)[:, None]
    ii = np.arange(128)[None, :]
    swm = np.zeros((8, 128, 512), np.float32)
    for j in range(6):
        for qb in range(4):
            dlt = (j - 1) - qb
            if dlt == -1:
                m = (jj >= ii)
            elif dlt == 0:
                m = np.ones((128, 128), bool)
            elif dlt == 1:
                m = (jj <= ii)
            else:
                m = np.zeros((128, 128), bool)
            swm[j, :, qb * 128:(qb + 1) * 128] = m
    return cmat, swm


def _rope_tables(core):
    t = np.arange(TOK, dtype=np.int64) + core * TOK
    row = (t // GW).astype(np.float32)
    col = (t % GW).astype(np.float32)
    out = np.zeros((4, 128, TOK), np.float32)
    for ti, dh in ((0, 64), (2, 32)):
        nf = dh // 4
        inv = (np.float32(10000.0) ** (-(np.arange(nf, dtype=np.float32)) / np.float32(nf))).astype(np.float32)
        ang_r = (row[:, None] * inv[None, :]).astype(np.float32)
        ang_c = (col[:, None] * inv[None, :]).astype(np.float32)
        ang = np.concatenate([ang_r, ang_r, ang_c, ang_c], axis=-1)
        reps = 128 // dh
        out[ti] = np.tile(np.cos(ang).T, (reps, 1))
        out[ti + 1] = np.tile(np.sin(ang).T, (reps, 1))
    return out


def _bandneg(core):
    bn = np.zeros((512,), np.float32)
    r0 = core * ROWS
    for r in range(ROWS):
        rg = r0 + r
        start = min(max(rg - 4, 0), 256 - 8)
        for e in range(15):
            krg = rg - e + 7
            ok = (start <= krg <= start + 7)
            bn[r * 16 + e] = 0.0 if ok else NEG
    return np.ascontiguousarray(np.tile(bn[None, :], (64, 1)))


def _layer_inputs(inp, l):
    sfx = "_l%d" % l
    f = lambda a: np.ascontiguousarray(np.asarray(a, np.float32))
    vec = np.zeros((128, 512), np.float32)
    vec[:, 0:8] = _cols(inp["norm1_w"][l], 8)
    vec[:, 8:16] = _cols(inp["norm2_w"][l], 8)
    vec[:, 16:64] = _cols(inp["ada_b"][l], 48)
    vec[:, 64:96] = _cols(inp["b_gate"][l], 32)
    vec[:, 96:104] = _cols(inp["final_norm_w"], 8)
    vec[:, 104:112] = _cols(np.asarray(inp["c"]).reshape(-1), 8)
    vec[:, 112:120] = _cols(inp["c_ctx"], 8)
    vec[:, 120:124] = np.asarray(inp["swa_sink"][l], np.float32)[None, :]
    vec[:, 124] = np.tile(np.asarray(inp["gqa_qk_norm_w"][l][0], np.float32), 2)
    vec[:, 125] = np.tile(np.asarray(inp["gqa_qk_norm_w"][l][1], np.float32), 2)
    vec[:, 126] = np.tile(np.asarray(inp["diff_subln_w"][l], np.float32), 2)
    vec[:, 128:256] = np.asarray(inp["diff_lam"][l], np.float32).reshape(1, 128)
    w_in = np.array(inp["w_in"][l], np.float32)
    for base in (1536, 2048):
        blk = w_in[:, base:base + 256].reshape(D, 4, 64)
        w_in[:, base:base + 256] = blk[:, [0, 2, 1, 3], :].reshape(D, 256)
    w_v = np.concatenate([w_in[:, 512:768], w_in[:, 1280:1536], w_in[:, 1920:2048], w_in[:, 2432:2560]], axis=1)
    rpb = np.asarray(inp["na_rpb"][l], np.float32)
    kc = np.arange(64)[:, None]
    qc = np.arange(64)[None, :]
    coff = np.clip(kc - qc, -15, 15) + 15
    cs = np.clip(qc - 8, 0, 48)
    ok = (kc >= cs) & (kc < cs + 16)
    rp = np.full((4, 64, 15, 64), NEG, np.float32)
    for e in range(15):
        val = rpb[:, 14 - e, :][:, coff]
        rp[:, :, e, :] = np.where(ok[None], val, NEG)
    keysT = np.ascontiguousarray(np.transpose(np.asarray(inp["peer_keys"][l], np.float32), (3, 1, 0, 2)).reshape(128, 16, 128))
    return {
        "vec" + sfx: vec, "ada_w" + sfx: f(inp["ada_w"][l]), "w_in" + sfx: np.ascontiguousarray(w_in),
        "w_v" + sfx: np.ascontiguousarray(w_v), "rpbr" + sfx: rp, "keysT" + sfx: keysT,
        "w_branch" + sfx: f(inp["w_branch"][l]), "w_gate" + sfx: f(inp["w_gate"][l]), "w_out" + sfx: f(inp["w_out"][l]),
        "peer_wq" + sfx: f(inp["peer_wq"][l]), "uT" + sfx: np.ascontiguousarray(np.asarray(inp["peer_u"][l], np.float32).T),
        "peer_v" + sfx: f(inp["peer_v"][l]),
    }


def _gather_kv(results, l):
    sfx = "_l%d" % l
    bf = ml_dtypes.bfloat16
    kT = np.concatenate([np.asarray(r["kT_own" + sfx]) for r in results], axis=2)
    VAd = np.concatenate([np.asarray(r["VAd_own" + sfx]) for r in results], axis=2)
    NV = np.concatenate([np.asarray(r["NV_own" + sfx]) for r in results], axis=2)
    SV = np.concatenate([np.asarray(r["SV_own" + sfx]) for r in results], axis=2)
    KTd = np.ascontiguousarray(kT[[2, 3, 5]])
    outs = []
    for i in range(NCORE):
        r0 = i * ROWS
        nak = np.zeros((2, 128, NWIN * 64), bf)
        nav = np.zeros((4, 64, NWIN, 128), bf)
        lo = max(r0 - 7, 0)
        hi = min(r0 + ROWS + 7, 256)
        nak[:, :, (lo - (r0 - 7)) * 64:(hi - (r0 - 7)) * 64] = kT[0:2, :, lo * 64:hi * 64]
        nav[:, :, lo - (r0 - 7):hi - (r0 - 7), :] = NV[:, :, lo:hi, :]
        swk = np.zeros((128, SWT), bf)
        t_lo = max(i * TOK - 128, 0)
        t_hi = min((i + 1) * TOK + 128, SEQ)
        swk[:, t_lo - (i * TOK - 128):t_hi - (i * TOK - 128)] = kT[4][:, t_lo:t_hi]
        swv = np.zeros((4, 128, 18, 128), bf)
        k_lo = max(i * 16 - 1, 0)
        k_hi = min((i + 1) * 16 + 1, 128)
        swv[:, :, k_lo - (i * 16 - 1):k_hi - (i * 16 - 1), :] = SV[:, :, k_lo:k_hi, :]
        outs.append({"KTd" + sfx: KTd, "VAd" + sfx: VAd, "naKwin" + sfx: nak, "naVwin" + sfx: nav,
                     "swKwin" + sfx: swk, "swVwin" + sfx: swv})
    return outs


def _percore(swm):
    out = []
    for i in range(NCORE):
        m = swm.copy()
        if i > 0:
            m[6] = swm[0]
        if i < NCORE - 1:
            m[7] = swm[5]
        sel = np.zeros((128, 16), np.float32)
        if i > 0:
            sel[:, i - 1] = 1.0
        if i < NCORE - 1:
            sel[:, 8 + i + 1] = 1.0
        out.append({"ropeT": _rope_tables(i), "bandneg": _bandneg(i), "swm": m, "sel": sel})
    return out


_PROG = {}


def _prog(key, *args, **kw):
    if key not in _PROG:
        _PROG[key] = KB(*args, **kw)
    return _PROG[key]


def _run(kb, provs):
    in_maps = []
    for i in range(NCORE):
        m = {}
        for name in kb.ins:
            for p in provs:
                src = p[i] if isinstance(p, list) else p
                if name in src:
                    m[name] = src[name]
                    break
            else:
                raise KeyError(name)
        in_maps.append(m)
    res = run_bass_kernel_spmd(kb.nc, in_maps, core_ids=list(range(NCORE)))
    return res.results


def kernel_unfused(**inp):
    x = np.asarray(inp["x"], np.float32)[0]
    ctx = np.asarray(inp["ctx"], np.float32)[0]
    cmat, swm = _consts()
    common = {"cmat": cmat}
    percore = _percore(swm)
    L = [_layer_inputs(inp, 0), _layer_inputs(inp, 1)]
    xin = [{"xT_in": np.ascontiguousarray(np.concatenate([x[i * TOK:(i + 1) * TOK], ctx], axis=0).T)} for i in range(NCORE)]
    r0 = _run(_prog("s0", [0], [], False), [xin, common, percore, L[0]])
    kv0 = _gather_kv(r0, 0)
    r1 = _run(_prog("s1", [1], [0], False), [xin, common, percore, L[0], L[1], kv0])
    kv1 = _gather_kv(r1, 1)
    xin2 = [{"xT_in": np.asarray(r["xT_out"])} for r in r1]
    r2 = _run(_prog("s2", [], [1], True), [xin2, common, percore, L[1], kv1])
    out = np.concatenate([np.asarray(r["outT"]).T for r in r2], axis=0)
    return out[None].astype(np.float32)


def kernel(**inp):
    x = np.asarray(inp["x"], np.float32)[0]
    ctx = np.asarray(inp["ctx"], np.float32)[0]
    cmat, swm = _consts()
    common = {"cmat": cmat}
    percore = _percore(swm)
    L = [_layer_inputs(inp, 0), _layer_inputs(inp, 1)]
    xin = [{"xT_in": np.ascontiguousarray(np.concatenate([x[i * TOK:(i + 1) * TOK], ctx], axis=0).T)} for i in range(NCORE)]
    r = _run(_prog("fused", [], [], True, fused=True), [xin, common, percore, L[0], L[1]])
    out = np.concatenate([np.asarray(q["outT"]).T for q in r], axis=0)
    return out[None].astype(np.float32)
```

```python
import math
from contextlib import ExitStack

import numpy as np
import ml_dtypes
import concourse.bass as bass
import concourse.mybir as mybir
from concourse.bass_utils import run_bass_kernel_spmd

F32 = mybir.dt.float32
BF16 = mybir.dt.bfloat16
AF = mybir.ActivationFunctionType
ALU = mybir.AluOpType
AX = mybir.AxisListType

D = 1024
SEQ = 16384
NCORE = 8
TOK = SEQ // NCORE
CTX = 256
NTT = TOK + CTX
GW = 64
ROWS = TOK // GW
EPS = 1e-6
NEG = -30000.0
SC64 = 64 ** -0.5
SC32 = 32 ** -0.5
NWIN = ROWS + 14
SWT = TOK + 256


class Tk:
    __slots__ = ("w", "r")

    def __init__(self):
        self.w = None
        self.r = {}


class Sched:
    ENG = ("pe", "act", "dve", "pool", "sp")

    def __init__(self, ndma=48, same_engine_sync=True):
        self.ops = {e: [] for e in self.ENG}
        self.cnt = {e: 0 for e in self.ENG}
        self.seen = {e: {} for e in self.ENG}
        self.ndma = ndma
        self.dma_cnt = [0] * ndma
        self.dma_next = 0
        self.dma_next_sw = 0
        self.same = same_engine_sync
        self.ncc = 0

    def cc(self, fn, eng="pool"):
        idx = self.ncc
        self.ncc += 1
        self.ops[eng].append(("cc", fn, idx))

    def _deps(self, reads, writes):
        deps = {}

        def add(v):
            if v is None:
                return
            key, val = v
            if deps.get(key, 0) < val:
                deps[key] = val
        for t in reads:
            add(t.w)
        for t in writes:
            add(t.w)
            for k, v in t.r.items():
                add((k, v))
        return deps

    def _wait(self, eng, deps):
        for key, val in deps.items():
            if key == eng and (eng == "pe" or not self.same):
                continue
            if self.seen[eng].get(key, 0) >= val:
                continue
            self.seen[eng][key] = val
            self.ops[eng].append(("wait", key, val))

    def op(self, eng, fn, reads=(), writes=(), sig=True):
        self._wait(eng, self._deps(reads, writes))
        if sig:
            self.cnt[eng] += 1
            n = self.cnt[eng]
            self.ops[eng].append(("op", fn))
        else:
            n = self.cnt[eng] + 1
            self.ops[eng].append(("opq", fn))
        for t in reads:
            t.r[eng] = n
        for t in writes:
            t.w = (eng, n)
            t.r = {}

    def dma(self, eng, out_ap, in_ap, reads=(), writes=()):
        deps = self._deps(reads, writes)
        half = self.ndma // 2
        if eng == "pool":
            k = half + self.dma_next_sw
            self.dma_next_sw = (self.dma_next_sw + 1) % half
        else:
            k = self.dma_next
            self.dma_next = (k + 1) % half
        key = ("dma", k)
        prev = self.dma_cnt[k] * 16
        if prev and deps.get(key, 0) < prev:
            deps[key] = prev
        self._wait(eng, deps)
        self.dma_cnt[k] += 1
        val = self.dma_cnt[k] * 16
        self.ops[eng].append(("dma", out_ap, in_ap, k))
        for t in reads:
            t.r[key] = val
        for t in writes:
            t.w = (key, val)
            t.r = {}

    def barrier(self):
        deps = {}
        for k in range(self.ndma):
            if self.dma_cnt[k]:
                deps[("dma", k)] = self.dma_cnt[k] * 16
        for e in self.ENG:
            if self.cnt[e]:
                deps[e] = self.cnt[e]
        for i in range(self.ncc):
            deps[("cc", i)] = 1
        for e in self.ENG:
            d = {k: v for k, v in deps.items() if k != e}
            self._wait(e, d)

    def emit(self, block, sems, dsems, ccsems=()):
        engobj = {"pe": "tensor", "act": "scalar", "dve": "vector", "pool": "gpsimd", "sp": "sync"}

        def semof(key):
            if isinstance(key, tuple):
                return dsems[key[1]] if key[0] == "dma" else ccsems[key[1]]
            return sems[key]

        def make(ename):
            ops = self.ops[ename]
            mysem = sems[ename]

            def body(eng):
                for o in ops:
                    if o[0] == "wait":
                        eng.wait_ge(semof(o[1]), o[2])
                    elif o[0] == "op":
                        o[1](eng).then_inc(mysem, 1)
                    elif o[0] == "opq":
                        o[1](eng)
                    elif o[0] == "cc":
                        o[1](eng).then_inc(ccsems[o[2]])
                    else:
                        eng.dma_start(out=o[1], in_=o[2]).then_inc(dsems[o[3]], 16)
            return body
        for ename in self.ENG:
            if self.ops[ename]:
                getattr(block, engobj[ename])(make(ename))


class Ring:
    def __init__(self, items):
        self.items = items
        self.i = 0

    def next(self):
        it = self.items[self.i]
        self.i = (self.i + 1) % len(self.items)
        return it


class B:
    __slots__ = ("a", "k")

    def __init__(self, a, k=None):
        self.a = a
        self.k = k if k is not None else Tk()


class KB:
    def __init__(self, layers_a, layers_b, final, dbg=False, own_blocks=4, fused=False):
        self.nc = bass.Bass("TRN2", target_bir_lowering=False)
        self.S = Sched()
        self.es = ExitStack()
        self.dbg = dbg
        self.dumps = []
        self.own_blocks = own_blocks
        self.fused = fused
        self.kvb = {}
        self.ins = {}
        self.build(layers_a, layers_b, final)

    def din(self, name, shape, dt=F32):
        if name not in self.ins:
            self.ins[name] = self.nc.dram_tensor(name, list(shape), dt, kind="ExternalInput").ap()
        return self.ins[name]

    def dout(self, name, shape, dt=F32):
        return self.nc.dram_tensor(name, list(shape), dt, kind="ExternalOutput").ap()

    def sb(self, name, shape, dt):
        return self.es.enter_context(self.nc.sbuf_tensor(name, list(shape), dt))

    def arena_reset(self):
        self.S.barrier()
        self.aoff = 0

    def al(self, shape, dt):
        n = 1
        for s in shape[1:]:
            n *= s
        words = n if dt == F32 else (n + 1) // 2
        a = self.arena[0:shape[0], self.aoff:self.aoff + words]
        self.aoff += words
        assert self.aoff <= self.AW, ("arena overflow", self.aoff)
        if dt != F32:
            a = a.bitcast(dt)
        if len(shape) == 3:
            a = a.rearrange("p (a b) -> p a b", a=shape[1])
        elif len(shape) == 4:
            a = a.rearrange("p (a b c) -> p a b c", a=shape[1], b=shape[2])
        return a

    def alb(self, shape, dt):
        return B(self.al(shape, dt))

    def ring(self, n, shape, dt):
        return Ring([self.alb(shape, dt) for _ in range(n)])

    def MM(self, out, lhsT, rhs, start, stop, R, W, sig=True):
        self.S.op("pe", lambda e: e.matmul(out, lhsT=lhsT, rhs=rhs, start=start, stop=stop, skip_group_check=True),
                  reads=R, writes=W, sig=sig)

    def TR(self, out, in_, ident, R, W, sig=True):
        self.S.op("pe", lambda e: e.transpose(out, in_, ident), reads=R, writes=W, sig=sig)

    def ACT(self, out, in_, func, R, W, bias=None, scale=None, accum=None):
        kw = {}
        if bias is not None:
            kw["bias"] = bias
        if scale is not None:
            kw["scale"] = scale
        if accum is not None:
            kw["accum_out"] = accum
        self.S.op("act", lambda e: e.activation(out=out, in_=in_, func=func, **kw), reads=R, writes=W)

    def TT(self, eng, out, in0, in1, op, R, W):
        self.S.op(eng, lambda e: e.tensor_tensor(out=out, in0=in0, in1=in1, op=op), reads=R, writes=W)

    def TS(self, eng, out, in0, s1, s2, op0, op1, R, W):
        if op1 is None:
            self.S.op(eng, lambda e: e.tensor_scalar(out=out, in0=in0, scalar1=s1, scalar2=None, op0=op0),
                      reads=R, writes=W)
        else:
            self.S.op(eng, lambda e: e.tensor_scalar(out=out, in0=in0, scalar1=s1, scalar2=s2, op0=op0, op1=op1),
                      reads=R, writes=W)

    def STT(self, out, in0, scalar, in1, op0, op1, R, W):
        self.S.op("dve", lambda e: e.scalar_tensor_tensor(out=out, in0=in0, scalar=scalar, in1=in1, op0=op0, op1=op1),
                  reads=R, writes=W)

    def CP(self, eng, out, in_, R, W):
        if eng == "act":
            self.S.op("act", lambda e: e.activation(out=out, in_=in_, func=AF.Copy), reads=R, writes=W)
        else:
            self.S.op(eng, lambda e: e.tensor_copy(out=out, in_=in_), reads=R, writes=W)

    def RCP(self, out, in_, R, W):
        self.S.op("dve", lambda e: e.reciprocal(out=out, in_=in_), reads=R, writes=W)

    def MSET(self, eng, ap, val, W):
        self.S.op(eng, lambda e: e.memset(ap, val), writes=W)

    def DMA(self, q, out, in_, R=(), W=()):
        self.S.dma(q, out, in_, reads=R, writes=W)

    @staticmethod
    def sap(base, dims):
        return bass.AP(base.tensor, base.offset, [list(base.ap[0])] + [list(d) for d in dims])

    def MAX8(self, out, in_, R, W):
        self.S.op("dve", lambda e: e.max(out=out, in_=in_), reads=R, writes=W)

    def MREP(self, out, rep, vals, R, W):
        self.S.op("dve", lambda e: e.match_replace(out=out, in_to_replace=rep, in_values=vals, imm_value=-1e30),
                  reads=R, writes=W)

    def dump(self, name, ap, k, shape, dt=F32):
        if not self.dbg:
            return
        o = self.dout("dbg_" + name, shape, dt)
        self.DMA("sp", o, ap, R=[k], W=[Tk()])

    def build(self, layers_a, layers_b, final):
        nc = self.nc
        self.banks = [B(self.es.enter_context(nc.psum_tensor("ps%d" % i, [128, 512], F32))) for i in range(8)]
        self.RS = Ring(self.banks[0:4])
        self.RO = Ring(self.banks[4:6])
        self.RX = Ring(self.banks[6:8])
        self.RS6 = Ring(self.banks[0:4] + self.banks[6:8])
        self.xT = self.sb("xT", [128, 8, NTT], F32)
        self.xk = [Tk() for _ in range(5)]
        self.hT = B(self.sb("hT", [128, 8, 512], BF16))
        self.cst = self.sb("cst", [128, 8], F32)
        self.cstk = Tk()
        self.identf = B(self.sb("identf", [128, 128], F32))
        self.identb = B(self.sb("identb", [128, 128], BF16))
        self.perm64 = B(self.sb("perm64", [128, 128], BF16))
        self.perm32 = B(self.sb("perm32", [128, 128], BF16))
        self.blk64 = B(self.sb("blk64", [128, 128], BF16))
        self.onesb = B(self.sb("onesb", [128, 128], BF16))
        self.vec = self.sb("vec", [128, 512], F32)
        self.veck = Tk()
        self.mod = B(self.sb("mod", [128, 48, 2], F32))
        self.drv = B(self.sb("drv", [128, 6, 8, 2], F32))
        self.ctxK = B(self.sb("ctxK", [128, 6, CTX], BF16))
        self.ctxV = B(self.sb("ctxV", [128, 2, 16, 128], BF16))
        self.keysT = B(self.sb("keysT", [128, 16, 128], BF16))
        self.AW = 27648
        self.arena = self.sb("arena", [128, self.AW], F32)
        self.aoff = 0

        S = self.S
        self.MSET("dve", self.cst[:, 0:1], EPS, [self.cstk])
        self.MSET("dve", self.cst[:, 1:2], 0.0, [self.cstk])
        self.MSET("dve", self.cst[:, 2:3], 1.0, [self.cstk])
        self.MSET("dve", self.onesb.a[:, :], 1.0, [self.onesb.k])
        cmat = self.din("cmat", [4, 128, 128])
        self.DMA("sp", self.identf.a[:, :], cmat[0], W=[self.identf.k])
        self.DMA("pool", self.identb.a[:, :], cmat[0], W=[self.identb.k])
        self.DMA("pool", self.perm64.a[:, :], cmat[1], W=[self.perm64.k])
        self.DMA("pool", self.perm32.a[:, :], cmat[2], W=[self.perm32.k])
        self.DMA("pool", self.blk64.a[:, :], cmat[3], W=[self.blk64.k])
        self.MSET("pool", self.ctxV.a[:, :, :, :], 1.0, [self.ctxV.k])

        xin = self.din("xT_in", [D, NTT])
        xv = xin.rearrange("(c p) t -> p c t", p=128)
        for b in range(5):
            t0, nt = self.blk(b)
            self.DMA("sp", self.xT[:, :, t0:t0 + nt], xv[:, :, t0:t0 + nt], W=[self.xk[b]])

        if self.fused:
            self.sel = self.sb("sel_sb", [128, 16], F32)
            self.selk = Tk()
            self.DMA("sp", self.sel[:, :], self.din("sel", [128, 16]), W=[self.selk])
            for l in (0, 1):
                self.layer_setup(l)
                outs = self.kv_outs(l)
                for b in range(4):
                    self.arena_reset()
                    self.block_kv(l, b, outs)
                self.exchange(l)
                self.arena_reset()
                self.block_kv(l, 4, None, ctx_only=True)
                for b in list(range(4)) + ([4] if l == 0 else []):
                    self.block_full(l, b)
            layers_b = [1]
        else:
            for l in layers_b:
                self.layer_setup(l)
                self.arena_reset()
                self.block_kv(l, 4, None, ctx_only=True)
                blocks = list(range(self.own_blocks)) + ([4] if l == 0 else [])
                for b in blocks:
                    self.block_full(l, b)
            for l in layers_a:
                self.layer_setup(l)
                outs = self.kv_outs(l)
                for b in range(self.own_blocks):
                    self.arena_reset()
                    self.block_kv(l, b, outs)
        if final:
            self.final_norm()
        elif layers_b:
            xo = self.dout("xT_out", [D, NTT]).rearrange("(c p) t -> p c t", p=128)
            for b in range(5):
                t0, nt = self.blk(b)
                self.DMA("sp", xo[:, :, t0:t0 + nt], self.xT[:, :, t0:t0 + nt], R=[self.xk[b]], W=[Tk()])
        S.barrier()
        sems = {e: self.es.enter_context(nc.semaphore("s_" + e)) for e in S.ENG}
        dsems = [self.es.enter_context(nc.semaphore("d%d" % i)) for i in range(S.ndma)]
        ccsems = [self.es.enter_context(nc.semaphore("c%d" % i)) for i in range(S.ncc)]
        block = self.es.enter_context(nc.Block())
        S.emit(block, sems, dsems, ccsems)
        self.es.close()

    def blk(self, b):
        return (b * 512, 512) if b < 4 else (TOK, CTX)

    def layer_setup(self, l):
        sfx = "_l%d" % l
        self.arena_reset()
        vec_d = self.din("vec" + sfx, [128, 512])
        self.DMA("sp", self.vec[:, :], vec_d, W=[self.veck])
        v = self.vec
        vk = self.veck
        keys_d = self.din("keysT" + sfx, [128, 16, 128])
        self.DMA("pool", self.keysT.a[:, :, :], keys_d, W=[self.keysT.k])
        sv = self.alb([128, 8, 2], F32)
        self.ACT(sv.a[:, :, 0], v[:, 104:112], AF.Silu, [vk], [sv.k])
        self.ACT(sv.a[:, :, 1], v[:, 112:120], AF.Silu, [vk], [sv.k])
        adaw = self.din("ada_w" + sfx, [D, 6 * D]).rearrange("(k p) n -> p k n", p=128)
        wr = self.ring(3, [128, 8, 128], F32)
        for j in range(48):
            wt = wr.next()
            self.DMA("sp", wt.a[:, :, :], adaw[:, :, j * 128:(j + 1) * 128], W=[wt.k])
            ps = self.RS.next()
            for k in range(8):
                self.MM(ps.a[:, 0:2], wt.a[:, k, :], sv.a[:, k, :], k == 0, k == 7, [wt.k, sv.k], [ps.k], sig=(k == 7))
            self.TS("dve", self.mod.a[:, j, :], ps.a[:, 0:2], v[:, 16 + j:17 + j], None, ALU.add, None,
                    [ps.k, vk], [self.mod.k])
        m = self.mod
        d = self.drv
        n1 = v[:, 0:8].unsqueeze(2).to_broadcast([128, 8, 2])
        n2 = v[:, 8:16].unsqueeze(2).to_broadcast([128, 8, 2])
        self.STT(d.a[:, 0, :, :], m.a[:, 8:16, :], 1.0, n1, ALU.add, ALU.mult, [m.k, vk], [d.k])
        self.CP("dve", d.a[:, 1, :, :], m.a[:, 0:8, :], [m.k], [d.k])
        self.CP("dve", d.a[:, 2, :, :], m.a[:, 16:24, :], [m.k], [d.k])
        self.STT(d.a[:, 3, :, :], m.a[:, 32:40, :], 1.0, n2, ALU.add, ALU.mult, [m.k, vk], [d.k])
        self.CP("dve", d.a[:, 4, :, :], m.a[:, 24:32, :], [m.k], [d.k])
        self.CP("dve", d.a[:, 5, :, :], m.a[:, 40:48, :], [m.k], [d.k])
        pr = self.alb([128, 64], F32)
        self.TT("dve", pr.a[:, 0:32], v[:, 128:160], v[:, 160:192], ALU.mult, [vk], [pr.k])
        self.TT("dve", pr.a[:, 32:64], v[:, 192:224], v[:, 224:256], ALU.mult, [vk], [pr.k])
        sm = self.alb([128, 4], F32)
        self.S.op("dve", lambda e: e.tensor_reduce(out=sm.a[:, 0:2], in_=pr.a[:, :].rearrange("p (a b) -> p a b", a=2),
                                                   axis=AX.X, op=ALU.add), reads=[pr.k], writes=[sm.k])
        self.ACT(sm.a[:, 2:4], sm.a[:, 0:2], AF.Exp, [sm.k], [sm.k])
        self.lamk = Tk()
        self.TT("dve", v[:, 256:257], sm.a[:, 2:3], sm.a[:, 3:4], ALU.subtract, [sm.k, vk], [self.lamk])
        lam_init = 0.8 - 0.6 * math.exp(-0.3 * l)
        self.lam_init = lam_init
        self.TS("dve", v[:, 256:257], v[:, 256:257], lam_init, None, ALU.add, None, [self.lamk], [self.lamk])
        self.ACT(v[:, 260:264], v[:, 120:124], AF.Exp, [vk], [self.lamk])
        self.dump("mod%d" % l, self.mod.a[:, :, :], self.mod.k, [128, 48, 2])

    def dr(self, which, c, ctx):
        return self.drv.a[:, which, c, (1 if ctx else 0):(2 if ctx else 1)]

    def norm_mod(self, b, wa, wb, tf, tb):
        t0, nt = self.blk(b)
        ctx = (b == 4)
        ps = self.RS.next()
        for c in range(8):
            sq = tb.next()
            self.ACT(sq.a[:, :nt], self.xT[:, c, t0:t0 + nt], AF.Square, [self.xk[b]], [sq.k])
            self.MM(ps.a[:, :nt], self.onesb.a[:, :], sq.a[:, :nt], c == 0, c == 7, [self.onesb.k, sq.k], [ps.k])
        rs = self.alb([128, 512], F32)
        self.ACT(rs.a[:, :nt], ps.a[:, :nt], AF.Sqrt, [ps.k, self.cstk], [rs.k], bias=self.cst[:, 0:1], scale=1.0 / D)
        self.RCP(rs.a[:, :nt], rs.a[:, :nt], [rs.k], [rs.k])
        for c in range(8):
            t = tf.next()
            self.TT("dve", t.a[:, :nt], self.xT[:, c, t0:t0 + nt], rs.a[:, :nt], ALU.mult, [self.xk[b], rs.k], [t.k])
            self.TS("pool", self.hT.a[:, c, :nt], t.a[:, :nt], self.dr(wa, c, ctx), self.dr(wb, c, ctx),
                    ALU.mult, ALU.add, [t.k, self.drv.k], [self.hT.k])

    def proj(self, wview, col0, nt, wring, ring=None):
        wt = wring.next()
        self.DMA("pool", wt.a[:, :, :], wview[:, :, col0:col0 + 128], W=[wt.k])
        ps = (ring or self.RX).next()
        for k in range(8):
            self.MM(ps.a[:, :nt], wt.a[:, k, :], self.hT.a[:, k, :nt], k == 0, k == 7, [wt.k, self.hT.k], [ps.k],
                    sig=(k == 7))
        return ps

    def rope(self, src, dst_ap, dst_k, nt, cos, sin, perm, tf):
        ps = self.RX.next()
        self.MM(ps.a[:, :nt], perm.a[:, :], src.a[:, :nt], True, True, [perm.k, src.k], [ps.k])
        t1 = tf.next()
        t2 = tf.next()
        self.TT("dve", t1.a[:, :nt], src.a[:, :nt], cos.a[:, :nt], ALU.mult, [src.k, cos.k], [t1.k])
        self.TT("dve", t2.a[:, :nt], ps.a[:, :nt], sin.a[:, :nt], ALU.mult, [ps.k, sin.k], [t2.k])
        self.TT("pool", dst_ap, t1.a[:, :nt], t2.a[:, :nt], ALU.add, [t1.k, t2.k], [dst_k])

    def qknorm(self, ps, nt, wcol, tf, tb, out):
        sq = tb.next()
        self.ACT(sq.a[:, :nt], ps.a[:, :nt], AF.Square, [ps.k], [sq.k])
        p2 = self.RX.next()
        self.MM(p2.a[:, :nt], self.blk64.a[:, :], sq.a[:, :nt], True, True, [self.blk64.k, sq.k], [p2.k])
        rs = tf.next()
        self.ACT(rs.a[:, :nt], p2.a[:, :nt], AF.Sqrt, [p2.k, self.cstk], [rs.k], bias=self.cst[:, 0:1], scale=1.0 / 64)
        self.RCP(rs.a[:, :nt], rs.a[:, :nt], [rs.k], [rs.k])
        t = tf.next()
        self.TT("dve", t.a[:, :nt], ps.a[:, :nt], rs.a[:, :nt], ALU.mult, [ps.k, rs.k], [t.k])
        self.TS("pool", out.a[:, :nt], t.a[:, :nt], self.vec[:, wcol:wcol + 1], None, ALU.mult, None,
                [t.k, self.veck], [out.k])

    def load_rope(self, b):
        if b == 4:
            return None
        t0, nt = self.blk(b)
        rt = self.din("ropeT", [4, 128, TOK])
        tabs = []
        for i in range(4):
            t = self.alb([128, 512], F32)
            self.DMA("sp", t.a[:, :], rt[i][:, t0:t0 + nt], W=[t.k])
            tabs.append(t)
        return tabs

    def dram(self, name, shape, dt):
        return self.nc.dram_tensor(name, list(shape), dt).ap()

    def kv_outs(self, l):
        sfx = "_l%d" % l
        if self.fused:
            d = {}
            d["s_kT"] = self.dram("s_kT" + sfx, [6 * 128, TOK], BF16)
            d["s_vd"] = self.dram("s_vd" + sfx, [8 * 128, 16 * 128], BF16)
            d["s_nv"] = self.dram("s_nv" + sfx, [4 * 64, ROWS * 128], BF16)
            d["s_sv"] = self.dram("s_sv" + sfx, [4 * 128, 16 * 128], BF16)
            d["g_kT"] = self.dram("g_kT" + sfx, [8 * 6 * 128, TOK], BF16)
            d["g_vd"] = self.dram("g_vd" + sfx, [8 * 8 * 128, 16 * 128], BF16)
            d["g_nv"] = self.dram("g_nv" + sfx, [8 * 4 * 64, ROWS * 128], BF16)
            d["g_sv"] = self.dram("g_sv" + sfx, [8 * 4 * 128, 16 * 128], BF16)
            d["naKwin"] = self.dram("naKwin" + sfx, [2, 128, NWIN * 64], BF16)
            d["naVwin"] = self.dram("naVwin" + sfx, [4, 64, NWIN, 128], BF16)
            d["swKwin"] = self.dram("swKwin" + sfx, [128, SWT], BF16)
            d["swVwin"] = self.dram("swVwin" + sfx, [4, 128, 18, 128], BF16)
            self.kvb[l] = d
            return dict(
                kT=d["s_kT"].rearrange("(c p) t -> c p t", p=128),
                vd=d["s_vd"].rearrange("(s p) (k c) -> s p k c", p=128, c=128),
                nv=d["s_nv"].rearrange("(h k) (r c) -> h k r c", k=64, c=128),
                sv=d["s_sv"].rearrange("(s p) (k c) -> s p k c", p=128, c=128),
            )
        return dict(
            kT=self.dout("kT_own" + sfx, [6, 128, TOK], BF16),
            vd=self.dout("VAd_own" + sfx, [8, 128, 16, 128], BF16),
            nv=self.dout("NV_own" + sfx, [4, 64, ROWS, 128], BF16),
            sv=self.dout("SV_own" + sfx, [4, 128, 16, 128], BF16),
        )

    def exchange(self, l):
        d = self.kvb[l]
        S = self.S
        S.barrier()
        for a, g in (("s_kT", "g_kT"), ("s_vd", "g_vd"), ("s_nv", "g_nv"), ("s_sv", "g_sv")):
            src, dst = d[a], d[g]
            S.cc((lambda src=src, dst=dst: lambda e: e.collective_compute(
                "AllGather", ALU.bypass, replica_groups=[list(range(NCORE))], ins=[src], outs=[dst]))())
        S.barrier()
        self.aoff = 0
        skT = d["s_kT"].rearrange("(c p) t -> c p t", p=128)
        snv = d["s_nv"].rearrange("(h k) (r c) -> h k r c", k=64, c=128)
        ssv = d["s_sv"].rearrange("(s p) (k c) -> s p k c", p=128, c=128)
        for c in range(2):
            self.DMA("sp", d["naKwin"][c][:, 448:448 + TOK], skT[c], W=[Tk()])
        for h in range(4):
            self.DMA("sp", d["naVwin"][h][:, 7:7 + ROWS, :], snv[h], W=[Tk()])
            self.DMA("sp", d["swVwin"][h][:, 1:17, :], ssv[h], W=[Tk()])
        self.DMA("sp", d["swKwin"][:, 128:128 + TOK], skT[4], W=[Tk()])
        gk = d["g_kT"].rearrange("(j c p) t -> p j c t", c=6, p=128)
        gn = d["g_nv"].rearrange("(j h k) (r c) -> k j h r c", h=4, k=64, c=128)
        gs = d["g_sv"].rearrange("(j s p) (t c) -> p j s t c", s=4, p=128, c=128)
        cring = self.ring(3, [128, 8, 896], BF16)
        aring = self.ring(2, [128, 896], F32)
        oring = self.ring(3, [128, 896], BF16)

        def select(cand, P, n, side, dst):
            cb = cring.next()
            self.DMA("sp", cb.a[0:P, :, 0:n], cand, W=[cb.k])
            acc = aring.next()
            self.TS("dve", acc.a[0:P, 0:n], cb.a[0:P, 0, 0:n], self.sel[0:P, side * 8:side * 8 + 1], None, ALU.mult, None,
                    [cb.k, self.selk], [acc.k])
            ob = oring.next()
            for j in range(1, 8):
                out = ob.a[0:P, 0:n] if j == 7 else acc.a[0:P, 0:n]
                self.STT(out, cb.a[0:P, j, 0:n], self.sel[0:P, side * 8 + j:side * 8 + j + 1], acc.a[0:P, 0:n],
                         ALU.mult, ALU.add, [cb.k, self.selk, acc.k], [ob.k if j == 7 else acc.k])
            self.DMA("sp", dst, ob.a[0:P, 0:n], R=[ob.k], W=[Tk()])
        for c in range(2):
            select(gk[:, :, c, TOK - 448:TOK], 128, 448, 0, d["naKwin"][c][:, 0:448])
            select(gk[:, :, c, 0:448], 128, 448, 1, d["naKwin"][c][:, 448 + TOK:448 + TOK + 448])
        select(gk[:, :, 4, TOK - 128:TOK], 128, 128, 0, d["swKwin"][:, 0:128])
        select(gk[:, :, 4, 0:128], 128, 128, 1, d["swKwin"][:, 128 + TOK:256 + TOK])
        for h in range(4):
            select(gn[:, :, h, ROWS - 7:ROWS, :].rearrange("k j r c -> k j (r c)"), 64, 896, 0,
                   d["naVwin"][h][:, 0:7, :].rearrange("k r c -> k (r c)"))
            select(gn[:, :, h, 0:7, :].rearrange("k j r c -> k j (r c)"), 64, 896, 1,
                   d["naVwin"][h][:, 7 + ROWS:14 + ROWS, :].rearrange("k r c -> k (r c)"))
            select(gs[:, :, h, 15, :], 128, 128, 0, d["swVwin"][h][:, 0, :])
            select(gs[:, :, h, 0, :], 128, 128, 1, d["swVwin"][h][:, 17, :])
        S.barrier()

    def block_kv(self, l, b, outs, ctx_only=False):
        sfx = "_l%d" % l
        t0, nt = self.blk(b)
        ctx = (b == 4)
        tf = self.ring(4, [128, 512], F32)
        tb = self.ring(3, [128, 512], BF16)
        wring = self.ring(3, [128, 8, 128], BF16)
        tabs = self.load_rope(b)
        self.norm_mod(b, 0, 1, tf, tb)
        win = self.din("w_in" + sfx, [D, 2560]).rearrange("(k p) n -> p k n", p=128)
        specs = [(256, None, False), (384, None, False), (1024, 32, False), (1152, 32, False),
                 (1792, 64, False), (2304, 64, True)]
        kst = self.ring(2, [128, 512], BF16)
        for ci, (col0, rk, nrm) in enumerate(specs):
            ps = self.proj(win, col0, nt, wring)
            if ctx:
                dst_ap, dst_k = self.ctxK.a[:, ci, :], self.ctxK.k
            else:
                st = kst.next()
                dst_ap, dst_k = st.a[:, :nt], st.k
            if nrm:
                xn = tb.next()
                self.qknorm(ps, nt, 125, tf, tb, xn)
                if ctx:
                    self.CP("act", dst_ap, xn.a[:, :nt], [xn.k], [dst_k])
                else:
                    self.rope(xn, dst_ap, dst_k, nt, tabs[0], tabs[1], self.perm64, tf)
            elif rk is None or ctx:
                self.CP("act", dst_ap, ps.a[:, :nt], [ps.k], [dst_k])
            else:
                xb = tb.next()
                self.CP("act", xb.a[:, :nt], ps.a[:, :nt], [ps.k], [xb.k])
                if rk == 64:
                    self.rope(xb, dst_ap, dst_k, nt, tabs[0], tabs[1], self.perm64, tf)
                else:
                    self.rope(xb, dst_ap, dst_k, nt, tabs[2], tabs[3], self.perm32, tf)
            if not ctx:
                self.DMA("sp", outs["kT"][ci][:, t0:t0 + nt], dst_ap, R=[dst_k], W=[Tk()])
        wv = self.din("w_v" + sfx, [D, 768]).rearrange("(k p) n -> p k n", p=128)
        wvt = self.alb([128, 8, 768], BF16)
        self.DMA("pool", wvt.a[:, :, :], wv, W=[wvt.k])
        if not ctx:
            vs = self.alb([128, 4, 16, 128], BF16)
            self.MSET("pool", vs.a[:, :, :, :], 1.0, [vs.k])
        for tt in range(nt // 128):
            pa = self.RX.next()
            pb = self.RX.next()
            for k in range(8):
                self.MM(pa.a[:, :512], self.hT.a[:, k, tt * 128:(tt + 1) * 128], wvt.a[:, k, 0:512], k == 0, k == 7,
                        [self.hT.k, wvt.k], [pa.k], sig=(k == 7))
            for k in range(8):
                self.MM(pb.a[:, :256], self.hT.a[:, k, tt * 128:(tt + 1) * 128], wvt.a[:, k, 512:768], k == 0, k == 7,
                        [self.hT.k, wvt.k], [pb.k], sig=(k == 7))
            if ctx:
                dst, dk = self.ctxV.a[:, tt, :, :], self.ctxV.k
            else:
                dst, dk = vs.a[:, tt, :, :], vs.k
            def hv(base_col, par, pa=pa):
                c0 = base_col + par * 64
                return self.sap(pa.a[:, c0:c0 + 1], [[128, 2], [1, 64]])
            def dv(slot0, par, dst=dst):
                return self.sap(dst[:, slot0 + par, par * 64:par * 64 + 1], [[256, 2], [1, 64]])
            self.CP("act", dv(8, 0), hv(0, 0), [pa.k], [dk])
            self.CP("dve", dv(8, 1), hv(0, 1), [pa.k], [dk])
            self.CP("act", dv(0, 0), hv(256, 0), [pa.k], [dk])
            self.CP("dve", dv(0, 1), hv(256, 1), [pa.k], [dk])
            def gsrc(base, pb=pb):
                return pb.a[:, base:base + 128].rearrange("p (g d) -> p g d", g=2)
            def gdst(slot0, par, dst=dst):
                return self.sap(dst[:, slot0 + par, par * 64:par * 64 + 1], [[256, 2], [1, 64]])
            self.CP("act", gdst(12, 0), gsrc(0), [pb.k], [dk])
            self.CP("dve", gdst(12, 1), gsrc(0), [pb.k], [dk])
            self.CP("act", gdst(4, 0), gsrc(128), [pb.k], [dk])
            self.CP("dve", gdst(4, 1), gsrc(128), [pb.k], [dk])
        if not ctx:
            kt0 = b * 4
            for s in range(8):
                self.DMA("sp", outs["vd"][s][:, kt0:kt0 + 4, :], vs.a[:, :, s, :], R=[vs.k], W=[Tk()])
            for s in range(4):
                self.DMA("sp", outs["sv"][s][:, kt0:kt0 + 4, :], vs.a[:, :, 12 + s, :], R=[vs.k], W=[Tk()])
                nvv = outs["nv"][s][:, 2 * kt0:2 * kt0 + 8, :].rearrange("k (t two) c -> two k t c", two=2)
                self.DMA("sp", nvv[0], vs.a[0:64, :, 8 + s, :], R=[vs.k], W=[Tk()])
                self.DMA("sp", nvv[1], vs.a[64:128, :, 8 + s, :], R=[vs.k], W=[Tk()])

    def finalize(self, O, par, nq, dst_ap, dst_k, tf, extra=None, mul=None):
        no = par * 64
        zo = (1 - par) * 64
        rz = tf.next()
        if extra is not None:
            self.TS("dve", rz.a[zo:zo + 64, :nq], O.a[zo:zo + 64, :nq], extra[zo:zo + 64, :], None, ALU.add, None,
                    [O.k, self.lamk], [rz.k])
            self.RCP(rz.a[zo:zo + 64, :nq], rz.a[zo:zo + 64, :nq], [rz.k], [rz.k])
        else:
            self.RCP(rz.a[zo:zo + 64, :nq], O.a[zo:zo + 64, :nq], [O.k], [rz.k])
        if mul is not None:
            self.TS("dve", rz.a[zo:zo + 64, :nq], rz.a[zo:zo + 64, :nq], mul[zo:zo + 64, :], None, ALU.mult, None,
                    [rz.k, self.lamk], [rz.k])
        self.TT("dve", dst_ap, O.a[no:no + 64, :nq], rz.a[zo:zo + 64, :nq], ALU.mult, [O.k, rz.k], [dst_k])

    def ctx_tiles(self, streams, O, nq, pt, scale, stop_last):
        for kt in range(2):
            for si, st in enumerate(streams):
                lo, hi = st["krows"]
                s = self.RS.next()
                self.MM(s.a[:, :nq], self.ctxK.a[lo:hi, st["ci"], kt * 128:(kt + 1) * 128], st["q"], True, True,
                        [self.ctxK.k, st["qk"]], [s.k])
                p = pt.next()
                self.ACT(p.a[:, :nq], s.a[:, :nq], AF.Exp, [s.k], [p.k], scale=scale)
                self.MM(O[si].a[:, :nq], self.ctxV.a[:, kt, st["cslot"], :], p.a[:, :nq], kt == 0,
                        stop_last and kt == 1, [self.ctxV.k, p.k], [O[si].k])

    def dense_pass(self, l, streams, nq, ci_d, vslots, pt, kring, vrings, scale):
        sfx = "_l%d" % l
        O = [self.RO.next() for _ in streams]
        self.ctx_tiles(streams, O, nq, pt, scale, False)
        NP = SEQ // 2048
        if self.fused:
            gk = self.kvb[l]["g_kT"].rearrange("(j c p) t -> j c p t", c=6, p=128)
            gv = self.kvb[l]["g_vd"].rearrange("(j s p) (k c) -> j s p k c", s=8, p=128, c=128)
            ksrc = lambda pc: gk[pc][(2, 3, 5)[ci_d]]
            vsrc = lambda vs, pc: gv[pc][vs]
        else:
            ktd = self.din("KTd" + sfx, [3, 128, SEQ], BF16)
            vad = self.din("VAd" + sfx, [8, 128, SEQ // 128, 128], BF16)
            ksrc = lambda pc: ktd[ci_d][:, pc * 2048:(pc + 1) * 2048]
            vsrc = lambda vs, pc: vad[vs][:, pc * 16:(pc + 1) * 16, :]

        def load(pc):
            kp = kring.next()
            self.DMA("sp", kp.a[:, :], ksrc(pc), W=[kp.k])
            vps = []
            for vi, vs in enumerate(vslots):
                vp = vrings[vi].next()
                self.DMA("sp", vp.a[:, :, :], vsrc(vs, pc), W=[vp.k])
                vps.append(vp)
            return kp, vps
        nxt = load(0)
        pend = []

        def flush():
            for (si, p, vt, stop) in pend:
                self.MM(O[si].a[:, :nq], vt[0], p.a[:, :nq], False, stop, [vt[1], p.k], [O[si].k])
            del pend[:]
        for pc in range(NP):
            kp, vps = nxt
            if pc + 1 < NP:
                flush()
                nxt = load(pc + 1)
            for kt in range(16):
                cur = []
                for si, st in enumerate(streams):
                    lo, hi = st["krows"]
                    s = self.RS6.next()
                    self.MM(s.a[:, :nq], kp.a[lo:hi, kt * 128:(kt + 1) * 128], st["q"], True, True, [kp.k, st["qk"]], [s.k])
                    p = pt.next()
                    self.ACT(p.a[:, :nq], s.a[:, :nq], AF.Exp, [s.k], [p.k], scale=scale)
                    vp = vps[st["vi"]]
                    cur.append((si, p, (vp.a[:, kt, :], vp.k), (pc == NP - 1 and kt == 15)))
                flush()
                pend.extend(cur)
        flush()
        return O

    def block_full(self, l, b):
        sfx = "_l%d" % l
        t0, nt = self.blk(b)
        ctx = (b == 4)
        v = self.vec
        self.arena_reset()
        qT = self.alb([128, 8, 512], BF16)
        ysT = self.alb([128, 8, 512], BF16)
        ysd = self.alb([128, 2, 512], F32)
        qz = self.alb([128, 2, 512], BF16)
        qpad = self.alb([128, 12, 512], BF16)
        keep = self.aoff
        tf = self.ring(4, [128, 512], F32)
        tb = self.ring(3, [128, 512], BF16)
        wring = self.ring(3, [128, 8, 128], BF16)
        tabs = self.load_rope(b)
        self.norm_mod(b, 0, 1, tf, tb)
        if self.dbg and b == 0:
            self.dump("h%d" % l, self.hT.a[:, :, :], self.hT.k, [128, 8, 512], BF16)
        win = self.din("w_in" + sfx, [D, 2560]).rearrange("(k p) n -> p k n", p=128)
        qspecs = [(0, None, False), (128, None, False), (768, 32, False), (896, 32, False),
                  (1536, 64, False), (1664, 64, False), (2048, 64, True), (2176, 64, True)]
        for qi, (col0, rk, nrm) in enumerate(qspecs):
            ps = self.proj(win, col0, nt, wring)
            dst_ap, dst_k = qT.a[:, qi, :nt], qT.k
            if nrm:
                xn = tb.next()
                self.qknorm(ps, nt, 124, tf, tb, xn)
                if ctx:
                    self.CP("act", dst_ap, xn.a[:, :nt], [xn.k], [dst_k])
                else:
                    self.rope(xn, dst_ap, dst_k, nt, tabs[0], tabs[1], self.perm64, tf)
            elif rk is None or ctx:
                self.CP("act", dst_ap, ps.a[:, :nt], [ps.k], [dst_k])
            else:
                xb = tb.next()
                self.CP("act", xb.a[:, :nt], ps.a[:, :nt], [ps.k], [xb.k])
                if rk == 64:
                    self.rope(xb, dst_ap, dst_k, nt, tabs[0], tabs[1], self.perm64, tf)
                else:
                    self.rope(xb, dst_ap, dst_k, nt, tabs[2], tabs[3], self.perm32, tf)
        for c in range(2):
            self.CP("dve", qz.a[64:128, c, :nt], qT.a[64:128, 2 + c, :nt], [qT.k], [qz.k])
            self.MSET("dve", qz.a[64:96, c, :nt], 0.0, [qz.k])
        self.MSET("pool", qpad.a[:, :, :], 0.0, [qpad.k])
        for c in range(2):
            for par in range(2):
                for m in range(2):
                    lo = par * 64 + m * 32
                    idx = c * 4 + par * 2 + m
                    if lo == 96:
                        self.CP("dve", qpad.a[64:128, idx, :nt], qz.a[64:128, c, :nt], [qz.k, qpad.k], [qpad.k])
                    else:
                        self.CP("dve", qpad.a[lo:lo + 32, idx, :nt], qT.a[lo:lo + 32, 2 + c, :nt], [qT.k, qpad.k], [qpad.k])
        for g in range(2):
            for par in range(2):
                self.CP("act", qpad.a[g * 64:g * 64 + 64, 8 + g * 2 + par, :nt], qT.a[g * 64:g * 64 + 64, 6 + par, :nt],
                        [qT.k, qpad.k], [qpad.k])
        if self.dbg and b == 0:
            self.dump("q%d" % l, qT.a[:, :, :], qT.k, [128, 8, 512], BF16)

        self.S.barrier()
        self.aoff = keep
        tf = self.ring(4, [128, 512], F32)
        pt = self.ring(4, [128, 512], BF16)
        nq = nt
        if not ctx:
            lr0 = 8 * b
            nak = self.alb([128, 2, 22 * 64], BF16)
            nakw = self.kvb[l]["naKwin"] if self.fused else self.din("naKwin" + sfx, [2, 128, NWIN * 64], BF16)
            for c in range(2):
                self.DMA("sp", nak.a[:, c, :], nakw[c][:, lr0 * 64:(lr0 + 22) * 64], W=[nak.k])
            navr = self.ring(2, [64, 22, 128], BF16)
            rpr = self.ring(2, [64, 15, 64], BF16)
            navw = self.kvb[l]["naVwin"] if self.fused else self.din("naVwin" + sfx, [4, 64, NWIN, 128], BF16)
            rpd = self.din("rpbr" + sfx, [4, 64, 15, 64])
            bn = self.alb([64, 512], F32)
            self.DMA("sp", bn.a[:, :], self.din("bandneg", [64, 512]), W=[bn.k])
        for h in range(4):
            c = h // 2
            po = (h % 2) * 64
            par = h % 2
            st = dict(krows=(po, po + 64), ci=c, q=qT.a[po:po + 64, c, :nq], qk=qT.k, cslot=8 + h)
            O = [self.RO.next()]
            self.ctx_tiles([st], O, nq, pt, SC64, ctx)
            if not ctx:
                nav = navr.next()
                self.DMA("sp", nav.a[:, :, :], navw[h][:, lr0:lr0 + 22, :], W=[nav.k])
                rp = rpr.next()
                self.DMA("pool", rp.a[:, :, :], rpd[h], W=[rp.k])
                for wr in range(22):
                    kr = lr0 - 7 + wr
                    r_lo = max(lr0, kr - 7)
                    r_hi = min(lr0 + 7, kr + 7)
                    nr = r_hi - r_lo + 1
                    n = nr * 64
                    qoff = (r_lo - lr0) * 64
                    e_lo = r_lo - kr + 7
                    s = self.RS.next()
                    self.MM(s.a[0:64, :n], nak.a[po:po + 64, c, wr * 64:(wr + 1) * 64], qT.a[po:po + 64, c, qoff:qoff + n],
                            True, True, [nak.k, qT.k], [s.k])
                    t = tf.next()
                    self.STT(t.a[0:64, :n].rearrange("p (r q) -> p r q", r=nr), s.a[0:64, :n].rearrange("p (r q) -> p r q", r=nr),
                             SC64, rp.a[:, e_lo:e_lo + nr, :], ALU.mult, ALU.add, [s.k, rp.k], [t.k])
                    bo = 17 * r_lo + 7 - kr
                    bnap = self.sap(bn.a[:, bo:bo + 1], [[17, nr], [0, 64]])
                    self.TT("pool", t.a[0:64, :n].rearrange("p (r q) -> p r q", r=nr),
                            t.a[0:64, :n].rearrange("p (r q) -> p r q", r=nr), bnap, ALU.add, [t.k, bn.k], [t.k])
                    p = pt.next()
                    self.ACT(p.a[0:64, :n], t.a[0:64, :n], AF.Exp, [t.k], [p.k])
                    self.MM(O[0].a[:, qoff:qoff + n], nav.a[:, wr, :], p.a[0:64, :n], False, wr == 21,
                            [nav.k, p.k], [O[0].k])
            self.finalize(O[0], par, nq, ysT.a[par * 64:par * 64 + 64, c, :nq], ysT.k, tf)

        self.S.barrier()
        self.aoff = keep
        tf = self.ring(4, [128, 512], F32)
        pt = self.ring(4, [128, 512], BF16)
        if not ctx:
            swk = self.alb([128, 768], BF16)
            swkw = self.kvb[l]["swKwin"] if self.fused else self.din("swKwin" + sfx, [128, SWT], BF16)
            self.DMA("sp", swk.a[:, :], swkw[:, t0:t0 + 768], W=[swk.k])
            swv = self.alb([128, 4, 6, 128], BF16)
            swvw = self.kvb[l]["swVwin"] if self.fused else self.din("swVwin" + sfx, [4, 128, 18, 128], BF16)
            for s4 in range(4):
                self.DMA("sp", swv.a[:, s4, :, :], swvw[s4][:, 4 * b:4 * b + 6, :], W=[swv.k])
            swm = self.alb([128, 6, 512], BF16)
            swmd = self.din("swm", [8, 128, 512])
            for j in range(6):
                mi = j
                if b == 0 and j == 0:
                    mi = 6
                if b == 3 and j == 5:
                    mi = 7
                self.DMA("pool", swm.a[:, j, :], swmd[mi], W=[swm.k])
        for h in range(4):
            g = h // 2
            par = h % 2
            qc = 4 + par
            st = dict(krows=(g * 64, g * 64 + 64), ci=4, q=qT.a[g * 64:g * 64 + 64, qc, :nq], qk=qT.k, cslot=12 + 2 * g + par)
            O = [self.RO.next()]
            self.ctx_tiles([st], O, nq, pt, SC64, ctx)
            if not ctx:
                for j in range(6):
                    s = self.RS.next()
                    self.MM(s.a[:, :nq], swk.a[g * 64:g * 64 + 64, j * 128:(j + 1) * 128], st["q"], True, True,
                            [swk.k, qT.k], [s.k])
                    p = pt.next()
                    self.ACT(p.a[:, :nq], s.a[:, :nq], AF.Exp, [s.k], [p.k], scale=SC64)
                    p2 = pt.next()
                    self.TT("pool", p2.a[:, :nq], p.a[:, :nq], swm.a[:, j, :nq], ALU.mult, [p.k, swm.k], [p2.k])
                    self.MM(O[0].a[:, :nq], swv.a[:, 2 * g + par, j, :], p2.a[:, :nq], False, j == 5, [swv.k, p2.k], [O[0].k])
            self.finalize(O[0], par, nq, ysT.a[par * 64:par * 64 + 64, 4 + g, :nq], ysT.k, tf,
                          extra=v[:, 260 + h:261 + h])

        self.S.barrier()
        self.aoff = keep
        tf = self.ring(4, [128, 512], F32)
        tb = self.ring(2, [128, 512], BF16)
        pt = self.ring(8, [128, 512], BF16)
        if not ctx:
            kring = self.ring(2, [128, 2048], BF16)
            vrings = [self.ring(2, [128, 16, 128], BF16), self.ring(2, [128, 16, 128], BF16)]
        for h in range(4):
            c = h // 2
            par = h % 2
            sts = []
            for m in range(2):
                sts.append(dict(krows=(0, 128), ci=2 + c, q=qpad.a[:, c * 4 + par * 2 + m, :nq], qk=qpad.k, cslot=h, vi=0))
            if ctx:
                O = [self.RO.next(), self.RO.next()]
                self.ctx_tiles(sts, O, nq, pt, SC32, True)
            else:
                O = self.dense_pass(l, sts, nq, c, [h], pt, kring, vrings, SC32)
            a0 = tf.next()
            a1 = tf.next()
            ro = par * 64
            self.finalize(O[0], par, nq, a0.a[ro:ro + 64, :nq], a0.k, tf)
            self.finalize(O[1], par, nq, a1.a[ro:ro + 64, :nq], a1.k, tf, mul=v[:, 256:257])
            self.TT("dve", ysd.a[ro:ro + 64, c, :nq], a0.a[ro:ro + 64, :nq], a1.a[ro:ro + 64, :nq], ALU.subtract,
                    [a0.k, a1.k], [ysd.k])
            if par == 1:
                sq = tb.next()
                self.ACT(sq.a[:, :nq], ysd.a[:, c, :nq], AF.Square, [ysd.k], [sq.k])
                p2 = self.RX.next()
                self.MM(p2.a[:, :nq], self.blk64.a[:, :], sq.a[:, :nq], True, True, [self.blk64.k, sq.k], [p2.k])
                rs = tf.next()
                self.ACT(rs.a[:, :nq], p2.a[:, :nq], AF.Sqrt, [p2.k, self.cstk], [rs.k], bias=self.cst[:, 0:1], scale=1.0 / 64)
                self.RCP(rs.a[:, :nq], rs.a[:, :nq], [rs.k], [rs.k])
                t = tf.next()
                self.TT("dve", t.a[:, :nq], ysd.a[:, c, :nq], rs.a[:, :nq], ALU.mult, [ysd.k, rs.k], [t.k])
                self.TS("pool", ysT.a[:, 2 + c, :nq], t.a[:, :nq], v[:, 126:127], 1.0 - self.lam_init, ALU.mult, ALU.mult,
                        [t.k, self.veck], [ysT.k])

        for g in range(2):
            sts = []
            for par in range(2):
                sts.append(dict(krows=(0, 128), ci=5, q=qpad.a[:, 8 + g * 2 + par, :nq], qk=qpad.k,
                                cslot=4 + 2 * g + par, vi=par))
            if ctx:
                O = [self.RO.next(), self.RO.next()]
                self.ctx_tiles(sts, O, nq, pt, SC64, True)
            else:
                O = self.dense_pass(l, sts, nq, 2, [4 + 2 * g, 5 + 2 * g], pt, kring, vrings, SC64)
            for par in range(2):
                self.finalize(O[par], par, nq, ysT.a[par * 64:par * 64 + 64, 6 + g, :nq], ysT.k, tf)
        if self.dbg and b in (0, 4):
            self.dump("ys%d_%d" % (l, b), ysT.a[:, :, :], ysT.k, [128, 8, 512], BF16)

        self.S.barrier()
        self.aoff = keep
        tf = self.ring(3, [128, 512], F32)
        wring = self.ring(3, [128, 8, 128], BF16)
        wbr = self.alb([128, 8, 1024], BF16)
        self.DMA("pool", wbr.a[:, :, :], self.din("w_branch" + sfx, [4, 256, D]).rearrange("n (w p) d -> p (n w) d", p=128),
                 W=[wbr.k])
        G = self.alb([128, 8, 512], BF16)
        Gf = self.ring(2, [128, 512], F32)
        gt = self.ring(2, [128, 512], F32)
        wg = self.din("w_gate" + sfx, [D, 4 * D]).rearrange("(k p) n -> p k n", p=128)
        for dc in range(8):
            gf = Gf.next()
            for n in range(4):
                ps = self.proj(wg, n * D + dc * 128, nt, wring, self.RS)
                ga = gt.next()
                self.ACT(ga.a[:, :nt], ps.a[:, :nt], AF.Sigmoid, [ps.k, self.veck], [ga.k],
                         bias=v[:, 64 + n * 8 + dc:65 + n * 8 + dc])
                pb = self.RX.next()
                for w in range(2):
                    self.MM(pb.a[:, :nt], wbr.a[:, 2 * n + w, dc * 128:(dc + 1) * 128], ysT.a[:, 2 * n + w, :nt], w == 0, w == 1,
                            [wbr.k, ysT.k], [pb.k], sig=(w == 1))
                if n == 0:
                    self.TT("dve", gf.a[:, :nt], ga.a[:, :nt], pb.a[:, :nt], ALU.mult, [ga.k, pb.k], [gf.k])
                else:
                    t = tf.next()
                    self.TT("dve", t.a[:, :nt], ga.a[:, :nt], pb.a[:, :nt], ALU.mult, [ga.k, pb.k], [t.k])
                    if n < 3:
                        self.TT("pool", gf.a[:, :nt], gf.a[:, :nt], t.a[:, :nt], ALU.add, [gf.k, t.k], [gf.k])
                    else:
                        self.TT("pool", G.a[:, dc, :nt], gf.a[:, :nt], t.a[:, :nt], ALU.add, [gf.k, t.k], [G.k])
        wo = self.din("w_out" + sfx, [D, D]).rearrange("(k p) n -> p k n", p=128)
        for dc in range(8):
            wt = wring.next()
            self.DMA("pool", wt.a[:, :, :], wo[:, :, dc * 128:(dc + 1) * 128], W=[wt.k])
            ps = self.RX.next()
            for k in range(8):
                self.MM(ps.a[:, :nt], wt.a[:, k, :], G.a[:, k, :nt], k == 0, k == 7, [wt.k, G.k], [ps.k], sig=(k == 7))
            self.STT(self.xT[:, dc, t0:t0 + nt], ps.a[:, :nt], self.dr(2, dc, ctx), self.xT[:, dc, t0:t0 + nt],
                     ALU.mult, ALU.add, [ps.k, self.drv.k, self.xk[b]], [self.xk[b]])
        if self.dbg and b in (0, 4):
            self.dump("xmid%d_%d" % (l, b), self.xT[:, :, t0:t0 + nt], self.xk[b], [128, 8, nt])

        self.peer(l, b)
        if self.dbg and b in (0, 4):
            self.dump("xout%d_%d" % (l, b), self.xT[:, :, t0:t0 + nt], self.xk[b], [128, 8, nt])

    def peer(self, l, b):
        sfx = "_l%d" % l
        t0, nt = self.blk(b)
        ctx = (b == 4)
        ntile = nt // 128
        self.arena_reset()
        sc = self.alb([128, 4, 16, 128], F32)
        stat = self.alb([128, 4, 8, 4], F32)
        keep = self.aoff
        tf = self.ring(3, [128, 512], F32)
        tb = self.ring(3, [128, 512], BF16)
        self.norm_mod(b, 3, 4, tf, tb)
        wring = self.ring(3, [128, 8, 128], BF16)
        qp = self.alb([128, 16, 512], BF16)
        wq = self.din("peer_wq" + sfx, [D, 2048]).rearrange("(k p) n -> p k n", p=128)
        for j in range(16):
            ps = self.proj(wq, j * 128, nt, wring)
            self.CP("act" if j % 2 else "dve", qp.a[:, j, :nt], ps.a[:, :nt], [ps.k], [qp.k])
        for tt in range(ntile):
            for q4 in range(4):
                ps = self.RS.next()
                for i in range(4):
                    j = q4 * 4 + i
                    self.MM(ps.a[:, i * 128:(i + 1) * 128], qp.a[:, j, tt * 128:(tt + 1) * 128], self.keysT.a[:, j, :],
                            True, True, [qp.k, self.keysT.k], [ps.k], sig=(i == 3))
                self.CP("act" if q4 % 2 else "dve", sc.a[:, tt, q4 * 4:(q4 + 1) * 4, :],
                        ps.a[:, :].rearrange("p (a b) -> p a b", a=4), [ps.k], [sc.k])
        top = self.alb([128, 16, 16], F32)
        mx = self.alb([128, 16, 8], F32)
        wk = self.ring(2, [128, 256], F32)
        cand = self.alb([128, 256], F32)
        best = self.alb([128, 16], F32)
        c3 = cand.a[:, :].rearrange("p (a b) -> p a b", a=16)
        for tt in range(ntile):
            for j in range(16):
                self.MAX8(mx.a[:, j, :], sc.a[:, tt, j, :], [sc.k], [mx.k])
                self.TS("dve", mx.a[:, j, 1:2], mx.a[:, j, 0:1], -1.0, None, ALU.mult, None, [mx.k], [mx.k])
                self.ACT(sc.a[:, tt, j, :], sc.a[:, tt, j, :], AF.Exp, [sc.k, mx.k], [sc.k], bias=mx.a[:, j, 1:2])
            for j in range(16):
                self.MAX8(top.a[:, j, 0:8], sc.a[:, tt, j, :], [sc.k], [top.k])
                w1 = wk.next()
                self.MREP(w1.a[:, 0:128], top.a[:, j, 0:8], sc.a[:, tt, j, :], [sc.k, top.k], [w1.k])
                self.MAX8(top.a[:, j, 8:16], w1.a[:, 0:128], [w1.k], [top.k])
            for h in range(8):
                sa = stat.a[:, tt, h, :]
                b0 = top.a[:, 2 * h, :].unsqueeze(2).to_broadcast([128, 16, 16])
                b1 = top.a[:, 2 * h + 1, :].unsqueeze(1).to_broadcast([128, 16, 16])
                for rnd in range(2):
                    self.TT("pool", c3, b0, b1, ALU.mult, [top.k], [cand.k])
                    self.MAX8(best.a[:, 0:8], cand.a[:, :], [cand.k], [best.k])
                    w1 = wk.next()
                    self.MREP(w1.a[:, :], best.a[:, 0:8], cand.a[:, :], [cand.k, best.k], [w1.k])
                    self.MAX8(best.a[:, 8:16], w1.a[:, :], [w1.k], [best.k])
                    if rnd == 0:
                        self.S.op("dve", (lambda sa=sa: lambda e: e.tensor_reduce(out=sa[:, 2:3], in_=best.a[:, :], axis=AX.X,
                                                                                   op=ALU.add))(), reads=[best.k], writes=[stat.k])
                        self.RCP(sa[:, 1:2], sa[:, 2:3], [stat.k], [stat.k])
                        self.TS("dve", sc.a[:, tt, 2 * h, :], sc.a[:, tt, 2 * h, :], sa[:, 1:2], None, ALU.mult, None,
                                [sc.k, stat.k], [sc.k])
                        self.TS("dve", top.a[:, 2 * h, :], top.a[:, 2 * h, :], sa[:, 1:2], None, ALU.mult, None,
                                [top.k, stat.k], [top.k])
                    else:
                        self.CP("dve", sa[:, 0:1], best.a[:, 15:16], [best.k], [stat.k])
        if self.dbg and b == 0:
            self.dump("pstat%d" % l, stat.a[:, :, :, :], stat.k, [128, 4, 8, 4])
        self.S.barrier()
        self.aoff = keep
        acc = self.alb([128, 4, 1024], F32)
        zb = self.alb([128, 512], BF16)
        self.MSET("dve", zb.a[:, :], 0.0, [zb.k])
        GE = 512
        NG = 16384 // GE
        utr = self.ring(2, [128, 8, GE], BF16)
        vwr = self.ring(2, [128, 4, 1024], BF16)
        tP = self.ring(6, [128, 512], F32)
        tM = self.ring(6, [128, 512], BF16)
        Ag = self.ring(2, [128, 512], BF16)
        WAT = self.ring(2, [128, 4, 128], BF16)
        uT = self.din("uT" + sfx, [D, 16384]).rearrange("(k p) e -> p k e", p=128)
        vv = self.din("peer_v" + sfx, [16384, D]).rearrange("(g c p) d -> g p c d", c=4, p=128)

        def load(g):
            ut = utr.next()
            self.DMA("pool", ut.a[:, :, :], uT[:, :, g * GE:(g + 1) * GE], W=[ut.k])
            vw = vwr.next()
            self.DMA("pool", vw.a[:, :, :], vv[g], W=[vw.k])
            return ut, vw
        items = [(g, tt) for g in range(NG) for tt in range(ntile)]
        loads = {0: load(0)}
        st_ = {}

        def emit_AT(i, c4):
            g, tt = items[i]
            ut = loads[g][0]
            if c4 == 0:
                st_[i] = {"pa": self.RS.next()}
            pa = st_[i]["pa"]
            for k in range(8):
                self.MM(pa.a[:, c4 * 128:(c4 + 1) * 128], ut.a[:, k, c4 * 128:(c4 + 1) * 128],
                        self.hT.a[:, k, tt * 128:(tt + 1) * 128], k == 0, k == 7, [self.hT.k, ut.k], [pa.k], sig=(k == 7))
            if c4 == 3:
                ag = Ag.next()
                self.ACT(ag.a[:, :], pa.a[:, :], AF.Gelu, [pa.k], [ag.k])
                st_[i]["ag"] = ag

        def emit_wat(i):
            d = st_[i]
            wat = WAT.next()
            self.TT("dve", wat.a[:, :, :], d["ag"].a[:, :].rearrange("p (a b) -> p a b", a=4),
                    d["pw"].a[:, :].rearrange("p (a b) -> p a b", a=4), ALU.mult, [d["ag"].k, d["pw"].k], [wat.k])
            d["wat"] = wat

        def emit_out(i, half):
            g, tt = items[i]
            vw = loads[g][1]
            wat = st_[i]["wat"]
            po = (self.RO if half == 0 else self.RX).next()
            for c4 in range(4):
                self.MM(po.a[:, :], wat.a[:, c4, :], vw.a[:, c4, half * 512:(half + 1) * 512], c4 == 0, c4 == 3,
                        [wat.k, vw.k], [po.k], sig=(c4 == 3))
            if g == 0:
                self.CP("dve", acc.a[:, tt, half * 512:(half + 1) * 512], po.a[:, :], [po.k], [acc.k])
            else:
                self.TT("dve", acc.a[:, tt, half * 512:(half + 1) * 512], acc.a[:, tt, half * 512:(half + 1) * 512],
                        po.a[:, :], ALU.add, [acc.k, po.k], [acc.k])
        for c4 in range(4):
            emit_AT(0, c4)
        for i, (g, tt) in enumerate(items):
            i0 = g * 4
            if tt == min(1, ntile - 1) and g + 1 < NG and (g + 1) not in loads:
                loads[g + 1] = load(g + 1)
            if i > 0:
                emit_wat(i - 1)
            pw = self.RS.next()
            st_[i]["pw"] = pw
            self.MM(pw.a[:, :], zb.a[:, 0:128], zb.a[:, :], True, False, [zb.k], [pw.k], sig=False)
            for h in range(8):
                pp = tP.next()
                p3 = pp.a[:, :].rearrange("p (i j) -> p i j", i=4)
                self.TT("pool", p3, sc.a[:, tt, 2 * h, i0:i0 + 4].unsqueeze(2).to_broadcast([128, 4, 128]),
                        sc.a[:, tt, 2 * h + 1, :].unsqueeze(1).to_broadcast([128, 4, 128]), ALU.mult, [sc.k], [pp.k])
                tm = tM.next()
                self.STT(tm.a[:, :], pp.a[:, :], stat.a[:, tt, h, 0:1], pp.a[:, :], ALU.is_ge, ALU.mult,
                         [pp.k, stat.k], [tm.k])
                for c4 in range(4):
                    self.MM(pw.a[:, c4 * 128:(c4 + 1) * 128], tm.a[:, c4 * 128:(c4 + 1) * 128], self.identb.a[:, :],
                            False, h == 7, [tm.k, self.identb.k], [pw.k], sig=(c4 == 3))
                if h < 4 and i + 1 < len(items):
                    emit_AT(i + 1, h)
                if h in (4, 5) and i > 0:
                    emit_out(i - 1, h - 4)
                    if h == 5:
                        del st_[i - 1]
        last = len(items) - 1
        emit_wat(last)
        emit_out(last, 0)
        emit_out(last, 1)
        for tt in range(ntile):
            for half in range(2):
                pt4 = self.RX.next()
                for c4 in range(4):
                    dc = half * 4 + c4
                    self.TR(pt4.a[:, c4 * 128:(c4 + 1) * 128], acc.a[:, tt, dc * 128:(dc + 1) * 128], self.identf.a[:, :],
                            [acc.k, self.identf.k], [pt4.k], sig=(c4 == 3))
                for c4 in range(4):
                    dc = half * 4 + c4
                    xs = self.xT[:, dc, t0 + tt * 128:t0 + (tt + 1) * 128]
                    self.STT(xs, pt4.a[:, c4 * 128:(c4 + 1) * 128], self.dr(5, dc, ctx), xs, ALU.mult, ALU.add,
                             [pt4.k, self.drv.k, self.xk[b]], [self.xk[b]])

    def final_norm(self):
        self.arena_reset()
        tf = self.ring(4, [128, 512], F32)
        tb = self.ring(3, [128, 512], BF16)
        oT = self.dout("outT", [D, TOK]).rearrange("(c p) t -> p c t", p=128)
        ob = self.ring(2, [128, 8, 512], F32)
        rsr = self.ring(2, [128, 512], F32)
        for b in range(self.own_blocks):
            t0, nt = self.blk(b)
            ps = self.RS.next()
            for c in range(8):
                sq = tb.next()
                self.ACT(sq.a[:, :nt], self.xT[:, c, t0:t0 + nt], AF.Square, [self.xk[b]], [sq.k])
                self.MM(ps.a[:, :nt], self.onesb.a[:, :], sq.a[:, :nt], c == 0, c == 7, [self.onesb.k, sq.k], [ps.k])
            rs = rsr.next()
            self.ACT(rs.a[:, :nt], ps.a[:, :nt], AF.Sqrt, [ps.k, self.cstk], [rs.k], bias=self.cst[:, 0:1], scale=1.0 / D)
            self.RCP(rs.a[:, :nt], rs.a[:, :nt], [rs.k], [rs.k])
            o = ob.next()
            for c in range(8):
                t = tf.next()
                self.TT("dve", t.a[:, :nt], self.xT[:, c, t0:t0 + nt], rs.a[:, :nt], ALU.mult, [self.xk[b], rs.k], [t.k])
                self.TS("pool", o.a[:, c, :nt], t.a[:, :nt], self.vec[:, 96 + c:97 + c], None, ALU.mult, None,
                        [t.k, self.veck], [o.k])
            self.DMA("sp", oT[:, :, t0:t0 + nt], o.a[:, :, :], R=[o.k], W=[Tk()])


def _cols(vv, n):
    return np.ascontiguousarray(np.asarray(vv, np.float32).reshape(n, 128).T)


def _consts():
    ident = np.eye(128, dtype=np.float32)

    def perm(dh):
        q = dh // 4
        P = np.zeros((128, 128), np.float32)
        for blk in range(128 // dh):
            o = blk * dh
            for i in range(q):
                P[o + q + i, o + i] = -1.0
                P[o + i, o + q + i] = 1.0
                P[o + 3 * q + i, o + 2 * q + i] = -1.0
                P[o + 2 * q + i, o + 3 * q + i] = 1.0
        return P
    blk64 = np.zeros((128, 128), np.float32)
    blk64[0:64, 0:64] = 1.0
    blk64[64:128, 64:128] = 1.0
    cmat = np.stack([ident, perm(64), perm(32), blk64])
    jj = np.arange(128)[:, None]
    ii = np.arange(128)[None, :]
    swm = np.zeros((8, 128, 512), np.float32)
    for j in range(6):
        for qb in range(4):
            dlt = (j - 1) - qb
            if dlt == -1:
                m = (jj >= ii)
            elif dlt == 0:
                m = np.ones((128, 128), bool)
            elif dlt == 1:
                m = (jj <= ii)
            else:
                m = np.zeros((128, 128), bool)
            swm[j, :, qb * 128:(qb + 1) * 128] = m
    return cmat, swm


def _rope_tables(core):
    t = np.arange(TOK, dtype=np.int64) + core * TOK
    row = (t // GW).astype(np.float32)
    col = (t % GW).astype(np.float32)
    out = np.zeros((4, 128, TOK), np.float32)
    for ti, dh in ((0, 64), (2, 32)):
        nf = dh // 4
        inv = (np.float32(10000.0) ** (-(np.arange(nf, dtype=np.float32)) / np.float32(nf))).astype(np.float32)
        ang_r = (row[:, None] * inv[None, :]).astype(np.float32)
        ang_c = (col[:, None] * inv[None, :]).astype(np.float32)
        ang = np.concatenate([ang_r, ang_r, ang_c, ang_c], axis=-1)
        reps = 128 // dh
        out[ti] = np.tile(np.cos(ang).T, (reps, 1))
        out[ti + 1] = np.tile(np.sin(ang).T, (reps, 1))
    return out


def _bandneg(core):
    bn = np.zeros((512,), np.float32)
    r0 = core * ROWS
    for r in range(ROWS):
        rg = r0 + r
        start = min(max(rg - 4, 0), 256 - 8)
        for e in range(15):
            krg = rg - e + 7
            ok = (start <= krg <= start + 7)
            bn[r * 16 + e] = 0.0 if ok else NEG
    return np.ascontiguousarray(np.tile(bn[None, :], (64, 1)))


def _layer_inputs(inp, l):
    sfx = "_l%d" % l
    f = lambda a: np.ascontiguousarray(np.asarray(a, np.float32))
    vec = np.zeros((128, 512), np.float32)
    vec[:, 0:8] = _cols(inp["norm1_w"][l], 8)
    vec[:, 8:16] = _cols(inp["norm2_w"][l], 8)
    vec[:, 16:64] = _cols(inp["ada_b"][l], 48)
    vec[:, 64:96] = _cols(inp["b_gate"][l], 32)
    vec[:, 96:104] = _cols(inp["final_norm_w"], 8)
    vec[:, 104:112] = _cols(np.asarray(inp["c"]).reshape(-1), 8)
    vec[:, 112:120] = _cols(inp["c_ctx"], 8)
    vec[:, 120:124] = np.asarray(inp["swa_sink"][l], np.float32)[None, :]
    vec[:, 124] = np.tile(np.asarray(inp["gqa_qk_norm_w"][l][0], np.float32), 2)
    vec[:, 125] = np.tile(np.asarray(inp["gqa_qk_norm_w"][l][1], np.float32), 2)
    vec[:, 126] = np.tile(np.asarray(inp["diff_subln_w"][l], np.float32), 2)
    vec[:, 128:256] = np.asarray(inp["diff_lam"][l], np.float32).reshape(1, 128)
    w_in = np.array(inp["w_in"][l], np.float32)
    for base in (1536, 2048):
        blk = w_in[:, base:base + 256].reshape(D, 4, 64)
        w_in[:, base:base + 256] = blk[:, [0, 2, 1, 3], :].reshape(D, 256)
    w_v = np.concatenate([w_in[:, 512:768], w_in[:, 1280:1536], w_in[:, 1920:2048], w_in[:, 2432:2560]], axis=1)
    rpb = np.asarray(inp["na_rpb"][l], np.float32)
    kc = np.arange(64)[:, None]
    qc = np.arange(64)[None, :]
    coff = np.clip(kc - qc, -15, 15) + 15
    cs = np.clip(qc - 8, 0, 48)
    ok = (kc >= cs) & (kc < cs + 16)
    rp = np.full((4, 64, 15, 64), NEG, np.float32)
    for e in range(15):
        val = rpb[:, 14 - e, :][:, coff]
        rp[:, :, e, :] = np.where(ok[None], val, NEG)
    keysT = np.ascontiguousarray(np.transpose(np.asarray(inp["peer_keys"][l], np.float32), (3, 1, 0, 2)).reshape(128, 16, 128))
    return {
        "vec" + sfx: vec, "ada_w" + sfx: f(inp["ada_w"][l]), "w_in" + sfx: np.ascontiguousarray(w_in),
        "w_v" + sfx: np.ascontiguousarray(w_v), "rpbr" + sfx: rp, "keysT" + sfx: keysT,
        "w_branch" + sfx: f(inp["w_branch"][l]), "w_gate" + sfx: f(inp["w_gate"][l]), "w_out" + sfx: f(inp["w_out"][l]),
        "peer_wq" + sfx: f(inp["peer_wq"][l]), "uT" + sfx: np.ascontiguousarray(np.asarray(inp["peer_u"][l], np.float32).T),
        "peer_v" + sfx: f(inp["peer_v"][l]),
    }


def _gather_kv(results, l):
    sfx = "_l%d" % l
    bf = ml_dtypes.bfloat16
    kT = np.concatenate([np.asarray(r["kT_own" + sfx]) for r in results], axis=2)
    VAd = np.concatenate([np.asarray(r["VAd_own" + sfx]) for r in results], axis=2)
    NV = np.concatenate([np.asarray(r["NV_own" + sfx]) for r in results], axis=2)
    SV = np.concatenate([np.asarray(r["SV_own" + sfx]) for r in results], axis=2)
    KTd = np.ascontiguousarray(kT[[2, 3, 5]])
    outs = []
    for i in range(NCORE):
        r0 = i * ROWS
        nak = np.zeros((2, 128, NWIN * 64), bf)
        nav = np.zeros((4, 64, NWIN, 128), bf)
        lo = max(r0 - 7, 0)
        hi = min(r0 + ROWS + 7, 256)
        nak[:, :, (lo - (r0 - 7)) * 64:(hi - (r0 - 7)) * 64] = kT[0:2, :, lo * 64:hi * 64]
        nav[:, :, lo - (r0 - 7):hi - (r0 - 7), :] = NV[:, :, lo:hi, :]
        swk = np.zeros((128, SWT), bf)
        t_lo = max(i * TOK - 128, 0)
        t_hi = min((i + 1) * TOK + 128, SEQ)
        swk[:, t_lo - (i * TOK - 128):t_hi - (i * TOK - 128)] = kT[4][:, t_lo:t_hi]
        swv = np.zeros((4, 128, 18, 128), bf)
        k_lo = max(i * 16 - 1, 0)
        k_hi = min((i + 1) * 16 + 1, 128)
        swv[:, :, k_lo - (i * 16 - 1):k_hi - (i * 16 - 1), :] = SV[:, :, k_lo:k_hi, :]
        outs.append({"KTd" + sfx: KTd, "VAd" + sfx: VAd, "naKwin" + sfx: nak, "naVwin" + sfx: nav,
                     "swKwin" + sfx: swk, "swVwin" + sfx: swv})
    return outs


def _percore(swm):
    out = []
    for i in range(NCORE):
        m = swm.copy()
        if i > 0:
            m[6] = swm[0]
        if i < NCORE - 1:
            m[7] = swm[5]
        sel = np.zeros((128, 16), np.float32)
        if i > 0:
            sel[:, i - 1] = 1.0
        if i < NCORE - 1:
            sel[:, 8 + i + 1] = 1.0
        out.append({"ropeT": _rope_tables(i), "bandneg": _bandneg(i), "swm": m, "sel": sel})
    return out


_PROG = {}


def _prog(key, *args, **kw):
    if key not in _PROG:
        _PROG[key] = KB(*args, **kw)
    return _PROG[key]


def _run(kb, provs):
    in_maps = []
    for i in range(NCORE):
        m = {}
        for name in kb.ins:
            for p in provs:
                src = p[i] if isinstance(p, list) else p
                if name in src:
                    m[name] = src[name]
                    break
            else:
                raise KeyError(name)
        in_maps.append(m)
    res = run_bass_kernel_spmd(kb.nc, in_maps, core_ids=list(range(NCORE)))
    return res.results


def kernel_unfused(**inp):
    x = np.asarray(inp["x"], np.float32)[0]
    ctx = np.asarray(inp["ctx"], np.float32)[0]
    cmat, swm = _consts()
    common = {"cmat": cmat}
    percore = _percore(swm)
    L = [_layer_inputs(inp, 0), _layer_inputs(inp, 1)]
    xin = [{"xT_in": np.ascontiguousarray(np.concatenate([x[i * TOK:(i + 1) * TOK], ctx], axis=0).T)} for i in range(NCORE)]
    r0 = _run(_prog("s0", [0], [], False), [xin, common, percore, L[0]])
    kv0 = _gather_kv(r0, 0)
    r1 = _run(_prog("s1", [1], [0], False), [xin, common, percore, L[0], L[1], kv0])
    kv1 = _gather_kv(r1, 1)
    xin2 = [{"xT_in": np.asarray(r["xT_out"])} for r in r1]
    r2 = _run(_prog("s2", [], [1], True), [xin2, common, percore, L[1], kv1])
    out = np.concatenate([np.asarray(r["outT"]).T for r in r2], axis=0)
    return out[None].astype(np.float32)


def kernel(**inp):
    x = np.asarray(inp["x"], np.float32)[0]
    ctx = np.asarray(inp["ctx"], np.float32)[0]
    cmat, swm = _consts()
    common = {"cmat": cmat}
    percore = _percore(swm)
    L = [_layer_inputs(inp, 0), _layer_inputs(inp, 1)]
    xin = [{"xT_in": np.ascontiguousarray(np.concatenate([x[i * TOK:(i + 1) * TOK], ctx], axis=0).T)} for i in range(NCORE)]
    r = _run(_prog("fused", [], [], True, fused=True), [xin, common, percore, L[0], L[1]])
    out = np.concatenate([np.asarray(q["outT"]).T for q in r], axis=0)
    return out[None].astype(np.float32)
```
